# Optimizing a Trainium2 kernel written in Bass

```python
import math
import jax
import jax.numpy as jnp
from jax import lax
import numpy as np

D_MODEL = 1024
BATCH = 8
SEQ = 2048
DEPTH = 1

EPS = 1e-6
GDN_HEADS = 4
GDN_DK = 128
GDN_DV = 128
GDN_CONV = 4
GDN_CHUNK = 64
RET_HEADS = 4
RET_DK = 128
RET_DV = 256
RET_CHUNK = 128
ROPE_BASE = 10000.0
N_GROUPS = 4
EXPERTS_PER_GROUP = 8
N_EXPERTS = N_GROUPS * EXPERTS_PER_GROUP
TOP_K = 2
D_EXPERT = 512

GDN_QK = GDN_HEADS * GDN_DK
GDN_V = GDN_HEADS * GDN_DV
GDN_CONV_DIM = 2 * GDN_QK + GDN_V
RET_QK = RET_HEADS * RET_DK
RET_V = RET_HEADS * RET_DV
SPLITS = (GDN_QK, GDN_QK, GDN_V, GDN_HEADS, GDN_HEADS, GDN_V, RET_QK, RET_QK, RET_V, RET_V, D_MODEL, D_MODEL)
D_IN = sum(SPLITS)

kernel_name = "hybrid_gdn_retention_hmoe"


def rms_norm(x, w=None):
    xf = x.astype(jnp.float32)
    y = xf * lax.rsqrt(jnp.mean(xf * xf, axis=-1, keepdims=True) + EPS)
    if w is not None:
        y = y * w.astype(jnp.float32)
    return y.astype(x.dtype)


def l2_norm(x):
    return x * lax.rsqrt(jnp.sum(x * x, axis=-1, keepdims=True) + EPS)


def to_chunks(a, chunk):
    b, t, h = a.shape[:3]
    a = a.reshape((b, t // chunk, chunk, h) + a.shape[3:])
    return jnp.moveaxis(a, (1, 3), (0, 2))


def from_chunks(a):
    n, b, h, c, d = a.shape
    return jnp.moveaxis(a, (0, 2), (1, 3)).reshape(b, n * c, h, d)


def causal_depthwise_conv(x, w):
    k, c = w.shape
    return lax.conv_general_dilated(
        x, w[:, None, :].astype(x.dtype), window_strides=(1,), padding=[(k - 1, 0)],
        dimension_numbers=("NWC", "WIO", "NWC"), feature_group_count=c)


def gated_delta_rule(q, k, v, g, beta):
    bsz, t, h, dk = q.shape
    dv = v.shape[-1]
    c = GDN_CHUNK
    q = to_chunks(q, c) * (dk ** -0.5)
    k = to_chunks(k, c)
    v = to_chunks(v, c)
    g = to_chunks(g, c)
    beta = to_chunks(beta, c)
    gc = jnp.cumsum(g, axis=-1)
    causal = jnp.tril(jnp.ones((c, c), dtype=bool))
    strict = jnp.tril(jnp.ones((c, c), dtype=bool), -1)
    diff = gc[..., :, None] - gc[..., None, :]
    decay = jnp.where(causal, jnp.exp(jnp.where(causal, diff, 0.0)), 0.0)
    kb = k * beta[..., None]
    lower = jnp.where(strict, jnp.einsum('nbhid,nbhjd->nbhij', kb, k) * decay, 0.0)
    rhs = jnp.concatenate([v * beta[..., None], kb * jnp.exp(gc)[..., None]], axis=-1)
    sol = lax.linalg.triangular_solve(lower, rhs, left_side=True, lower=True, unit_diagonal=True)
    u, w = sol[..., :dv], sol[..., dv:]
    attn = jnp.where(causal, jnp.einsum('nbhid,nbhjd->nbhij', q, k) * decay, 0.0)
    qg = q * jnp.exp(gc)[..., None]
    kd = k * jnp.exp(gc[..., -1:] - gc)[..., None]
    glast = jnp.exp(gc[..., -1])

    def step(state, inp):
        qg_i, w_i, u_i, attn_i, kd_i, gl_i = inp
        v_new = u_i - jnp.einsum('bhck,bhkv->bhcv', w_i, state)
        o = jnp.einsum('bhck,bhkv->bhcv', qg_i, state) + jnp.einsum('bhij,bhjv->bhiv', attn_i, v_new)
        state = state * gl_i[..., None, None] + jnp.einsum('bhck,bhcv->bhkv', kd_i, v_new)
        return state, o

    s0 = jnp.zeros((bsz, h, dk, dv), jnp.float32)
    _, o = lax.scan(step, s0, (qg, w, u, attn, kd, glast))
    return from_chunks(o)


def gdn_branch(q, k, v, a, b, z, conv_w, A_log, dt_bias, norm_w):
    bsz, t, _ = q.shape
    f32 = jnp.float32
    qkv = jax.nn.silu(causal_depthwise_conv(jnp.concatenate([q, k, v], axis=-1), conv_w)).astype(f32)
    q, k, v = jnp.split(qkv, [GDN_QK, 2 * GDN_QK], axis=-1)
    q = l2_norm(q.reshape(bsz, t, GDN_HEADS, GDN_DK))
    k = l2_norm(k.reshape(bsz, t, GDN_HEADS, GDN_DK))
    v = v.reshape(bsz, t, GDN_HEADS, GDN_DV)
    beta = jax.nn.sigmoid(b.astype(f32))
    g = -jnp.exp(A_log.astype(f32)) * jax.nn.softplus(a.astype(f32) + dt_bias.astype(f32))
    o = gated_delta_rule(q, k, v, g, beta)
    o = rms_norm(o, norm_w) * jax.nn.silu(z.astype(f32).reshape(bsz, t, GDN_HEADS, GDN_DV))
    return o.reshape(bsz, t, GDN_V)


def xpos_rotate(x):
    t, d = x.shape[1], x.shape[-1]
    inv_freq = 1.0 / (ROPE_BASE ** jnp.linspace(0.0, 1.0, d // 2, dtype=jnp.float32))
    ang = jnp.arange(t, dtype=jnp.float32)[:, None] * inv_freq[None, :]
    sin = jnp.repeat(jnp.sin(ang), 2, axis=-1)[None, :, None, :]
    cos = jnp.repeat(jnp.cos(ang), 2, axis=-1)[None, :, None, :]
    rot = jnp.stack([-x[..., 1::2], x[..., 0::2]], axis=-1).reshape(x.shape)
    return x * cos + rot * sin


def retention_chunkwise(q, k, v):
    bsz, t, h, dk = q.shape
    dv = v.shape[-1]
    c = RET_CHUNK
    log_gamma = jnp.log(1.0 - 2.0 ** (-5.0 - jnp.arange(h, dtype=jnp.float32)))
    q, k, v = to_chunks(q, c), to_chunks(k, c), to_chunks(v, c)
    idx = jnp.arange(c, dtype=jnp.float32)
    causal = jnp.tril(jnp.ones((c, c), dtype=bool))
    rel = jnp.where(causal, idx[:, None] - idx[None, :], 0.0)
    inner_decay = jnp.where(causal, jnp.exp(rel[None] * log_gamma[:, None, None]), 0.0)
    scores = jnp.einsum('nbhid,nbhjd->nbhij', q, k) * inner_decay
    o_intra = jnp.einsum('nbhij,nbhjv->nbhiv', scores, v)
    k_decay = jnp.exp(log_gamma[:, None] * (c - 1.0 - idx)[None, :])
    q_decay = jnp.exp(log_gamma[:, None] * (idx + 1.0)[None, :])
    kv = jnp.einsum('nbhck,hc,nbhcv->nbhkv', k, k_decay, v)
    chunk_decay = jnp.exp(log_gamma * c)

    def step(state, kv_n):
        return state * chunk_decay[:, None, None] + kv_n, state

    _, state_prev = lax.scan(step, jnp.zeros((bsz, h, dk, dv), jnp.float32), kv)
    o_inter = jnp.einsum('nbhck,hc,nbhkv->nbhcv', q, q_decay, state_prev)
    return from_chunks(o_intra + o_inter)


def retention_branch(q, k, v, gate):
    bsz, t, _ = q.shape
    f32 = jnp.float32
    q = xpos_rotate(q.astype(f32).reshape(bsz, t, RET_HEADS, RET_DK))
    k = xpos_rotate(k.astype(f32).reshape(bsz, t, RET_HEADS, RET_DK)) * (RET_DK ** -0.5)
    v = v.astype(f32).reshape(bsz, t, RET_HEADS, RET_DV)
    o = rms_norm(retention_chunkwise(q, k, v))
    o = o * jax.nn.silu(gate.astype(f32).reshape(bsz, t, RET_HEADS, RET_DV))
    return o.reshape(bsz, t, RET_V)


def hier_moe(x, w_group, b_group, w_expert, b_expert, w_gate, w_up, w_down):
    bsz, t, d = x.shape
    f32 = jnp.float32
    xt = x.reshape(-1, d)
    g_logits = (xt @ w_group).astype(f32) + b_group.astype(f32)
    g_w, g_idx = lax.top_k(jax.nn.softmax(g_logits, axis=-1), 1)
    e_logits = ((xt @ w_expert).astype(f32) + b_expert.astype(f32)).reshape(-1, N_GROUPS, EXPERTS_PER_GROUP)
    e_logits = jnp.take_along_axis(e_logits, g_idx[:, :, None], axis=1)[:, 0]
    e_val, e_idx = lax.top_k(e_logits, TOP_K)
    e_w = jax.nn.softmax(e_val, axis=-1) * g_w
    e_global = g_idx * EXPERTS_PER_GROUP + e_idx
    combine = jnp.sum(jax.nn.one_hot(e_global, N_EXPERTS, dtype=f32) * e_w[..., None], axis=1)
    y = jnp.zeros(xt.shape, f32)
    for e in range(N_EXPERTS):
        hid = jax.nn.silu(xt @ w_gate[e]) * (xt @ w_up[e])
        y = y + combine[:, e:e + 1] * (hid @ w_down[e]).astype(f32)
    return y.astype(x.dtype).reshape(bsz, t, d)


def setup_inputs(seed: int = 0) -> dict:
    key = jax.random.key(seed)
    ks = jax.random.split(key, 20)
    f32 = jnp.float32
    L = DEPTH

    def nrm(k, shape, scale):
        return jax.random.normal(k, shape, f32) * scale

    x = jax.random.normal(ks[0], (BATCH, SEQ, D_MODEL), f32)
    norm_mix_w = 1.0 + nrm(ks[1], (L, D_MODEL), 0.02)
    w_in = nrm(ks[2], (L, D_MODEL, D_IN), D_MODEL ** -0.5)
    conv_w = nrm(ks[3], (L, GDN_CONV, GDN_CONV_DIM), GDN_CONV ** -0.5)
    A_log = jnp.log(jax.random.uniform(ks[4], (L, GDN_HEADS), f32, 1.0, 16.0))
    dt = jnp.exp(jax.random.uniform(ks[5], (L, GDN_HEADS), f32, math.log(1e-3), math.log(1e-1)))
    dt_bias = dt + jnp.log(-jnp.expm1(-dt))
    gdn_norm_w = 1.0 + nrm(ks[6], (L, GDN_DV), 0.02)
    w_up_gdn = nrm(ks[7], (L, GDN_V, D_MODEL), GDN_V ** -0.5)
    w_up_ret = nrm(ks[8], (L, RET_V, D_MODEL), RET_V ** -0.5)
    w_out = nrm(ks[9], (L, D_MODEL, D_MODEL), D_MODEL ** -0.5)
    norm_ffn_w = 1.0 + nrm(ks[10], (L, D_MODEL), 0.02)
    w_group = nrm(ks[11], (L, D_MODEL, N_GROUPS), D_MODEL ** -0.5)
    b_group = nrm(ks[12], (L, N_GROUPS), 0.01)
    w_expert = nrm(ks[13], (L, D_MODEL, N_EXPERTS), D_MODEL ** -0.5)
    b_expert = nrm(ks[14], (L, N_EXPERTS), 0.01)
    w_gate = nrm(ks[15], (L, N_EXPERTS, D_MODEL, D_EXPERT), D_MODEL ** -0.5)
    w_up = nrm(ks[16], (L, N_EXPERTS, D_MODEL, D_EXPERT), D_MODEL ** -0.5)
    w_down = nrm(ks[17], (L, N_EXPERTS, D_EXPERT, D_MODEL), D_EXPERT ** -0.5)
    norm_final_w = 1.0 + nrm(ks[18], (D_MODEL,), 0.02)
    return {"x": x, "norm_mix_w": norm_mix_w, "w_in": w_in, "conv_w": conv_w, "A_log": A_log,
            "dt_bias": dt_bias, "gdn_norm_w": gdn_norm_w, "w_up_gdn": w_up_gdn, "w_up_ret": w_up_ret,
            "w_out": w_out, "norm_ffn_w": norm_ffn_w, "w_group": w_group, "b_group": b_group,
            "w_expert": w_expert, "b_expert": b_expert, "w_gate": w_gate, "w_up": w_up,
            "w_down": w_down, "norm_final_w": norm_final_w}


def reference(x, norm_mix_w, w_in, conv_w, A_log, dt_bias, gdn_norm_w, w_up_gdn, w_up_ret, w_out,
              norm_ffn_w, w_group, b_group, w_expert, b_expert, w_gate, w_up, w_down, norm_final_w):
    dt = x.dtype
    split_points = np.cumsum(np.array(SPLITS))[:-1].tolist()
    h = x
    for l in range(DEPTH):
        u = rms_norm(h, norm_mix_w[l])
        proj = jnp.einsum('btd,de->bte', u, w_in[l])
        (gq, gk, gv, ga, gb, gz, rq, rk, rv, rg, m_a, m_b) = jnp.split(proj, split_points, axis=-1)
        y_a = gdn_branch(gq, gk, gv, ga, gb, gz, conv_w[l], A_log[l], dt_bias[l], gdn_norm_w[l]).astype(dt)
        y_b = retention_branch(rq, rk, rv, rg).astype(dt)
        merged = (jax.nn.sigmoid(m_a) * jnp.einsum('bte,ed->btd', y_a, w_up_gdn[l])
                  + jax.nn.sigmoid(m_b) * jnp.einsum('bte,ed->btd', y_b, w_up_ret[l]))
        h = h + jnp.einsum('btd,de->bte', merged, w_out[l])
        h = h + hier_moe(rms_norm(h, norm_ffn_w[l]), w_group[l], b_group[l], w_expert[l], b_expert[l],
                         w_gate[l], w_up[l], w_down[l])
    return rms_norm(h, norm_final_w)
```

```python
import os
from contextlib import ExitStack
import numpy as np
import concourse.bass as bass
import concourse.mybir as mybir
from concourse.bass_utils import run_bass_kernel_spmd

F32 = mybir.dt.float32
BF16 = mybir.dt.bfloat16
AF = mybir.ActivationFunctionType
ALU = mybir.AluOpType
AX = mybir.AxisListType

T = 2048
D = 1024
NT = 16
EPS = 1e-6
ENGINES = ("pe", "act", "dve", "pool", "sp")
N_DMA_SEMS = 12
BIG = 1.0e30


class Sched:
    def __init__(self, nc, stack):
        self.nc = nc
        self.ops = []
        self.last_writer = {}
        self.readers = {}
        self.sem = {e: stack.enter_context(nc.semaphore("s_" + e)) for e in ENGINES}
        self.dma_sems = {}
        for q in ("sp", "act", "pool"):
            self.dma_sems[q] = [stack.enter_context(nc.semaphore(f"d_{q}{i}")) for i in range(N_DMA_SEMS)]
        self.n_dma = {q: 0 for q in self.dma_sems}
        self.cnt = {e: 0 for e in ENGINES}
        self.waited = {e: {} for e in ENGINES}

    PSUM_PREFIXES = ("ptA", "pbG", "pab", "BG", "BH", "BSC", "pbR", "BR", "BM", "PTE", "PLE", "BE")

    def add(self, eng, fn, reads=(), writes=(), dma=False):
        ex = [k for k in reads if k.startswith(self.PSUM_PREFIXES)]
        if ex:
            reads = [k for k in reads if k not in ex]
            writes = list(writes) + ex
        idx = len(self.ops)
        deps = set()
        for k in reads:
            w = self.last_writer.get(k)
            if w is not None:
                deps.add((w, "raw"))
        for k in writes:
            w = self.last_writer.get(k)
            if w is not None:
                deps.add((w, "waw"))
            for r in self.readers.get(k, ()):
                deps.add((r, "war"))
        op = dict(eng=eng, fn=fn, deps=deps, dma=dma, idx=idx, signal=False, ticket=None, dsem=None)
        if dma:
            n = self.n_dma[eng]
            self.n_dma[eng] = n + 1
            op["dsem"] = (eng, n % N_DMA_SEMS, 16 * (n // N_DMA_SEMS + 1))
        self.ops.append(op)
        for k in reads:
            self.readers.setdefault(k, []).append(idx)
        for k in writes:
            self.last_writer[k] = idx
            self.readers[k] = []
        return idx

    def barrier(self):
        keys = set(self.last_writer) | set(self.readers)
        keys.add("__bar__")
        for e in ENGINES:
            self.add(e, None, reads=(), writes=tuple(keys))

    def emit(self):
        ops = self.ops
        for op in ops:
            for (d, kind) in op["deps"]:
                dop = ops[d]
                if dop["dma"]:
                    continue
                if dop["eng"] == op["eng"]:
                    if op["eng"] in ("pe", "sp") or kind == "war":
                        continue
                dop["signal"] = True
        for op in ops:
            if op["dma"]:
                continue
            if op["signal"]:
                self.cnt[op["eng"]] += 1
                op["ticket"] = self.cnt[op["eng"]]
        per_eng = {e: [op for op in ops if op["eng"] == e] for e in ENGINES}
        sem = self.sem
        dma_sems = self.dma_sems

        def run(e, engobj):
            waited = self.waited[e]
            for op in per_eng[e]:
                waits = {}
                for (d, kind) in op["deps"]:
                    dop = ops[d]
                    if dop["dma"]:
                        q, si, val = dop["dsem"]
                        key = ("d", q, si)
                        waits[key] = max(waits.get(key, 0), val)
                        continue
                    if dop["eng"] == e:
                        if e in ("pe", "sp") or kind == "war":
                            continue
                    if dop["ticket"] is None:
                        continue
                    key = ("e", dop["eng"])
                    waits[key] = max(waits.get(key, 0), dop["ticket"])
                if op["dma"]:
                    q, si, val = op["dsem"]
                    if val > 16:
                        key = ("d", q, si)
                        waits[key] = max(waits.get(key, 0), val - 16)
                for key, val in waits.items():
                    if waited.get(key, 0) >= val:
                        continue
                    waited[key] = val
                    s = sem[key[1]] if key[0] == "e" else dma_sems[key[1]][key[2]]
                    engobj.wait_ge(s, val)
                if op["fn"] is None:
                    if op["signal"]:
                        engobj.nop().then_inc(sem[e], 1)
                    continue
                ins = op["fn"](engobj)
                if op["dma"]:
                    q, si, val = op["dsem"]
                    ins.then_inc(dma_sems[q][si], 16)
                elif op["signal"]:
                    ins.then_inc(sem[e], 1)

        with self.nc.Block() as block:
            @block.tensor
            def _(eng):
                run("pe", eng)

            @block.scalar
            def _(eng):
                run("act", eng)

            @block.vector
            def _(eng):
                run("dve", eng)

            @block.gpsimd
            def _(eng):
                run("pool", eng)

            @block.sync
            def _(eng):
                run("sp", eng)
        self.ops = []
        self.last_writer = {}
        self.readers = {}


CM_ID, CM_TL, CM_SU, CM_MLS, CM_MCT, CM_SEL0, CM_SEL1, CM_BO, CM_ONES, CM_UT = range(10)


def make_consts():
    i = np.arange(128)
    same = (i[:, None] // 64) == (i[None, :] // 64)
    cm = np.zeros((10, 128, 128), np.float32)
    cm[CM_ID] = np.eye(128)
    cm[CM_TL] = same & (i[:, None] <= i[None, :])
    cm[CM_SU] = same & (i[:, None] > i[None, :])
    cm[CM_MLS] = same & (i[:, None] > i[None, :])
    cm[CM_MCT] = same & (i[None, :] >= i[:, None])
    cm[CM_SEL0] = (i[:, None] < 64) & np.ones((1, 128), bool)
    cm[CM_SEL1] = (i[:, None] >= 64) & np.ones((1, 128), bool)
    cm[CM_BO] = same
    cm[CM_ONES] = 1.0
    cm[CM_UT] = i[:, None] < i[None, :]
    cm = np.ascontiguousarray(cm.transpose(1, 0, 2))
    h = np.arange(4, dtype=np.float64)
    lg = np.log(1.0 - 2.0 ** (-5.0 - h))
    idx = np.arange(128, dtype=np.float64)
    rel = idx[None, :] - idx[:, None]
    dt = np.where(rel[None] >= 0, np.exp(np.maximum(rel[None], 0) * lg[:, None, None]), 0.0) * (128 ** -0.5)
    ret_dt = np.ascontiguousarray(dt.transpose(1, 0, 2)).astype(np.float32)
    kdec = np.exp(lg[None, :] * (127.0 - idx)[:, None]) * (128 ** -0.5)
    qdec = np.exp(lg[None, :] * (idx + 1.0)[:, None])
    retvec = np.concatenate([kdec, qdec], axis=1).astype(np.float32)
    cdec = [float(np.exp(lg[k] * 128.0)) for k in range(4)]
    inv_freq = 1.0 / (10000.0 ** np.linspace(0.0, 1.0, 64).astype(np.float32).astype(np.float64))
    ang = np.arange(T, dtype=np.float64)[None, :] * np.repeat(inv_freq, 2)[:, None].astype(np.float32).astype(np.float64)
    ang32 = (np.arange(T, dtype=np.float32)[None, :] * np.repeat(inv_freq.astype(np.float32), 2)[:, None]).astype(np.float64)
    cosT = np.cos(ang32).astype(np.float32)
    sgn = np.where(np.arange(128) % 2 == 0, -1.0, 1.0)[:, None]
    sinT = (np.sin(ang32) * sgn).astype(np.float32)
    return cm, ret_dt, retvec, cdec, cosT, sinT


def build(debug=False, stop_after=None):
    nc = bass.Bass("TRN2", target_bir_lowering=False)
    cdec = make_consts()[3]

    def din(name, shape):
        return nc.dram_tensor(name, list(shape), F32, kind="ExternalInput").ap()

    x = din("x", [T, D])
    w_in = din("w_in", [D, 7176])
    w_perm = din("w_perm", [D, 1024])
    cw_d = din("cw", [128, 12, 4])
    alog_d = din("A_log", [4])
    dtb_d = din("dt_bias", [4])
    gnw_d = din("gdn_norm_w", [128])
    wupa_d = din("w_up_gdn", [512, D])
    wupr_d = din("w_up_ret", [D, D])
    wout_d = din("w_out", [D, D])
    nmix_d = din("nmix", [128, 8])
    nffn_d = din("nffn", [128, 8])
    wr_d = din("w_router", [D, 36])
    br_d = din("b_router", [36])
    wg_d = din("w_gate", [32, D, 512])
    wu_d = din("w_up", [32, D, 512])
    wd_d = din("w_down", [32, 512, D])
    nfin_d = din("norm_final_w", [D])
    cm_d = din("cm", [128, 10, 128])
    misc_d = din("misc", [128, 83])
    nffnrow_d = din("nffn_row", [D])
    retdt_d = din("ret_dt", [128, 4, 128])
    retvec_d = din("retvec", [128, 8])
    cos_d = din("cosT", [128, T])
    sin_d = din("sinT", [128, T])
    out = nc.dram_tensor("out", [T, D], F32, kind="ExternalOutput").ap()
    h1s = nc.dram_tensor("dbg_h1" if debug else "h1s", [T, D], F32, kind="ExternalOutput" if debug else "Internal").ap()
    dbg = {}
    if debug:
        dbg["yT"] = nc.dram_tensor("dbg_yT", [12 * 128, T], BF16, kind="ExternalOutput").ap()
        dbg["uT"] = nc.dram_tensor("dbg_uT", [8 * 128, T], BF16, kind="ExternalOutput").ap()

    with ExitStack() as st:
        S = Sched(nc, st)

        def sbuf(stk, name, shape, dt):
            return stk.enter_context(nc.sbuf_tensor("sb_" + name, list(shape), dt))

        def psum(stk, name, shape, dt):
            return stk.enter_context(nc.psum_tensor("ps_" + name, list(shape), dt))

        def dma(out_ap, in_ap, reads=(), writes=(), q="sp", **kw):
            S.add(q, lambda e: e.dma_start(out=out_ap, in_=in_ap, **kw), reads, writes, dma=True)

        def mm(o, lhsT, rhs, start, stop, reads, writes):
            S.add("pe", lambda e: e.matmul(o, lhsT=lhsT, rhs=rhs, start=start, stop=stop), reads, writes)

        def tr(o, in_, ident, reads, writes):
            S.add("pe", lambda e: e.transpose(o, in_, ident), reads, writes)

        def act(o, in_, func, reads, writes, **kw):
            S.add("act", lambda e: e.activation(out=o, in_=in_, func=func, **kw), reads, writes)

        def tt(o, a, b, op, reads, writes, eng="dve"):
            S.add(eng, lambda e: e.tensor_tensor(o, a, b, op), reads, writes)

        def ts(o, a, s1, s2, op0, op1, reads, writes, eng="dve"):
            if op1 is None:
                S.add(eng, lambda e: e.tensor_scalar(o, a, s1, None, op0), reads, writes)
            else:
                S.add(eng, lambda e: e.tensor_scalar(o, a, s1, s2, op0, op1), reads, writes)

        def stt(o, a, s, b, op0, op1, reads, writes, eng="dve"):
            S.add(eng, lambda e: e.scalar_tensor_tensor(o, a, s, b, op0, op1), reads, writes)

        def cp(o, a, reads, writes, eng="dve"):
            S.add(eng, lambda e: e.tensor_copy(o, a), reads, writes)

        def red(o, a, op, reads, writes, eng="dve"):
            S.add(eng, lambda e: e.tensor_reduce(o, a, AX.X, op), reads, writes)

        def memset(o, v, writes, eng="dve"):
            S.add(eng, lambda e: e.memset(o, v), (), writes)

        def recip(o, a, reads, writes):
            S.add("dve", lambda e: e.reciprocal(o, a), reads, writes)

        def rsqrt_small(o, a, scale, reads, writes):
            act(o, a, AF.Ln, list(reads) + ["epsb"], writes, scale=scale, bias=epsb[:])
            act(o, o, AF.Exp, writes, writes, scale=-0.5)

        cm = sbuf(st, "cm", [128, 10, 128], F32)
        identb = sbuf(st, "identb", [128, 128], BF16)
        onesb = sbuf(st, "onesb", [128, 128], BF16)
        epsb = sbuf(st, "epsb", [128, 1], F32)
        nmix = sbuf(st, "nmix", [128, 8], F32)
        nffn = sbuf(st, "nffn", [128, 8], F32)
        wstage = {}
        wcnt = [0]

        def alloc_wstage(stk):
            wstage["st"] = [sbuf(stk, f"wst{i}_{wcnt[0]}", [128, 8, 256], F32) for i in range(2)]
            wstage["bf"] = [sbuf(stk, f"wbf{i}_{wcnt[0]}", [128, 8, 256], BF16) for i in range(2)]

        dma(cm[:], cm_d, writes=["cm"])
        dma(nmix[:], nmix_d, writes=["nmix"])
        dma(nffn[:], nffn_d, writes=["nffn"])
        cp(identb[:], cm[:, CM_ID, :], ["cm"], ["identb"])
        cp(onesb[:], cm[:, CM_ONES, :], ["cm"], ["onesb"])
        memset(epsb[:], EPS, ["epsb"])
        ident = cm[:, CM_ID, :]

        def load_w(src_ap, ncols):
            wst, wbf = wstage["st"], wstage["bf"]
            i = wcnt[0] % 2
            wcnt[0] += 1
            dma(wst[i][:, :, 0:ncols], src_ap.rearrange("(c p) n -> p c n", p=128), writes=[f"wst{i}"])
            cp(wbf[i][:, :, 0:ncols], wst[i][:, :, 0:ncols], [f"wst{i}"], [f"wbf{i}"], eng="pool")
            return wbf[i], f"wbf{i}"

        with ExitStack() as mix:
            uT = sbuf(mix, "uT", [128, 8, T], BF16)
            yT = sbuf(mix, "yT", [128, 12, T], BF16)

            with ExitStack() as ph:
                xt = [sbuf(ph, f"xt{i}", [128, D], F32) for i in range(2)]
                sq = sbuf(ph, "sq", [128, D], F32)
                xs = [sbuf(ph, f"xs{i}", [128, D], BF16) for i in range(2)]
                ss = sbuf(ph, "ssA", [128, NT], F32)
                rstd = sbuf(ph, "rstdA", [128, NT], F32)
                pt = [psum(ph, f"ptA{i}", [128, 8, 128], BF16) for i in range(2)]
                memset(ss[:], 0.0, ["ssA"])
                for t in range(NT):
                    i = t % 2
                    dma(xt[i][:], x[t * 128:(t + 1) * 128, :], writes=[f"xt{i}"])
                    act(sq[:], xt[i][:], AF.Square, [f"xt{i}", "ssA"], ["sq", "ssA"], accum_out=ss[:, t:t + 1])
                    rsqrt_small(rstd[:, t:t + 1], ss[:, t:t + 1], 1.0 / D, ["ssA"], [f"rstdA{t}"])
                    ts(xs[i][:], xt[i][:], rstd[:, t:t + 1], None, ALU.mult, None, [f"xt{i}", f"rstdA{t}"], [f"xs{i}"])
                    for c in range(8):
                        tr(pt[i][:, c, :], xs[i][:, c * 128:(c + 1) * 128], identb[:], [f"xs{i}", "identb"], [f"ptA{i}"])
                    tt(uT[:, :, t * 128:(t + 1) * 128], pt[i][:], nmix[:, :].unsqueeze(2).to_broadcast([128, 8, 128]),
                       ALU.mult, [f"ptA{i}", "nmix"], [f"uT{t}"])
                S.barrier()
                S.emit()
                if stop_after == "A":
                    return nc
            uT_keys = [f"uT{t}" for t in range(NT)]

            def proj_fm(bank, bkey, wt, wkey, col, blk):
                for c in range(8):
                    mm(bank, wt[:, c, col:col + 128], uT[:, c, blk * 512:(blk + 1) * 512], c == 0, c == 7,
                       [wkey], [bkey])

            def proj_tm(bank_ap, bkey, wt_ap_fn, wkey, t):
                for c in range(8):
                    mm(bank_ap, uT[:, c, t * 128:(t + 1) * 128], wt_ap_fn(c), c == 0, c == 7, [wkey], [bkey])

            with ExitStack() as ph:
                qkvT = sbuf(ph, "qkvT", [128, 12, T], BF16)
                cwt = sbuf(ph, "cwt", [128, 12, 4], F32)
                dma(cwt[:], cw_d, writes=["cwt"])
                with ExitStack() as g1:
                    alloc_wstage(g1)
                    xc = [sbuf(g1, f"xc{i}", [128, 3 + T], BF16) for i in range(2)]
                    diag = [sbuf(g1, f"diag{i}", [128, 4, 128], BF16) for i in range(2)]
                    s16 = [sbuf(g1, f"s16_{i}", [128, 512], BF16) for i in range(2)]
                    sq16 = [sbuf(g1, f"sq16_{i}", [128, 512], BF16) for i in range(2)]
                    rn = [sbuf(g1, f"rn{i}", [128, 512], F32) for i in range(2)]
                    pb = [psum(g1, f"pbG{i}", [128, 512], F32) for i in range(8)]
                    for i in range(2):
                        memset(xc[i][:, 0:3], 0.0, [f"xc{i}"])
                    for ch in range(12):
                        i = ch % 2
                        if ch % 2 == 0:
                            wt, wkey = load_w(w_in[:, ch * 128:(ch + 2) * 128], 256)
                        col = (ch % 2) * 128
                        for blk in range(4):
                            proj_fm(pb[blk][:], f"pbG{blk}", wt, wkey, col, blk)
                            act(xc[i][:, 3 + blk * 512:3 + (blk + 1) * 512], pb[blk][:], AF.Copy, [f"pbG{blk}"], [f"xc{i}"])
                        for k in range(4):
                            ts(diag[i][:, k, :], identb[:], cwt[:, ch, k:k + 1], None, ALU.mult, None,
                               ["identb", "cwt"], [f"diag{i}"])
                        for blk in range(4):
                            b2 = 4 + (blk % 2)
                            j = blk % 2
                            for k in range(4):
                                mm(pb[b2][:], diag[i][:, k, :], xc[i][:, blk * 512 + k:blk * 512 + k + 512], k == 0, k == 3,
                                   [f"diag{i}", f"xc{i}"], [f"pbG{b2}"])
                            dst = qkvT[:, ch, blk * 512:(blk + 1) * 512]
                            act(dst, pb[b2][:], AF.Silu, [f"pbG{b2}"], [f"qkvT{ch}_{blk}"])
                    for ch in range(8):
                        for blk in range(4):
                            j = blk % 2
                            b3 = 6 + j
                            dst = qkvT[:, ch, blk * 512:(blk + 1) * 512]
                            tt(sq16[j][:], dst, dst, ALU.mult, [f"qkvT{ch}_{blk}"], [f"sq16_{j}"])
                            mm(pb[b3][:], onesb[:], sq16[j][:], True, True, ["onesb", f"sq16_{j}"], [f"pbG{b3}"])
                            act(rn[j][:], pb[b3][:], AF.Ln, [f"pbG{b3}", "epsb"], [f"rn{j}"], bias=epsb[:])
                            act(rn[j][:], rn[j][:], AF.Exp, [f"rn{j}"], [f"rn{j}"], scale=-0.5)
                            tt(dst, dst, rn[j][:], ALU.mult, [f"qkvT{ch}_{blk}", f"rn{j}"], [f"qkvT{ch}_{blk}"])
                    S.barrier()
                    S.emit()
                    if stop_after == "G1":
                        return nc
                gg = sbuf(ph, "gg", [128, NT, 4], F32)
                beta = sbuf(ph, "beta", [128, NT, 4], F32)
                wz = sbuf(ph, "wz", [128, 8, 512], BF16)
                gnw = sbuf(ph, "gnw", [128, 128], F32)
                with ExitStack() as g2:
                    alloc_wstage(g2)
                    ab = sbuf(g2, "ab", [128, NT, 8], F32)
                    tmp4 = sbuf(g2, "tmp4", [128, NT, 4], F32)
                    alog = sbuf(g2, "alog", [128, 4], F32)
                    dtb = sbuf(g2, "dtb", [128, 4], F32)
                    pab = psum(g2, "pab", [128, NT, 8], F32)
                    dma(alog[:], alog_d.partition_broadcast(128), writes=["alog"])
                    dma(dtb[:], dtb_d.partition_broadcast(128), writes=["dtb"])
                    dma(gnw[:], gnw_d.partition_broadcast(128), writes=["gnw"])
                    wt, wkey = load_w(w_in[:, 1536:1544], 8)
                    for t in range(NT):
                        proj_tm(pab[:, t, :], "pab", lambda c: wt[:, c, 0:8], wkey, t)
                    cp(ab[:], pab[:], ["pab"], ["ab"])
                    for j in range(2):
                        wt2, wkey2 = load_w(w_in[:, 1544 + j * 256:1544 + (j + 1) * 256], 256)
                        cp(wz[:, :, j * 256:(j + 1) * 256], wt2[:, :, 0:256], [wkey2], ["wz"])
                    tt(tmp4[:], ab[:, :, 0:4], dtb[:, :].unsqueeze(1).to_broadcast([128, NT, 4]), ALU.add, ["ab", "dtb"], ["tmp4"])
                    act(tmp4[:], tmp4[:], AF.Exp, ["tmp4"], ["tmp4"])
                    act(tmp4[:], tmp4[:], AF.Ln, ["tmp4"], ["tmp4"], bias=1.0)
                    act(alog[:], alog[:], AF.Exp, ["alog"], ["alog"])
                    stt(gg[:], tmp4[:], -1.0, alog[:, :].unsqueeze(1).to_broadcast([128, NT, 4]), ALU.mult, ALU.mult,
                        ["tmp4", "alog"], ["gg"])
                    act(beta[:], ab[:, :, 4:8], AF.Exp, ["ab"], ["beta"], scale=-1.0)
                    ts(beta[:], beta[:], 1.0, None, ALU.add, None, ["beta"], ["beta"])
                    recip(beta[:], beta[:], ["beta"], ["beta"])
                    S.barrier()
                    S.emit()
                    if stop_after == "G2":
                        return nc
                with ExitStack() as g3:
                    B0 = psum(g3, "BG0", [128, 512], F32)
                    BT = psum(g3, "BGT", [128, 8, 128], BF16)
                    H = [psum(g3, f"BH{h}", [128, 512], F32) for h in range(4)]
                    SC = [psum(g3, f"BSC{i}", [128, 512], F32) for i in range(2)]
                    gs = sbuf(g3, "gs", [128, 16], F32)
                    es = sbuf(g3, "es", [128, 16], F32)
                    bg = sbuf(g3, "bg", [128, 4], F32)
                    kbg = sbuf(g3, "kbg", [128, 4, 128], BF16)
                    kd = sbuf(g3, "kd", [128, 4, 128], BF16)
                    vb = sbuf(g3, "vb", [128, 4, 128], BF16)
                    Gg = [sbuf(g3, f"Gg{h}", [128, 128], F32) for h in range(4)]
                    Ed = [sbuf(g3, f"Ed{h}", [128, 2, 128], F32) for h in range(4)]
                    Dm = [sbuf(g3, f"Dm{h}", [128, 2, 128], F32) for h in range(4)]
                    LN = [[sbuf(g3, f"LN{h}_{i}", [128, 2, 128], F32) for i in range(2)] for h in range(4)]
                    Pm = [[sbuf(g3, f"Pm{h}_{i}", [128, 128], F32) for i in range(2)] for h in range(4)]
                    TTb = [sbuf(g3, f"TTb{h}", [128, 128], BF16) for h in range(4)]
                    dg = [sbuf(g3, f"dg{h}", [128, 128], BF16) for h in range(4)]
                    attnT = sbuf(g3, "attnT", [128, 4, 128], BF16)
                    qgT = sbuf(g3, "qgT", [128, 4, 128], BF16)
                    wT = sbuf(g3, "wT", [128, 4, 128], BF16)
                    uu = sbuf(g3, "uu", [128, 4, 128], F32)
                    vnew = sbuf(g3, "vnew", [128, 4, 128], BF16)
                    S32 = sbuf(g3, "S32", [128, 4, 128], F32)
                    S16 = sbuf(g3, "S16", [128, 4, 128], BF16)
                    oo = sbuf(g3, "oo", [128, 4, 128], F32)
                    osq = sbuf(g3, "osq", [128, 4, 128], F32)
                    oss = sbuf(g3, "oss", [128, 4], F32)
                    orst = sbuf(g3, "orst", [128, 4], F32)
                    sz = sbuf(g3, "sz", [128, 512], F32)
                    ya = sbuf(g3, "ya", [128, 512], BF16)
                    memset(S32[:], 0.0, [f"S32_{h}" for h in range(4)])
                    memset(S16[:], 0.0, [f"S16_{h}" for h in range(4)])
                    TL = cm[:, CM_TL, :]
                    SCL = float(128 ** -0.5)

                    def head_prep(n, h):
                        tsl = slice(n * 128, (n + 1) * 128)
                        Hh, hk = H[h], f"BH{h}"
                        kTh = qkvT[:, 4 + h, tsl]
                        qTh = qkvT[:, h, tsl]
                        ts(Gg[h][:], cm[:, CM_SU, :], gg[:, n, h:h + 1], None, ALU.mult, None, ["cm", "gg"], [f"Gg{h}"])
                        ts(dg[h][:], identb[:], es[:, h:h + 1], None, ALU.mult, None, ["identb", "es"], [f"dg{h}"])
                        yield
                        mm(Hh[:, 0:128], Gg[h][:], TL, True, True, [f"Gg{h}", "cm"], [hk])
                        mm(Hh[:, 128:256], TL, Gg[h][:], True, True, [f"Gg{h}", "cm"], [hk])
                        mm(Hh[:, 256:384], kTh, kTh, True, True, [], [hk])
                        mm(Hh[:, 384:512], kTh, qTh, True, True, [], [hk])
                        yield
                        act(Ed[h][:], Hh[:, 0:256].rearrange("p (a b) -> p a b", a=2), AF.Exp, [hk], [f"Ed{h}"])
                        yield
                        tt(Dm[h][:, 0, :], Ed[h][:, 0, :], cm[:, CM_MCT, :], ALU.mult, [f"Ed{h}", "cm"], [f"DmA{h}"], eng="pool")
                        tt(Dm[h][:, 1, :], Ed[h][:, 1, :], cm[:, CM_MLS, :], ALU.mult, [f"Ed{h}", "cm"], [f"DmB{h}"])
                        yield
                        stt(LN[h][0][:, 0, :], Hh[:, 256:384], beta[:, n, h:h + 1], Dm[h][:, 1, :], ALU.mult, ALU.mult,
                            [hk, "beta", f"DmB{h}"], [f"LN{h}_0"])
                        stt(attnT[:, h, :], Hh[:, 384:512], SCL, Dm[h][:, 0, :], ALU.mult, ALU.mult,
                            [hk, f"DmA{h}"], [f"attnT{h}"])
                        yield
                        mm(Hh[:, 0:128], LN[h][0][:, 0, :], ident, True, True, [f"LN{h}_0", "cm"], [hk])
                        yield
                        act(LN[h][0][:, 1, :], Hh[:, 0:128], AF.Copy, [hk], [f"LN{h}_0"])
                        yield
                        tt(Pm[h][0][:], ident, LN[h][0][:, 1, :], ALU.subtract, ["cm", f"LN{h}_0"], [f"Pm{h}_0"])
                        a, p = 0, 0
                        for lvl in range(1, 6):
                            na = 1 - a
                            Lc, Nc = LN[h][a][:, 0, :], LN[h][a][:, 1, :]
                            mm(Hh[:, 0:128], Nc, Lc, True, True, [f"LN{h}_{a}"], [hk])
                            if lvl < 5:
                                mm(Hh[:, 128:256], Lc, Nc, True, True, [f"LN{h}_{a}"], [hk])
                            yield
                            if lvl < 5:
                                act(LN[h][na][:], Hh[:, 0:256].rearrange("p (a b) -> p a b", a=2), AF.Copy, [hk], [f"LN{h}_{na}"])
                            else:
                                act(LN[h][na][:, 0, :], Hh[:, 0:128], AF.Copy, [hk], [f"LN{h}_{na}"])
                            yield
                            mm(Hh[:, 256:384], LN[h][na][:, 0, :], Pm[h][p][:], True, True, [f"LN{h}_{na}", f"Pm{h}_{p}"], [hk])
                            yield
                            if lvl < 5:
                                tt(Pm[h][1 - p][:], Hh[:, 256:384], Pm[h][p][:], ALU.add, [hk, f"Pm{h}_{p}"], [f"Pm{h}_{1 - p}"])
                            else:
                                tt(TTb[h][:], Hh[:, 256:384], Pm[h][p][:], ALU.add, [hk, f"Pm{h}_{p}"], [f"TTb{h}"])
                            yield
                            a, p = na, 1 - p
                        mm(Hh[:, 0:128], TTb[h][:], vb[:, h, :], True, True, [f"TTb{h}", "vb"], [hk])
                        mm(Hh[:, 128:256], kbg[:, h, :], TTb[h][:], True, True, [f"TTb{h}", "kbg"], [hk])
                        mm(Hh[:, 256:384], onesb[:], dg[h][:], True, True, ["onesb", f"dg{h}"], [hk])
                        yield
                        cp(uu[:, h, :], Hh[:, 0:128], [hk], [f"uu{h}"])
                        stt(qgT[:, h, :], Hh[:, 256:384], SCL, qTh, ALU.mult, ALU.mult, [hk], [f"qgT{h}"])
                        act(wT[:, h, :], Hh[:, 128:256], AF.Copy, [hk], [f"wT{h}"])
                        yield

                    def head_scan(n, h):
                        Hh, hk = H[h], f"BH{h}"
                        for half in range(2):
                            hs = slice(half * 64, (half + 1) * 64)
                            mm(Hh[:, 0:128], wT[:, h, :], S16[:, h, :], True, True, [f"wT{h}", f"S16_{h}"], [hk])
                            yield
                            tt(vnew[hs, h, :], uu[hs, h, :], Hh[hs, 0:128], ALU.subtract, [f"uu{h}", hk], [f"vnew{h}"])
                            yield
                            mm(Hh[:, 128:256], qgT[:, h, :], S16[:, h, :], True, False, [f"qgT{h}", f"S16_{h}"], [hk])
                            mm(Hh[:, 128:256], attnT[hs, h, :], vnew[hs, h, :], False, True, [f"attnT{h}", f"vnew{h}"], [hk])
                            mm(Hh[:, 256:384], kd[hs, h, :], vnew[hs, h, :], True, True, ["kd", f"vnew{h}"], [hk])
                            yield
                            stt(S32[:, h, :], S32[:, h, :], es[:, 8 + 4 * half + h:9 + 4 * half + h], Hh[:, 256:384],
                                ALU.mult, ALU.add, [hk, "es", f"S32_{h}"], [f"S32_{h}"])
                            act(oo[hs, h, :], Hh[hs, 128:256], AF.Copy, [hk], [f"oo{h}"])
                            yield
                            act(S16[:, h, :], S32[:, h, :], AF.Copy, [f"S32_{h}"], [f"S16_{h}"])
                            yield

                    def rr(gens):
                        gens = list(gens)
                        while gens:
                            for g in list(gens):
                                try:
                                    next(g)
                                except StopIteration:
                                    gens.remove(g)

                    G3N = int(os.environ.get("G3N", str(NT)))
                    for n in range(G3N):
                        tsl = slice(n * 128, (n + 1) * 128)
                        mm(B0[:, 0:4], TL, gg[:, n, :], True, True, ["cm", "gg"], ["BG0"])
                        mm(B0[:, 4:8], cm[:, CM_BO, :], gg[:, n, :], True, True, ["cm", "gg"], ["BG0"])
                        mm(B0[:, 8:12], cm[:, CM_SEL0, :], gg[:, n, :], True, True, ["cm", "gg"], ["BG0"])
                        mm(B0[:, 12:16], cm[:, CM_SEL1, :], gg[:, n, :], True, True, ["cm", "gg"], ["BG0"])
                        cp(gs[:], B0[:, 0:16], ["BG0"], ["gs"])
                        tt(gs[:, 4:8], gs[:, 4:8], gs[:, 0:4], ALU.subtract, ["gs"], ["gs"])
                        act(es[:], gs[:], AF.Exp, ["gs"], ["es"])
                        tt(bg[:], es[:, 0:4], beta[:, n, :], ALU.mult, ["es", "beta"], ["bg"])
                        for h in range(4):
                            tr(BT[:, h, :], qkvT[:, 4 + h, tsl], identb[:], ["identb"], ["BGT"])
                            tr(BT[:, 4 + h, :], qkvT[:, 8 + h, tsl], identb[:], ["identb"], ["BGT"])
                        tt(kbg[:], BT[:, 0:4, :], bg[:, :].unsqueeze(2).to_broadcast([128, 4, 128]), ALU.mult, ["BGT", "bg"], ["kbg"])
                        tt(kd[:], BT[:, 0:4, :], es[:, 4:8].unsqueeze(2).to_broadcast([128, 4, 128]), ALU.mult, ["BGT", "es"], ["kd"])
                        tt(vb[:], BT[:, 4:8, :], beta[:, n, :].unsqueeze(2).to_broadcast([128, 4, 128]), ALU.mult, ["BGT", "beta"], ["vb"])
                        rr(head_prep(n, h) for h in range(4))
                        rr(head_scan(n, h) for h in range(4))
                        ook = [f"oo{h}" for h in range(4)]
                        tt(osq[:], oo[:], oo[:], ALU.mult, ook, ["osq"])
                        red(oss[:], osq[:], ALU.add, ["osq"], ["oss"])
                        rsqrt_small(orst[:], oss[:], 1.0 / 128, ["oss"], ["orst"])
                        proj_tm(B0[:], "BG0", lambda c: wz[:, c, :], "wz", n)
                        act(sz[:], B0[:], AF.Silu, ["BG0"], ["sz"])
                        tt(osq[:], oo[:], orst[:, :].unsqueeze(2).to_broadcast([128, 4, 128]), ALU.mult, ook + ["orst"], ["osq"])
                        tt(osq[:], osq[:], gnw[:, :].unsqueeze(1).to_broadcast([128, 4, 128]), ALU.mult, ["osq", "gnw"], ["osq"])
                        tt(ya[:], osq[:].rearrange("p a b -> p (a b)"), sz[:], ALU.mult, ["osq", "sz"], ["ya"])
                        for h in range(4):
                            tr(BT[:, h, :], ya[:, h * 128:(h + 1) * 128], identb[:], ["ya", "identb"], ["BGT"])
                        cp(yT[:, 0:4, tsl], BT[:, 0:4, :], ["BGT"], [f"yT{n}"])
                    S.barrier()
                    S.emit()
                    if stop_after == "G3":
                        return nc

            with ExitStack() as ph:
                rqkT = sbuf(ph, "rqkT", [128, 8, T], BF16)
                retdt = sbuf(ph, "retdt", [128, 4, 128], F32)
                retvec = sbuf(ph, "retvec", [128, 8], F32)
                dma(retdt[:], retdt_d, writes=["retdt"])
                dma(retvec[:], retvec_d, writes=["retvec"])
                with ExitStack() as r1:
                    alloc_wstage(r1)
                    cosT = sbuf(r1, "cosT", [128, T], F32)
                    sinT = sbuf(r1, "sinT", [128, T], F32)
                    t1 = [sbuf(r1, f"t1_{i}", [128, 512], F32) for i in range(2)]
                    t2 = [sbuf(r1, f"t2_{i}", [128, 512], F32) for i in range(2)]
                    pb = [psum(r1, f"pbR{i}", [128, 512], F32) for i in range(8)]
                    dma(cosT[:], cos_d, writes=["cosT"])
                    dma(sinT[:], sin_d, writes=["sinT"])
                    for ch in range(8):
                        if ch % 2 == 0:
                            wt, wkey = load_w(w_in[:, 2056 + ch * 128:2056 + (ch + 2) * 128], 256)
                            wtp, wkeyp = load_w(w_perm[:, ch * 128:(ch + 2) * 128], 256)
                        col = (ch % 2) * 128
                        for blk in range(4):
                            j = blk % 2
                            b1, b2 = 2 * (blk % 4), 2 * (blk % 4) + 1
                            proj_fm(pb[b1][:], f"pbR{b1}", wt, wkey, col, blk)
                            proj_fm(pb[b2][:], f"pbR{b2}", wtp, wkeyp, col, blk)
                            bs = slice(blk * 512, (blk + 1) * 512)
                            tt(t1[j][:], pb[b1][:], cosT[:, bs], ALU.mult, [f"pbR{b1}", "cosT"], [f"t1_{j}"])
                            tt(t2[j][:], pb[b2][:], sinT[:, bs], ALU.mult, [f"pbR{b2}", "sinT"], [f"t2_{j}"])
                            tt(rqkT[:, ch, bs], t1[j][:], t2[j][:], ALU.add, [f"t1_{j}", f"t2_{j}"], [f"rqkT{ch}"], eng="pool")
                    S.barrier()
                    S.emit()
                    if stop_after == "R1":
                        return nc
                with ExitStack() as r2:
                    alloc_wstage(r2)
                    wv = sbuf(r2, "wv", [128, 8, 1024], BF16)
                    wgt = sbuf(r2, "wgt", [128, 8, 1024], BF16)
                    vtok = sbuf(r2, "vtok", [128, 1024], BF16)
                    szr = sbuf(r2, "szr", [128, 1024], F32)
                    kdk = sbuf(r2, "kdk", [128, 4, 128], BF16)
                    sc16 = [sbuf(r2, f"sc16_{h}", [128, 128], BF16) for h in range(4)]
                    R32 = sbuf(r2, "R32", [128, 4, 256], F32)
                    R16 = sbuf(r2, "R16", [128, 4, 256], BF16)
                    otmp = [sbuf(r2, f"otmp{h}", [128, 256], F32) for h in range(4)]
                    ro = sbuf(r2, "ro", [128, 4, 256], F32)
                    rsq = sbuf(r2, "rsq", [128, 4, 256], F32)
                    rss = sbuf(r2, "rss", [128, 4], F32)
                    rrst = sbuf(r2, "rrst", [128, 4], F32)
                    yb = sbuf(r2, "yb", [128, 1024], BF16)
                    Bv = [psum(r2, f"BRv{i}", [128, 512], F32) for i in range(2)]
                    H = [psum(r2, f"BRH{h}", [128, 512], F32) for h in range(4)]
                    BT = psum(r2, "BRT", [128, 8, 128], BF16)
                    for j in range(4):
                        wt2, wkey2 = load_w(w_in[:, 3080 + j * 256:3080 + (j + 1) * 256], 256)
                        cp(wv[:, :, j * 256:(j + 1) * 256], wt2[:, :, 0:256], [wkey2], ["wv"])
                    for j in range(4):
                        wt2, wkey2 = load_w(w_in[:, 4104 + j * 256:4104 + (j + 1) * 256], 256)
                        cp(wgt[:, :, j * 256:(j + 1) * 256], wt2[:, :, 0:256], [wkey2], ["wgt"])
                    memset(R32[:], 0.0, [f"R32_{h}" for h in range(4)])
                    memset(R16[:], 0.0, [f"R16_{h}" for h in range(4)])

                    def ret_head(n, h):
                        tsl = slice(n * 128, (n + 1) * 128)
                        Hh, hk = H[h], f"BRH{h}"
                        qTh = rqkT[:, h, tsl]
                        kTh = rqkT[:, 4 + h, tsl]
                        vh = vtok[:, h * 256:(h + 1) * 256]
                        mm(Hh[:, 0:128], kTh, qTh, True, True, [], [hk])
                        mm(Hh[:, 256:512], kdk[:, h, :], vh, True, True, ["kdk", "vtok"], [hk])
                        yield
                        tt(sc16[h][:], Hh[:, 0:128], retdt[:, h, :], ALU.mult, [hk, "retdt"], [f"sc16_{h}"])
                        stt(R32[:, h, :], R32[:, h, :], float(cdec[h]), Hh[:, 256:512], ALU.mult, ALU.add,
                            [hk, f"R32_{h}"], [f"R32_{h}"])
                        yield
                        mm(Hh[:, 0:256], sc16[h][:], vh, True, True, [f"sc16_{h}", "vtok"], [hk])
                        mm(Hh[:, 256:512], qTh, R16[:, h, :], True, True, [f"R16_{h}"], [hk])
                        yield
                        act(otmp[h][:], Hh[:, 0:256], AF.Copy, [hk], [f"otmp{h}"])
                        yield
                        stt(ro[:, h, :], Hh[:, 256:512], retvec[:, 4 + h:5 + h], otmp[h][:], ALU.mult, ALU.add,
                            [hk, f"otmp{h}", "retvec"], [f"ro{h}"])
                        act(R16[:, h, :], R32[:, h, :], AF.Copy, [f"R32_{h}"], [f"R16_{h}"])
                        yield

                    def rr2(gens):
                        gens = list(gens)
                        while gens:
                            for g in list(gens):
                                try:
                                    next(g)
                                except StopIteration:
                                    gens.remove(g)

                    for n in range(NT):
                        tsl = slice(n * 128, (n + 1) * 128)
                        for j in range(2):
                            proj_tm(Bv[j][:], f"BRv{j}", lambda c, j=j: wv[:, c, j * 512:(j + 1) * 512], "wv", n)
                            act(vtok[:, j * 512:(j + 1) * 512], Bv[j][:], AF.Copy, [f"BRv{j}"], ["vtok"])
                        for h in range(4):
                            tr(BT[:, h, :], rqkT[:, 4 + h, tsl], identb[:], ["identb"], ["BRT"])
                        tt(kdk[:], BT[:, 0:4, :], retvec[:, 0:4].unsqueeze(2).to_broadcast([128, 4, 128]), ALU.mult,
                           ["BRT", "retvec"], ["kdk"])
                        rr2(ret_head(n, h) for h in range(4))
                        for j in range(2):
                            proj_tm(Bv[j][:], f"BRv{j}", lambda c, j=j: wgt[:, c, j * 512:(j + 1) * 512], "wgt", n)
                            act(szr[:, j * 512:(j + 1) * 512], Bv[j][:], AF.Silu, [f"BRv{j}"], ["szr"])
                        rok = [f"ro{h}" for h in range(4)]
                        tt(rsq[:], ro[:], ro[:], ALU.mult, rok, ["rsq"])
                        red(rss[:], rsq[:], ALU.add, ["rsq"], ["rss"])
                        rsqrt_small(rrst[:], rss[:], 1.0 / 256, ["rss"], ["rrst"])
                        tt(rsq[:], ro[:], rrst[:, :].unsqueeze(2).to_broadcast([128, 4, 256]), ALU.mult, rok + ["rrst"], ["rsq"])
                        tt(yb[:], rsq[:].rearrange("p a b -> p (a b)"), szr[:], ALU.mult, ["rsq", "szr"], ["yb"])
                        for c in range(8):
                            tr(BT[:, c, :], yb[:, c * 128:(c + 1) * 128], identb[:], ["yb", "identb"], ["BRT"])
                        cp(yT[:, 4:12, tsl], BT[:], ["BRT"], [f"yT{n}"])
                    S.barrier()
                    S.emit()
                    if stop_after == "R2":
                        return nc
            if debug:
                for c in range(12):
                    dma(dbg["yT"][c * 128:(c + 1) * 128, :], yT[:, c, :], writes=["dbgyT"])
                for c in range(8):
                    dma(dbg["uT"][c * 128:(c + 1) * 128, :], uT[:, c, :], writes=["dbguT"])

            with ExitStack() as ph:
                mT = sbuf(ph, "mT", [128, 8, T], BF16)
                B = [psum(ph, f"BM{i}", [128, 512], F32) for i in range(8)]
                with ExitStack() as ms1:
                    wsm = [sbuf(ms1, f"wsm{i}", [128, 8, 128], F32) for i in range(4)]
                    wsb = [[sbuf(ms1, f"wsb{s_}_{i}", [128, 8, 128], BF16) for i in range(4)] for s_ in range(2)]
                    tA = [sbuf(ms1, f"tA{i}", [128, 512], F32) for i in range(2)]
                    m1 = [sbuf(ms1, f"m1_{i}", [128, 512], F32) for i in range(2)]
                    for ec in range(8):
                        es_ = slice(ec * 128, (ec + 1) * 128)
                        s_ = ec % 2
                        dma(wsm[0][:, 0:4, :], wupa_d[:, es_].rearrange("(c p) n -> p c n", p=128), writes=["wsm0"])
                        dma(wsm[1][:], wupr_d[:, es_].rearrange("(c p) n -> p c n", p=128), writes=["wsm1"])
                        dma(wsm[2][:], w_in[:, 5128 + ec * 128:5128 + (ec + 1) * 128].rearrange("(c p) n -> p c n", p=128), writes=["wsm2"])
                        dma(wsm[3][:], w_in[:, 6152 + ec * 128:6152 + (ec + 1) * 128].rearrange("(c p) n -> p c n", p=128), writes=["wsm3"])
                        cp(wsb[s_][0][:, 0:4, :], wsm[0][:, 0:4, :], ["wsm0"], [f"wsb{s_}_0"], eng="pool")
                        for i in range(1, 4):
                            cp(wsb[s_][i][:], wsm[i][:], [f"wsm{i}"], [f"wsb{s_}_{i}"], eng="pool")
                        for blk in range(4):
                            bs = slice(blk * 512, (blk + 1) * 512)
                            j = blk % 2
                            bA, bB, bMA, bMB = 4 * j, 4 * j + 1, 4 * j + 2, 4 * j + 3
                            for c in range(4):
                                mm(B[bA][:], wsb[s_][0][:, c, :], yT[:, c, bs], c == 0, c == 3, [f"wsb{s_}_0"], [f"BM{bA}"])
                            for c in range(8):
                                mm(B[bB][:], wsb[s_][1][:, c, :], yT[:, 4 + c, bs], c == 0, c == 7, [f"wsb{s_}_1"], [f"BM{bB}"])
                            for c in range(8):
                                mm(B[bMA][:], wsb[s_][2][:, c, :], uT[:, c, bs], c == 0, c == 7, [f"wsb{s_}_2"], [f"BM{bMA}"])
                            for c in range(8):
                                mm(B[bMB][:], wsb[s_][3][:, c, :], uT[:, c, bs], c == 0, c == 7, [f"wsb{s_}_3"], [f"BM{bMB}"])
                            act(tA[j][:], B[bMA][:], AF.Tanh, [f"BM{bMA}"], [f"tA{j}"], scale=0.5)
                            stt(m1[j][:], tA[j][:], 1.0, B[bA][:], ALU.add, ALU.mult, [f"tA{j}", f"BM{bA}"], [f"m1_{j}"])
                            act(tA[j][:], B[bMB][:], AF.Tanh, [f"BM{bMB}"], [f"tA{j}"], scale=0.5)
                            stt(tA[j][:], tA[j][:], 1.0, B[bB][:], ALU.add, ALU.mult, [f"tA{j}", f"BM{bB}"], [f"tA{j}"])
                            tt(mT[:, ec, bs], m1[j][:], tA[j][:], ALU.add, [f"m1_{j}", f"tA{j}"], [f"mT{blk}"], eng="pool")
                    S.barrier()
                    S.emit()
                with ExitStack() as ms2:
                    alloc_wstage(ms2)
                    wo = sbuf(ms2, "wo", [128, 8, 1024], BF16)
                    xr = [sbuf(ms2, f"xr{i}", [128, D], F32) for i in range(2)]
                    ho = [sbuf(ms2, f"ho{i}", [128, D], F32) for i in range(2)]
                    for j in range(4):
                        wt2, wkey2 = load_w(wout_d[:, j * 256:(j + 1) * 256], 256)
                        cp(wo[:, :, j * 256:(j + 1) * 256], wt2[:, :, 0:256], [wkey2], ["wo"])
                    for t in range(NT):
                        i = t % 2
                        dma(xr[i][:], x[t * 128:(t + 1) * 128, :], writes=[f"xr{i}"])
                        for hf in range(2):
                            bk = (2 * t + hf) % 8
                            for c in range(8):
                                mm(B[bk][:], mT[:, c, t * 128:(t + 1) * 128], wo[:, c, hf * 512:(hf + 1) * 512], c == 0, c == 7,
                                   ["wo"], [f"BM{bk}"])
                            stt(ho[i][:, hf * 512:(hf + 1) * 512], B[bk][:], 0.5, xr[i][:, hf * 512:(hf + 1) * 512], ALU.mult, ALU.add,
                                [f"BM{bk}", f"xr{i}"], [f"ho{i}"])
                        dma(h1s[t * 128:(t + 1) * 128, :], ho[i][:], reads=[f"ho{i}"], writes=["h1s"])
                    S.barrier()
                    S.emit()
                    if stop_after == "M":
                        return nc

        TS = 512
        NOVER = 8
        NTILE = 32 + NOVER
        NSLOT = NTILE * TS
        I32 = mybir.dt.int32
        XS = nc.dram_tensor("xs_scr", [NSLOT, D], BF16).ap()
        WS = nc.dram_tensor("ws_scr", [NSLOT, 1], F32).ap()
        YS = nc.dram_tensor("ys_scr", [NSLOT, D], F32).ap()
        TE = nc.dram_tensor("dbg_te" if debug else "te_scr", [128, NOVER], I32, kind="ExternalOutput" if debug else "Internal").ap()
        with ExitStack() as ph:
            hacc = sbuf(ph, "hacc", [128, NT, D], F32)
            s1i = sbuf(ph, "s1i", [128, NT], I32)
            s2i = sbuf(ph, "s2i", [128, NT], I32)
            w1v = sbuf(ph, "w1v", [128, NT], F32)
            w2v = sbuf(ph, "w2v", [128, NT], F32)
            gidx = sbuf(ph, "gidx", [128, NOVER, 8], I32)
            didx = sbuf(ph, "didx", [128, NOVER, 4], I32)
            for t in range(NT):
                dma(hacc[:, t, :], h1s[t * 128:(t + 1) * 128, :], reads=["h1s"], writes=[f"hacc{t}"])
            with ExitStack() as e1:
                xntok = sbuf(e1, "xntok", [128, NT, D], BF16)
                nfrow = sbuf(e1, "nfrow", [128, D], F32)
                misc = sbuf(e1, "misc", [128, 83], F32)
                hs_ = [sbuf(e1, f"hs{i}", [128, D], F32) for i in range(2)]
                sq = sbuf(e1, "sqE", [128, D], F32)
                ssE = sbuf(e1, "ssE", [128, NT], F32)
                rstE = sbuf(e1, "rstE", [128, NT], F32)
                xn32 = [sbuf(e1, f"xn32_{i}", [128, 8, 128], F32) for i in range(2)]
                wr = sbuf(e1, "wr", [128, 8, 36], F32)
                brt = sbuf(e1, "brt", [128, 36], F32)
                lg = sbuf(e1, "lg", [128, NT, 36], F32)
                PT = [psum(e1, f"PTE{i}", [128, 8, 128], F32) for i in range(2)]
                PL = psum(e1, "PLE", [128, 512], F32)
                PS1 = psum(e1, "PTE_rank", [128, 512], F32)
                PS2 = psum(e1, "PTE_cnt", [128, 512], F32)
                dma(wr[:], wr_d.rearrange("(c p) n -> p c n", p=128), writes=["wr"])
                dma(brt[:], br_d.partition_broadcast(128), writes=["brt"])
                dma(nfrow[:], nffnrow_d.partition_broadcast(128), writes=["nfrow"])
                dma(misc[:], misc_d, writes=["misc"])
                memset(ssE[:], 0.0, ["ssE"])
                for t in range(NT):
                    i = t % 2
                    act(sq[:], hacc[:, t, :], AF.Square, ["ssE", f"hacc{t}"], ["sqE", "ssE"], accum_out=ssE[:, t:t + 1])
                    rsqrt_small(rstE[:, t:t + 1], ssE[:, t:t + 1], 1.0 / D, ["ssE"], [f"rstE{t}"])
                    ts(hs_[i][:], hacc[:, t, :], rstE[:, t:t + 1], None, ALU.mult, None, [f"rstE{t}", f"hacc{t}"], [f"hs{i}"])
                    tt(xntok[:, t, :], hs_[i][:], nfrow[:], ALU.mult, [f"hs{i}", "nfrow"], [f"xntok{t}"], eng="pool")
                    for c in range(8):
                        mm(PT[i][:, c, :], hs_[i][:, c * 128:(c + 1) * 128], ident, True, True, [f"hs{i}", "cm"], [f"PTE{i}"])
                    tt(xn32[i][:], PT[i][:], nffn[:, :].unsqueeze(2).to_broadcast([128, 8, 128]), ALU.mult,
                       [f"PTE{i}", "nffn"], [f"xn32_{i}"])
                    for c in range(8):
                        mm(PL[:, 0:36], xn32[i][:, c, :], wr[:, c, :], c == 0, c == 7, [f"xn32_{i}", "wr"], ["PLE"])
                    tt(lg[:, t, :], PL[:, 0:36], brt[:], ALU.add, ["PLE", "brt"], ["lg"])
                gmax = sbuf(e1, "gmax", [128, NT], F32)
                ohg = sbuf(e1, "ohg", [128, NT, 4], F32)
                sh4 = sbuf(e1, "sh4", [128, NT, 4], F32)
                gw = sbuf(e1, "gw", [128, NT], F32)
                M32 = sbuf(e1, "M32", [128, NT, 32], F32)
                oh1 = sbuf(e1, "oh1", [128, NT, 32], F32)
                oh2 = sbuf(e1, "oh2", [128, NT, 32], F32)
                m1v = sbuf(e1, "m1v", [128, NT], F32)
                m2v = sbuf(e1, "m2v", [128, NT], F32)
                L4 = lg[:, :, 0:4]
                L32 = lg[:, :, 4:36]
                bc4 = lambda a: a.unsqueeze(2).to_broadcast([128, NT, 4])
                bc32 = lambda a: a.unsqueeze(2).to_broadcast([128, NT, 32])
                red(gmax[:], L4, ALU.max, ["lg"], ["gmax"])
                tt(ohg[:], L4, bc4(gmax[:, :]), ALU.is_equal, ["lg", "gmax"], ["ohg"])
                tt(sh4[:], L4, bc4(gmax[:, :]), ALU.subtract, ["lg", "gmax"], ["sh4"])
                act(sh4[:], sh4[:], AF.Exp, ["sh4"], ["sh4"])
                red(gw[:], sh4[:], ALU.add, ["sh4"], ["gw"])
                recip(gw[:], gw[:], ["gw"], ["gw"])
                ts(ohg[:], ohg[:], BIG, -BIG, ALU.mult, ALU.add, ["ohg"], ["ohg"])
                tt(M32[:].rearrange("p t (g e) -> p t g e", g=4), L32.rearrange("p t (g e) -> p t g e", g=4),
                   ohg[:, :, :].unsqueeze(3).to_broadcast([128, NT, 4, 8]), ALU.add, ["lg", "ohg"], ["M32"])
                red(m1v[:], M32[:], ALU.max, ["M32"], ["m1v"])
                tt(oh1[:], M32[:], bc32(m1v[:, :]), ALU.is_equal, ["M32", "m1v"], ["oh1"])
                stt(M32[:], oh1[:], -BIG, M32[:], ALU.mult, ALU.add, ["oh1", "M32"], ["M32"])
                red(m2v[:], M32[:], ALU.max, ["M32"], ["m2v"])
                tt(oh2[:], M32[:], bc32(m2v[:, :]), ALU.is_equal, ["M32", "m2v"], ["oh2"])
                tt(w2v[:], m2v[:], m1v[:], ALU.subtract, ["m1v", "m2v"], ["w2v"])
                act(w2v[:], w2v[:], AF.Exp, ["w2v"], ["w2v"])
                ts(w1v[:], w2v[:], 1.0, None, ALU.add, None, ["w2v"], ["w1v"])
                recip(w1v[:], w1v[:], ["w1v"], ["w1v"])
                tt(w2v[:], w2v[:], w1v[:], ALU.mult, ["w2v", "w1v"], ["w2v"])
                tt(w1v[:], w1v[:], gw[:], ALU.mult, ["w1v", "gw"], ["w1v"])
                tt(w2v[:], w2v[:], gw[:], ALU.mult, ["w2v", "gw"], ["w2v"])
                sel = sbuf(e1, "sel", [128, NT, 32], F32)
                tcs = sbuf(e1, "tcs", [128, NT, 32], F32)
                off = sbuf(e1, "off", [128, NT, 32], F32)
                slot = sbuf(e1, "slot", [128, NT, 32], F32)
                cnt = sbuf(e1, "cnt", [128, 32], F32)
                cmp3 = sbuf(e1, "cmp3", [128, 32, 3], F32)
                pfa = sbuf(e1, "pfa", [128, 32], F32)
                pfb = sbuf(e1, "pfb", [128, 32], F32)
                nov = sbuf(e1, "nov", [128, 32], F32)
                ost = sbuf(e1, "ost", [128, 32], F32)
                oen = sbuf(e1, "oen", [128, 32], F32)
                dlt = sbuf(e1, "dlt", [128, 32], F32)
                isov = sbuf(e1, "isov", [128, NT, 32], F32)
                s1f = sbuf(e1, "s1f", [128, NT], F32)
                s2f = sbuf(e1, "s2f", [128, NT], F32)
                A1 = sbuf(e1, "A1", [128, NOVER, 32], F32)
                A2 = sbuf(e1, "A2", [128, NOVER, 32], F32)
                tef = sbuf(e1, "tef", [128, NOVER], F32)
                tei = sbuf(e1, "tei", [128, NOVER], I32)
                bgf = sbuf(e1, "bgf", [128, NOVER], F32)
                gidxf = sbuf(e1, "gidxf", [128, NOVER, 8], F32)
                didxf = sbuf(e1, "didxf", [128, NOVER, 4], F32)
                thr3 = misc[:, 0:3]
                kk8 = misc[:, 3:11]
                eio = misc[:, 11:43]
                pc8 = misc[:, 43:51]
                e512 = misc[:, 51:83]
                flat = lambda a: a.rearrange("p t e -> p (t e)")
                tt(sel[:], oh1[:], oh2[:], ALU.add, ["oh1", "oh2"], ["sel"])
                mm(PS1[:], cm[:, CM_UT, :], flat(sel[:]), True, True, ["cm", "sel"], ["PTE_rank"])
                mm(PS2[:], cm[:, CM_ONES, :], flat(sel[:]), True, True, ["cm", "sel"], ["PTE_cnt"])
                cp(flat(tcs[:]), PS2[:], ["PTE_cnt"], ["tcs"])
                memset(off[:, 0, :], 0.0, ["off"])
                for t in range(1, NT):
                    tt(off[:, t, :], off[:, t - 1, :], tcs[:, t - 1, :], ALU.add, ["off", "tcs"], ["off"])
                tt(cnt[:], off[:, NT - 1, :], tcs[:, NT - 1, :], ALU.add, ["off", "tcs"], ["cnt"])
                tt(cmp3[:], cnt[:, :].unsqueeze(2).to_broadcast([128, 32, 3]), thr3.unsqueeze(1).to_broadcast([128, 32, 3]),
                   ALU.is_gt, ["cnt", "misc"], ["cmp3"])
                red(nov[:], cmp3[:], ALU.add, ["cmp3"], ["nov"])
                cp(pfa[:], nov[:], ["nov"], ["pfa"])
                cur, nxt, ck, nk = pfa, pfb, "pfa", "pfb"
                for dd in (1, 2, 4, 8, 16):
                    cp(nxt[:], cur[:], [ck], [nk])
                    tt(nxt[:, dd:32], cur[:, dd:32], cur[:, 0:32 - dd], ALU.add, [ck, nk], [nk])
                    cur, nxt, ck, nk = nxt, cur, nk, ck
                incl, ik = cur, ck
                ts(oen[:], incl[:], 32.0, None, ALU.add, None, [ik], ["oen"])
                tt(ost[:], oen[:], nov[:], ALU.subtract, ["oen", "nov"], ["ost"])
                ts(dlt[:], ost[:], float(TS), -float(TS), ALU.mult, ALU.add, ["ost"], ["dlt"])
                tt(dlt[:], dlt[:], e512, ALU.subtract, ["dlt", "misc"], ["dlt"])
                tt(flat(slot[:]), PS1[:], flat(off[:]), ALU.add, ["PTE_rank", "off"], ["slot"])
                ts(isov[:], slot[:], float(TS), None, ALU.is_ge, None, ["slot"], ["isov"])
                tt(isov[:], isov[:], dlt[:, :].unsqueeze(1).to_broadcast([128, NT, 32]), ALU.mult, ["isov", "dlt"], ["isov"])
                tt(slot[:], slot[:], e512.unsqueeze(1).to_broadcast([128, NT, 32]), ALU.add, ["slot", "misc"], ["slot"])
                tt(slot[:], slot[:], isov[:], ALU.add, ["slot", "isov"], ["slot"])
                tt(sel[:], oh1[:], slot[:], ALU.mult, ["oh1", "slot"], ["sel"])
                red(s1f[:], sel[:], ALU.add, ["sel"], ["s1f"])
                tt(sel[:], oh2[:], slot[:], ALU.mult, ["oh2", "slot"], ["sel"])
                red(s2f[:], sel[:], ALU.add, ["sel"], ["s2f"])
                cp(s1i[:], s1f[:], ["s1f"], ["s1i"])
                cp(s2i[:], s2f[:], ["s2f"], ["s2i"])
                kkb = kk8.unsqueeze(2).to_broadcast([128, NOVER, 32])
                tt(A1[:], kkb, ost[:, :].unsqueeze(1).to_broadcast([128, NOVER, 32]), ALU.is_ge, ["misc", "ost"], ["A1"])
                tt(A2[:], kkb, oen[:, :].unsqueeze(1).to_broadcast([128, NOVER, 32]), ALU.is_lt, ["misc", "oen"], ["A2"])
                tt(A1[:], A1[:], A2[:], ALU.mult, ["A1", "A2"], ["A1"])
                tt(A1[:], A1[:], eio.unsqueeze(1).to_broadcast([128, NOVER, 32]), ALU.mult, ["A1", "misc"], ["A1"])
                red(tef[:], A1[:], ALU.add, ["A1"], ["tef"])
                cp(tei[:], tef[:], ["tef"], ["tei"])
                dma(TE, tei[:], reads=["tei"], writes=["TE"])
                ts(bgf[:], tef[:], 1024.0, None, ALU.mult, None, ["tef"], ["bgf"])
                tt(gidxf[:], bgf[:, :].unsqueeze(2).to_broadcast([128, NOVER, 8]), pc8.unsqueeze(1).to_broadcast([128, NOVER, 8]),
                   ALU.add, ["bgf", "misc"], ["gidxf"])
                ts(bgf[:], tef[:], 512.0, None, ALU.mult, None, ["tef"], ["bgf"])
                tt(didxf[:], bgf[:, :].unsqueeze(2).to_broadcast([128, NOVER, 4]), pc8[:, 0:4].unsqueeze(1).to_broadcast([128, NOVER, 4]),
                   ALU.add, ["bgf", "misc"], ["didxf"])
                cp(gidx[:], gidxf[:], ["gidxf"], ["gidx"])
                cp(didx[:], didxf[:], ["didxf"], ["didx"])
                for t in range(NT):
                    for (si, wv_, nm) in ((s1i, w1v, "a"), (s2i, w2v, "b")):
                        S.add("pool", lambda e, t=t, si=si: e.indirect_dma_start(
                            out=XS[:, :], out_offset=bass.IndirectOffsetOnAxis(ap=si[:, t:t + 1], axis=0),
                            in_=xntok[:, t, :], in_offset=None),
                            reads=[f"xntok{t}", "s1i", "s2i"], writes=["XS"], dma=True)
                        S.add("pool", lambda e, t=t, si=si, wv_=wv_: e.indirect_dma_start(
                            out=WS[:, :], out_offset=bass.IndirectOffsetOnAxis(ap=si[:, t:t + 1], axis=0),
                            in_=wv_[:, t:t + 1], in_offset=None),
                            reads=["w1v", "w2v", "s1i", "s2i"], writes=["WS"], dma=True)
                if debug:
                    dbg["s12"] = nc.dram_tensor("dbg_s12", [128, 2 * NT], I32, kind="ExternalOutput").ap()
                    dma(dbg["s12"][:, 0:NT], s1i[:], reads=["s1i"], writes=["dbgs12"])
                    dma(dbg["s12"][:, NT:2 * NT], s2i[:], reads=["s2i"], writes=["dbgs12"])
                S.barrier()
                S.emit()
                if stop_after == "router":
                    return nc
            with ExitStack() as e2:
                NB = 2
                NH = TS // 128
                ewg = [sbuf(e2, f"ewg{i}", [128, 8, 512], BF16) for i in range(NB)]
                ewu = [sbuf(e2, f"ewu{i}", [128, 8, 512], BF16) for i in range(NB)]
                ewd = [sbuf(e2, f"ewd{i}", [128, 4, 1024], BF16) for i in range(NB)]
                xst = [sbuf(e2, f"xst{i}", [128, NH, D], BF16) for i in range(2)]
                wst_ = [sbuf(e2, f"wsl{i}", [128, NH], F32) for i in range(2)]
                xT = [sbuf(e2, f"xT{i}", [128, 8, TS], BF16) for i in range(2)]
                hidT = [sbuf(e2, f"hidT{i}", [128, 4, TS], BF16) for i in range(2)]
                sg = [sbuf(e2, f"sg{i}", [128, TS], BF16) for i in range(2)]
                yt = [sbuf(e2, f"yt{i}", [128, NH, D], F32) for i in range(2)]
                PTk = [psum(e2, f"BEt{i}", [128, 8, 128], BF16) for i in range(2)]
                Bgu = [psum(e2, f"BEg{i}", [128, 512], F32) for i in range(4)]
                Bd = [psum(e2, f"BEd{i}", [128, 512], F32) for i in range(2)]
                wg_flat = wg_d.rearrange("e d n -> (e d) n")
                wu_flat = wu_d.rearrange("e d n -> (e d) n")
                wd_flat = wd_d.rearrange("e f n -> (e f) n")

                def gather_w(dst, src_flat, idx_ap, key):
                    S.add("pool", lambda e: e.indirect_dma_start(
                        out=dst, out_offset=None, in_=src_flat[:, :],
                        in_offset=bass.IndirectOffsetOnAxis(ap=idx_ap, axis=0)),
                        reads=["gidx", "didx"], writes=[key], dma=True)

                dcount = [0]
                for k in range(NTILE):
                    b3 = k % NB
                    b2 = k % 2
                    rs = slice(k * TS, (k + 1) * TS)
                    if k < 32:
                        dma(ewg[b3][:], wg_d[k].rearrange("(c p) n -> p c n", p=128), writes=[f"ewg{b3}_{c}" for c in range(8)], q="pool")
                        dma(ewu[b3][:], wu_d[k].rearrange("(c p) n -> p c n", p=128), writes=[f"ewu{b3}_{c}" for c in range(8)], q="pool")
                        dma(ewd[b3][:], wd_d[k].rearrange("(c p) n -> p c n", p=128), writes=[f"ewd{b3}_{c}" for c in range(4)], q="pool")
                    else:
                        ko = k - 32
                        for c in range(8):
                            gather_w(ewg[b3][:, c, :], wg_flat, gidx[:, ko, c:c + 1], f"ewg{b3}_{c}")
                        for c in range(8):
                            gather_w(ewu[b3][:, c, :], wu_flat, gidx[:, ko, c:c + 1], f"ewu{b3}_{c}")
                        for c in range(4):
                            gather_w(ewd[b3][:, c, :], wd_flat, didx[:, ko, c:c + 1], f"ewd{b3}_{c}")
                    dma(xst[b2][:], XS[rs, :].rearrange("(h p) d -> p h d", p=128), reads=["XS"], writes=[f"xst{b2}"])
                    dma(wst_[b2][:], WS[rs, :].rearrange("(h p) o -> p (h o)", p=128), reads=["WS"], writes=[f"wsl{b2}"],
                        allow_slow_non_contiguous=True)
                    for hh in range(NH):
                        pj = hh % 2
                        for c in range(8):
                            tr(PTk[pj][:, c, :], xst[b2][:, hh, c * 128:(c + 1) * 128], identb[:], [f"xst{b2}", "identb"], [f"BEt{pj}"])
                        if pj == 0:
                            cp(xT[b2][:, :, hh * 128:(hh + 1) * 128], PTk[pj][:], [f"BEt{pj}"], [f"xT{b2}"])
                        else:
                            act(xT[b2][:, :, hh * 128:(hh + 1) * 128], PTk[pj][:], AF.Copy, [f"BEt{pj}"], [f"xT{b2}"])
                    for f in range(4):
                        j = f % 2
                        bg_, bu_ = Bgu[2 * j], Bgu[2 * j + 1]
                        for c in range(8):
                            mm(bg_[:, 0:TS], ewg[b3][:, c, f * 128:(f + 1) * 128], xT[b2][:, c, :], c == 0, c == 7,
                               [f"ewg{b3}_{c}", f"xT{b2}"], [f"BEg{2 * j}"])
                        for c in range(8):
                            mm(bu_[:, 0:TS], ewu[b3][:, c, f * 128:(f + 1) * 128], xT[b2][:, c, :], c == 0, c == 7,
                               [f"ewu{b3}_{c}", f"xT{b2}"], [f"BEg{2 * j + 1}"])
                        act(sg[j][:], bg_[:, 0:TS], AF.Silu, [f"BEg{2 * j}"], [f"sg{j}"])
                        tt(hidT[b2][:, f, :], bu_[:, 0:TS], sg[j][:], ALU.mult, [f"BEg{2 * j + 1}", f"sg{j}"], [f"hidT{b2}"])
                    for hh in range(NH):
                        for cc in range(2):
                            bk = dcount[0] % 2
                            dcount[0] += 1
                            for f in range(4):
                                mm(Bd[bk][:], hidT[b2][:, f, hh * 128:(hh + 1) * 128], ewd[b3][:, f, cc * 512:(cc + 1) * 512],
                                   f == 0, f == 3, [f"ewd{b3}_{f}", f"hidT{b2}"], [f"BEd{bk}"])
                            if bk == 0:
                                ts(yt[b2][:, hh, cc * 512:(cc + 1) * 512], Bd[bk][:], wst_[b2][:, hh:hh + 1], None, ALU.mult, None,
                                   [f"BEd{bk}", f"wsl{b2}"], [f"yt{b2}"])
                            else:
                                act(yt[b2][:, hh, cc * 512:(cc + 1) * 512], Bd[bk][:], AF.Copy, [f"BEd{bk}", f"wsl{b2}"], [f"yt{b2}"],
                                    scale=wst_[b2][:, hh:hh + 1])
                    dma(YS[rs, :].rearrange("(h p) d -> p h d", p=128), yt[b2][:], reads=[f"yt{b2}"], writes=["YS"])
                S.barrier()
                S.emit()
                if stop_after == "experts":
                    return nc
            with ExitStack() as e3:
                nfw = sbuf(e3, "nfw", [128, D], F32)
                sq = sbuf(e3, "sqF", [128, D], F32)
                ssF = sbuf(e3, "ssF", [128, NT], F32)
                rstF = sbuf(e3, "rstF", [128, NT], F32)
                y1 = [sbuf(e3, f"y1_{i}", [128, D], F32) for i in range(2)]
                y2 = [sbuf(e3, f"y2_{i}", [128, D], F32) for i in range(2)]
                ob = [sbuf(e3, f"ob{i}", [128, D], F32) for i in range(2)]
                dma(nfw[:], nfin_d.partition_broadcast(128), writes=["nfw"])
                memset(ssF[:], 0.0, ["ssF"])
                for t in range(NT):
                    i = t % 2
                    S.add("pool", lambda e, t=t, i=i: e.indirect_dma_start(
                        out=y1[i][:, :], out_offset=None, in_=YS[:, :],
                        in_offset=bass.IndirectOffsetOnAxis(ap=s1i[:, t:t + 1], axis=0)),
                        reads=["YS", "s1i"], writes=[f"y1_{i}"], dma=True)
                    S.add("pool", lambda e, t=t, i=i: e.indirect_dma_start(
                        out=y2[i][:, :], out_offset=None, in_=YS[:, :],
                        in_offset=bass.IndirectOffsetOnAxis(ap=s2i[:, t:t + 1], axis=0)),
                        reads=["YS", "s2i"], writes=[f"y2_{i}"], dma=True)
                    tt(y1[i][:], y1[i][:], y2[i][:], ALU.add, [f"y1_{i}", f"y2_{i}"], [f"y1_{i}"], eng="pool")
                    tt(hacc[:, t, :], hacc[:, t, :], y1[i][:], ALU.add, [f"y1_{i}", f"hacc{t}"], [f"hacc{t}"])
                    act(sq[:], hacc[:, t, :], AF.Square, ["ssF", f"hacc{t}"], ["sqF", "ssF"], accum_out=ssF[:, t:t + 1])
                    rsqrt_small(rstF[:, t:t + 1], ssF[:, t:t + 1], 1.0 / D, ["ssF"], [f"rstF{t}"])
                    stt(ob[i][:], hacc[:, t, :], rstF[:, t:t + 1], nfw[:], ALU.mult, ALU.mult, [f"rstF{t}", "nfw", f"hacc{t}"], [f"ob{i}"])
                    dma(out[t * 128:(t + 1) * 128, :], ob[i][:], reads=[f"ob{i}"], writes=["out"])
                S.barrier()
                S.emit()
    return nc


_CACHE = {}


def _host_inputs(inputs):
    f = lambda a: np.ascontiguousarray(np.asarray(a, dtype=np.float32))
    w_in = f(inputs["w_in"][0])
    perm = np.arange(1024) ^ 1
    w_perm = np.ascontiguousarray(w_in[:, 2056:3080][:, perm])
    cw = np.ascontiguousarray(f(inputs["conv_w"][0]).T.reshape(12, 128, 4).transpose(1, 0, 2))
    cm, ret_dt, retvec, cdec, cosT, sinT = make_consts()
    shared = {
        "w_in": w_in, "w_perm": w_perm, "cw": cw,
        "A_log": f(inputs["A_log"][0]), "dt_bias": f(inputs["dt_bias"][0]),
        "gdn_norm_w": f(inputs["gdn_norm_w"][0]),
        "w_up_gdn": f(inputs["w_up_gdn"][0]), "w_up_ret": f(inputs["w_up_ret"][0]), "w_out": f(inputs["w_out"][0]),
        "nmix": np.ascontiguousarray(f(inputs["norm_mix_w"][0]).reshape(8, 128).T),
        "nffn": np.ascontiguousarray(f(inputs["norm_ffn_w"][0]).reshape(8, 128).T),
        "w_router": np.ascontiguousarray(np.concatenate([f(inputs["w_group"][0]), f(inputs["w_expert"][0])], axis=1)),
        "b_router": np.ascontiguousarray(np.concatenate([f(inputs["b_group"][0]), f(inputs["b_expert"][0])], axis=0)),
        "w_gate": f(inputs["w_gate"][0]), "w_up": f(inputs["w_up"][0]), "w_down": f(inputs["w_down"][0]),
        "norm_final_w": f(inputs["norm_final_w"]),
        "misc": np.ascontiguousarray(np.concatenate([
            np.broadcast_to(np.concatenate([512.0 * np.arange(1, 4), 32.0 + np.arange(8), np.arange(32)]).astype(np.float32)[None, :], (128, 43)),
            (np.arange(8)[None, :] * 128 + np.arange(128)[:, None]).astype(np.float32),
            np.broadcast_to((512.0 * np.arange(32)).astype(np.float32)[None, :], (128, 32))], axis=1)),
        "nffn_row": f(inputs["norm_ffn_w"][0]),
        "cm": cm, "ret_dt": ret_dt, "retvec": retvec, "cosT": cosT, "sinT": sinT,
    }
    return shared


def kernel(**inputs):
    x = np.asarray(inputs["x"], dtype=np.float32)
    nb = x.shape[0]
    shared = _host_inputs(inputs)
    nc = build()
    in_maps = []
    for b in range(nb):
        m = dict(shared)
        m["x"] = np.ascontiguousarray(x[b])
        in_maps.append(m)
    res = run_bass_kernel_spmd(nc, in_maps, core_ids=list(range(nb)))
    return np.stack([np.asarray(r["out"], dtype=np.float32) for r in res.results], axis=0)
```

```python
import os
from contextlib import ExitStack
import numpy as np
import concourse.bass as bass
import concourse.mybir as mybir
from concourse.bass_utils import run_bass_kernel_spmd

F32 = mybir.dt.float32
BF16 = mybir.dt.bfloat16
AF = mybir.ActivationFunctionType
ALU = mybir.AluOpType
AX = mybir.AxisListType

T = 2048
D = 1024
NT = 16
EPS = 1e-6
ENGINES = ("pe", "act", "dve", "pool", "sp")
N_DMA_SEMS = 12
BIG = 1.0e30


class Sched:
    def __init__(self, nc, stack):
        self.nc = nc
        self.ops = []
        self.last_writer = {}
        self.readers = {}
        self.sem = {e: stack.enter_context(nc.semaphore("s_" + e)) for e in ENGINES}
        self.dma_sems = {}
        for q in ("sp", "act", "pool"):
            self.dma_sems[q] = [stack.enter_context(nc.semaphore(f"d_{q}{i}")) for i in range(N_DMA_SEMS)]
        self.n_dma = {q: 0 for q in self.dma_sems}
        self.cnt = {e: 0 for e in ENGINES}
        self.waited = {e: {} for e in ENGINES}

    PSUM_PREFIXES = ("ptA", "pbG", "pab", "BG", "BH", "BSC", "pbR", "BR", "BM", "PTE", "PLE", "BE")

    def add(self, eng, fn, reads=(), writes=(), dma=False):
        ex = [k for k in reads if k.startswith(self.PSUM_PREFIXES)]
        if ex:
            reads = [k for k in reads if k not in ex]
            writes = list(writes) + ex
        idx = len(self.ops)
        deps = set()
        for k in reads:
            w = self.last_writer.get(k)
            if w is not None:
                deps.add((w, "raw"))
        for k in writes:
            w = self.last_writer.get(k)
            if w is not None:
                deps.add((w, "waw"))
            for r in self.readers.get(k, ()):
                deps.add((r, "war"))
        op = dict(eng=eng, fn=fn, deps=deps, dma=dma, idx=idx, signal=False, ticket=None, dsem=None)
        if dma:
            n = self.n_dma[eng]
            self.n_dma[eng] = n + 1
            op["dsem"] = (eng, n % N_DMA_SEMS, 16 * (n // N_DMA_SEMS + 1))
        self.ops.append(op)
        for k in reads:
            self.readers.setdefault(k, []).append(idx)
        for k in writes:
            self.last_writer[k] = idx
            self.readers[k] = []
        return idx

    def barrier(self):
        keys = set(self.last_writer) | set(self.readers)
        keys.add("__bar__")
        for e in ENGINES:
            self.add(e, None, reads=(), writes=tuple(keys))

    def emit(self):
        ops = self.ops
        for op in ops:
            for (d, kind) in op["deps"]:
                dop = ops[d]
                if dop["dma"]:
                    continue
                if dop["eng"] == op["eng"]:
                    if op["eng"] in ("pe", "sp") or kind == "war":
                        continue
                dop["signal"] = True
        for op in ops:
            if op["dma"]:
                continue
            if op["signal"]:
                self.cnt[op["eng"]] += 1
                op["ticket"] = self.cnt[op["eng"]]
        per_eng = {e: [op for op in ops if op["eng"] == e] for e in ENGINES}
        sem = self.sem
        dma_sems = self.dma_sems

        def run(e, engobj):
            waited = self.waited[e]
            for op in per_eng[e]:
                waits = {}
                for (d, kind) in op["deps"]:
                    dop = ops[d]
                    if dop["dma"]:
                        q, si, val = dop["dsem"]
                        key = ("d", q, si)
                        waits[key] = max(waits.get(key, 0), val)
                        continue
                    if dop["eng"] == e:
                        if e in ("pe", "sp") or kind == "war":
                            continue
                    if dop["ticket"] is None:
                        continue
                    key = ("e", dop["eng"])
                    waits[key] = max(waits.get(key, 0), dop["ticket"])
                if op["dma"]:
                    q, si, val = op["dsem"]
                    if val > 16:
                        key = ("d", q, si)
                        waits[key] = max(waits.get(key, 0), val - 16)
                for key, val in waits.items():
                    if waited.get(key, 0) >= val:
                        continue
                    waited[key] = val
                    s = sem[key[1]] if key[0] == "e" else dma_sems[key[1]][key[2]]
                    engobj.wait_ge(s, val)
                if op["fn"] is None:
                    if op["signal"]:
                        engobj.nop().then_inc(sem[e], 1)
                    continue
                ins = op["fn"](engobj)
                if op["dma"]:
                    q, si, val = op["dsem"]
                    ins.then_inc(dma_sems[q][si], 16)
                elif op["signal"]:
                    ins.then_inc(sem[e], 1)

        with self.nc.Block() as block:
            @block.tensor
            def _(eng):
                run("pe", eng)

            @block.scalar
            def _(eng):
                run("act", eng)

            @block.vector
            def _(eng):
                run("dve", eng)

            @block.gpsimd
            def _(eng):
                run("pool", eng)

            @block.sync
            def _(eng):
                run("sp", eng)
        self.ops = []
        self.last_writer = {}
        self.readers = {}


CM_ID, CM_TL, CM_SU, CM_MLS, CM_MCT, CM_SEL0, CM_SEL1, CM_BO, CM_ONES, CM_UT = range(10)


def make_consts():
    i = np.arange(128)
    same = (i[:, None] // 64) == (i[None, :] // 64)
    cm = np.zeros((10, 128, 128), np.float32)
    cm[CM_ID] = np.eye(128)
    cm[CM_TL] = same & (i[:, None] <= i[None, :])
    cm[CM_SU] = same & (i[:, None] > i[None, :])
    cm[CM_MLS] = same & (i[:, None] > i[None, :])
    cm[CM_MCT] = same & (i[None, :] >= i[:, None])
    cm[CM_SEL0] = (i[:, None] < 64) & np.ones((1, 128), bool)
    cm[CM_SEL1] = (i[:, None] >= 64) & np.ones((1, 128), bool)
    cm[CM_BO] = same
    cm[CM_ONES] = 1.0
    cm[CM_UT] = i[:, None] < i[None, :]
    cm = np.ascontiguousarray(cm.transpose(1, 0, 2))
    h = np.arange(4, dtype=np.float64)
    lg = np.log(1.0 - 2.0 ** (-5.0 - h))
    idx = np.arange(128, dtype=np.float64)
    rel = idx[None, :] - idx[:, None]
    dt = np.where(rel[None] >= 0, np.exp(np.maximum(rel[None], 0) * lg[:, None, None]), 0.0) * (128 ** -0.5)
    ret_dt = np.ascontiguousarray(dt.transpose(1, 0, 2)).astype(np.float32)
    kdec = np.exp(lg[None, :] * (127.0 - idx)[:, None]) * (128 ** -0.5)
    qdec = np.exp(lg[None, :] * (idx + 1.0)[:, None])
    retvec = np.concatenate([kdec, qdec], axis=1).astype(np.float32)
    cdec = [float(np.exp(lg[k] * 128.0)) for k in range(4)]
    inv_freq = 1.0 / (10000.0 ** np.linspace(0.0, 1.0, 64).astype(np.float32).astype(np.float64))
    ang = np.arange(T, dtype=np.float64)[None, :] * np.repeat(inv_freq, 2)[:, None].astype(np.float32).astype(np.float64)
    ang32 = (np.arange(T, dtype=np.float32)[None, :] * np.repeat(inv_freq.astype(np.float32), 2)[:, None]).astype(np.float64)
    cosT = np.cos(ang32).astype(np.float32)
    sgn = np.where(np.arange(128) % 2 == 0, -1.0, 1.0)[:, None]
    sinT = (np.sin(ang32) * sgn).astype(np.float32)
    return cm, ret_dt, retvec, cdec, cosT, sinT


def build(debug=False, stop_after=None):
    nc = bass.Bass("TRN2", target_bir_lowering=False)
    cdec = make_consts()[3]

    def din(name, shape):
        return nc.dram_tensor(name, list(shape), F32, kind="ExternalInput").ap()

    x = din("x", [T, D])
    w_in = din("w_in", [D, 7176])
    w_perm = din("w_perm", [D, 1024])
    cw_d = din("cw", [128, 12, 4])
    alog_d = din("A_log", [4])
    dtb_d = din("dt_bias", [4])
    gnw_d = din("gdn_norm_w", [128])
    wupa_d = din("w_up_gdn", [512, D])
    wupr_d = din("w_up_ret", [D, D])
    wout_d = din("w_out", [D, D])
    nmix_d = din("nmix", [128, 8])
    nffn_d = din("nffn", [128, 8])
    wr_d = din("w_router", [D, 36])
    br_d = din("b_router", [36])
    wg_d = din("w_gate", [32, D, 512])
    wu_d = din("w_up", [32, D, 512])
    wd_d = din("w_down", [32, 512, D])
    nfin_d = din("norm_final_w", [D])
    cm_d = din("cm", [128, 10, 128])
    misc_d = din("misc", [128, 95])
    nffnrow_d = din("nffn_row", [D])
    retdt_d = din("ret_dt", [128, 4, 128])
    retvec_d = din("retvec", [128, 8])
    cos_d = din("cosT", [128, T])
    sin_d = din("sinT", [128, T])
    out = nc.dram_tensor("out", [T, D], F32, kind="ExternalOutput").ap()
    h1s = nc.dram_tensor("dbg_h1" if debug else "h1s", [T, D], F32, kind="ExternalOutput" if debug else "Internal").ap()
    dbg = {}
    if debug:
        dbg["yT"] = nc.dram_tensor("dbg_yT", [12 * 128, T], BF16, kind="ExternalOutput").ap()
        dbg["uT"] = nc.dram_tensor("dbg_uT", [8 * 128, T], BF16, kind="ExternalOutput").ap()

    with ExitStack() as st:
        S = Sched(nc, st)

        def sbuf(stk, name, shape, dt):
            return stk.enter_context(nc.sbuf_tensor("sb_" + name, list(shape), dt))

        def psum(stk, name, shape, dt):
            return stk.enter_context(nc.psum_tensor("ps_" + name, list(shape), dt))

        def dma(out_ap, in_ap, reads=(), writes=(), q="sp", **kw):
            S.add(q, lambda e: e.dma_start(out=out_ap, in_=in_ap, **kw), reads, writes, dma=True)

        def mm(o, lhsT, rhs, start, stop, reads, writes):
            S.add("pe", lambda e: e.matmul(o, lhsT=lhsT, rhs=rhs, start=start, stop=stop), reads, writes)

        def tr(o, in_, ident, reads, writes):
            S.add("pe", lambda e: e.transpose(o, in_, ident), reads, writes)

        def act(o, in_, func, reads, writes, **kw):
            S.add("act", lambda e: e.activation(out=o, in_=in_, func=func, **kw), reads, writes)

        def tt(o, a, b, op, reads, writes, eng="dve"):
            S.add(eng, lambda e: e.tensor_tensor(o, a, b, op), reads, writes)

        def ts(o, a, s1, s2, op0, op1, reads, writes, eng="dve"):
            if op1 is None:
                S.add(eng, lambda e: e.tensor_scalar(o, a, s1, None, op0), reads, writes)
            else:
                S.add(eng, lambda e: e.tensor_scalar(o, a, s1, s2, op0, op1), reads, writes)

        def stt(o, a, s, b, op0, op1, reads, writes, eng="dve"):
            S.add(eng, lambda e: e.scalar_tensor_tensor(o, a, s, b, op0, op1), reads, writes)

        def cp(o, a, reads, writes, eng="dve"):
            S.add(eng, lambda e: e.tensor_copy(o, a), reads, writes)

        def red(o, a, op, reads, writes, eng="dve"):
            S.add(eng, lambda e: e.tensor_reduce(o, a, AX.X, op), reads, writes)

        def memset(o, v, writes, eng="dve"):
            S.add(eng, lambda e: e.memset(o, v), (), writes)

        def recip(o, a, reads, writes):
            S.add("dve", lambda e: e.reciprocal(o, a), reads, writes)

        def rsqrt_small(o, a, scale, reads, writes):
            act(o, a, AF.Ln, list(reads) + ["epsb"], writes, scale=scale, bias=epsb[:])
            act(o, o, AF.Exp, writes, writes, scale=-0.5)

        cm = sbuf(st, "cm", [128, 10, 128], F32)
        identb = sbuf(st, "identb", [128, 128], BF16)
        onesb = sbuf(st, "onesb", [128, 128], BF16)
        epsb = sbuf(st, "epsb", [128, 1], F32)
        nmix = sbuf(st, "nmix", [128, 8], F32)
        nffn = sbuf(st, "nffn", [128, 8], F32)
        wstage = {}
        wcnt = [0]

        def alloc_wstage(stk):
            wstage["st"] = [sbuf(stk, f"wst{i}_{wcnt[0]}", [128, 8, 256], F32) for i in range(2)]
            wstage["bf"] = [sbuf(stk, f"wbf{i}_{wcnt[0]}", [128, 8, 256], BF16) for i in range(2)]

        dma(cm[:], cm_d, writes=["cm"])
        dma(nmix[:], nmix_d, writes=["nmix"])
        dma(nffn[:], nffn_d, writes=["nffn"])
        cp(identb[:], cm[:, CM_ID, :], ["cm"], ["identb"])
        cp(onesb[:], cm[:, CM_ONES, :], ["cm"], ["onesb"])
        memset(epsb[:], EPS, ["epsb"])
        ident = cm[:, CM_ID, :]

        def load_w(src_ap, ncols):
            wst, wbf = wstage["st"], wstage["bf"]
            i = wcnt[0] % 2
            wcnt[0] += 1
            dma(wst[i][:, :, 0:ncols], src_ap.rearrange("(c p) n -> p c n", p=128), writes=[f"wst{i}"])
            cp(wbf[i][:, :, 0:ncols], wst[i][:, :, 0:ncols], [f"wst{i}"], [f"wbf{i}"], eng="pool")
            return wbf[i], f"wbf{i}"

        with ExitStack() as mix:
            uT = sbuf(mix, "uT", [128, 8, T], BF16)
            yT = sbuf(mix, "yT", [128, 12, T], BF16)

            with ExitStack() as ph:
                xt = [sbuf(ph, f"xt{i}", [128, D], F32) for i in range(2)]
                sq = sbuf(ph, "sq", [128, D], F32)
                xs = [sbuf(ph, f"xs{i}", [128, D], BF16) for i in range(2)]
                ss = sbuf(ph, "ssA", [128, NT], F32)
                rstd = sbuf(ph, "rstdA", [128, NT], F32)
                pt = [psum(ph, f"ptA{i}", [128, 8, 128], BF16) for i in range(2)]
                memset(ss[:], 0.0, ["ssA"])
                for t in range(NT):
                    i = t % 2
                    dma(xt[i][:], x[t * 128:(t + 1) * 128, :], writes=[f"xt{i}"])
                    act(sq[:], xt[i][:], AF.Square, [f"xt{i}", "ssA"], ["sq", "ssA"], accum_out=ss[:, t:t + 1])
                    rsqrt_small(rstd[:, t:t + 1], ss[:, t:t + 1], 1.0 / D, ["ssA"], [f"rstdA{t}"])
                    ts(xs[i][:], xt[i][:], rstd[:, t:t + 1], None, ALU.mult, None, [f"xt{i}", f"rstdA{t}"], [f"xs{i}"])
                    for c in range(8):
                        tr(pt[i][:, c, :], xs[i][:, c * 128:(c + 1) * 128], identb[:], [f"xs{i}", "identb"], [f"ptA{i}"])
                    tt(uT[:, :, t * 128:(t + 1) * 128], pt[i][:], nmix[:, :].unsqueeze(2).to_broadcast([128, 8, 128]),
                       ALU.mult, [f"ptA{i}", "nmix"], [f"uT{t}"])
                S.barrier()
                S.emit()
                if stop_after == "A":
                    return nc
            uT_keys = [f"uT{t}" for t in range(NT)]

            def proj_fm(bank, bkey, wt, wkey, col, blk):
                for c in range(8):
                    mm(bank, wt[:, c, col:col + 128], uT[:, c, blk * 512:(blk + 1) * 512], c == 0, c == 7,
                       [wkey], [bkey])

            def proj_tm(bank_ap, bkey, wt_ap_fn, wkey, t):
                for c in range(8):
                    mm(bank_ap, uT[:, c, t * 128:(t + 1) * 128], wt_ap_fn(c), c == 0, c == 7, [wkey], [bkey])

            with ExitStack() as ph:
                qkvT = sbuf(ph, "qkvT", [128, 12, T], BF16)
                cwt = sbuf(ph, "cwt", [128, 12, 4], F32)
                dma(cwt[:], cw_d, writes=["cwt"])
                with ExitStack() as g1:
                    alloc_wstage(g1)
                    xc = [sbuf(g1, f"xc{i}", [128, 3 + T], BF16) for i in range(2)]
                    diag = [sbuf(g1, f"diag{i}", [128, 4, 128], BF16) for i in range(2)]
                    s16 = [sbuf(g1, f"s16_{i}", [128, 512], BF16) for i in range(2)]
                    sq16 = [sbuf(g1, f"sq16_{i}", [128, 512], BF16) for i in range(2)]
                    rn = [sbuf(g1, f"rn{i}", [128, 512], F32) for i in range(2)]
                    pb = [psum(g1, f"pbG{i}", [128, 512], F32) for i in range(8)]
                    for i in range(2):
                        memset(xc[i][:, 0:3], 0.0, [f"xc{i}"])
                    for ch in range(12):
                        i = ch % 2
                        if ch % 2 == 0:
                            wt, wkey = load_w(w_in[:, ch * 128:(ch + 2) * 128], 256)
                        col = (ch % 2) * 128
                        for blk in range(4):
                            proj_fm(pb[blk][:], f"pbG{blk}", wt, wkey, col, blk)
                            act(xc[i][:, 3 + blk * 512:3 + (blk + 1) * 512], pb[blk][:], AF.Copy, [f"pbG{blk}"], [f"xc{i}"])
                        for k in range(4):
                            ts(diag[i][:, k, :], identb[:], cwt[:, ch, k:k + 1], None, ALU.mult, None,
                               ["identb", "cwt"], [f"diag{i}"])
                        for blk in range(4):
                            b2 = 4 + (blk % 2)
                            j = blk % 2
                            for k in range(4):
                                mm(pb[b2][:], diag[i][:, k, :], xc[i][:, blk * 512 + k:blk * 512 + k + 512], k == 0, k == 3,
                                   [f"diag{i}", f"xc{i}"], [f"pbG{b2}"])
                            dst = qkvT[:, ch, blk * 512:(blk + 1) * 512]
                            act(dst, pb[b2][:], AF.Silu, [f"pbG{b2}"], [f"qkvT{ch}_{blk}"])
                    for ch in range(8):
                        for blk in range(4):
                            j = blk % 2
                            b3 = 6 + j
                            dst = qkvT[:, ch, blk * 512:(blk + 1) * 512]
                            tt(sq16[j][:], dst, dst, ALU.mult, [f"qkvT{ch}_{blk}"], [f"sq16_{j}"])
                            mm(pb[b3][:], onesb[:], sq16[j][:], True, True, ["onesb", f"sq16_{j}"], [f"pbG{b3}"])
                            act(rn[j][:], pb[b3][:], AF.Ln, [f"pbG{b3}", "epsb"], [f"rn{j}"], bias=epsb[:])
                            act(rn[j][:], rn[j][:], AF.Exp, [f"rn{j}"], [f"rn{j}"], scale=-0.5)
                            tt(dst, dst, rn[j][:], ALU.mult, [f"qkvT{ch}_{blk}", f"rn{j}"], [f"qkvT{ch}_{blk}"])
                    S.barrier()
                    S.emit()
                    if stop_after == "G1":
                        return nc
                gg = sbuf(ph, "gg", [128, NT, 4], F32)
                beta = sbuf(ph, "beta", [128, NT, 4], F32)
                wz = sbuf(ph, "wz", [128, 8, 512], BF16)
                gnw = sbuf(ph, "gnw", [128, 128], F32)
                with ExitStack() as g2:
                    alloc_wstage(g2)
                    ab = sbuf(g2, "ab", [128, NT, 8], F32)
                    tmp4 = sbuf(g2, "tmp4", [128, NT, 4], F32)
                    alog = sbuf(g2, "alog", [128, 4], F32)
                    dtb = sbuf(g2, "dtb", [128, 4], F32)
                    pab = psum(g2, "pab", [128, NT, 8], F32)
                    dma(alog[:], alog_d.partition_broadcast(128), writes=["alog"])
                    dma(dtb[:], dtb_d.partition_broadcast(128), writes=["dtb"])
                    dma(gnw[:], gnw_d.partition_broadcast(128), writes=["gnw"])
                    wt, wkey = load_w(w_in[:, 1536:1544], 8)
                    for t in range(NT):
                        proj_tm(pab[:, t, :], "pab", lambda c: wt[:, c, 0:8], wkey, t)
                    cp(ab[:], pab[:], ["pab"], ["ab"])
                    for j in range(2):
                        wt2, wkey2 = load_w(w_in[:, 1544 + j * 256:1544 + (j + 1) * 256], 256)
                        cp(wz[:, :, j * 256:(j + 1) * 256], wt2[:, :, 0:256], [wkey2], ["wz"])
                    tt(tmp4[:], ab[:, :, 0:4], dtb[:, :].unsqueeze(1).to_broadcast([128, NT, 4]), ALU.add, ["ab", "dtb"], ["tmp4"])
                    act(tmp4[:], tmp4[:], AF.Exp, ["tmp4"], ["tmp4"])
                    act(tmp4[:], tmp4[:], AF.Ln, ["tmp4"], ["tmp4"], bias=1.0)
                    act(alog[:], alog[:], AF.Exp, ["alog"], ["alog"])
                    stt(gg[:], tmp4[:], -1.0, alog[:, :].unsqueeze(1).to_broadcast([128, NT, 4]), ALU.mult, ALU.mult,
                        ["tmp4", "alog"], ["gg"])
                    act(beta[:], ab[:, :, 4:8], AF.Exp, ["ab"], ["beta"], scale=-1.0)
                    ts(beta[:], beta[:], 1.0, None, ALU.add, None, ["beta"], ["beta"])
                    recip(beta[:], beta[:], ["beta"], ["beta"])
                    S.barrier()
                    S.emit()
                    if stop_after == "G2":
                        return nc
                with ExitStack() as g3:
                    B0 = psum(g3, "BG0", [128, 512], F32)
                    BT = psum(g3, "BGT", [128, 8, 128], BF16)
                    H = [psum(g3, f"BH{h}", [128, 512], F32) for h in range(4)]
                    SC = [psum(g3, f"BSC{i}", [128, 512], F32) for i in range(2)]
                    gs = sbuf(g3, "gs", [128, 16], F32)
                    es = sbuf(g3, "es", [128, 16], F32)
                    bg = sbuf(g3, "bg", [128, 4], F32)
                    kbg = sbuf(g3, "kbg", [128, 4, 128], BF16)
                    kd = sbuf(g3, "kd", [128, 4, 128], BF16)
                    vb = sbuf(g3, "vb", [128, 4, 128], BF16)
                    Gg = [sbuf(g3, f"Gg{h}", [128, 128], F32) for h in range(4)]
                    Ed = [sbuf(g3, f"Ed{h}", [128, 2, 128], F32) for h in range(4)]
                    Dm = [sbuf(g3, f"Dm{h}", [128, 2, 128], F32) for h in range(4)]
                    LN = [[sbuf(g3, f"LN{h}_{i}", [128, 2, 128], F32) for i in range(2)] for h in range(4)]
                    Pm = [[sbuf(g3, f"Pm{h}_{i}", [128, 128], F32) for i in range(2)] for h in range(4)]
                    TTb = [sbuf(g3, f"TTb{h}", [128, 128], BF16) for h in range(4)]
                    dg = [sbuf(g3, f"dg{h}", [128, 128], BF16) for h in range(4)]
                    attnT = sbuf(g3, "attnT", [128, 4, 128], BF16)
                    qgT = sbuf(g3, "qgT", [128, 4, 128], BF16)
                    wT = sbuf(g3, "wT", [128, 4, 128], BF16)
                    uu = sbuf(g3, "uu", [128, 4, 128], F32)
                    vnew = sbuf(g3, "vnew", [128, 4, 128], BF16)
                    S32 = sbuf(g3, "S32", [128, 4, 128], F32)
                    S16 = sbuf(g3, "S16", [128, 4, 128], BF16)
                    oo = sbuf(g3, "oo", [128, 4, 128], F32)
                    osq = sbuf(g3, "osq", [128, 4, 128], F32)
                    oss = sbuf(g3, "oss", [128, 4], F32)
                    orst = sbuf(g3, "orst", [128, 4], F32)
                    sz = sbuf(g3, "sz", [128, 512], F32)
                    ya = sbuf(g3, "ya", [128, 512], BF16)
                    memset(S32[:], 0.0, [f"S32_{h}" for h in range(4)])
                    memset(S16[:], 0.0, [f"S16_{h}" for h in range(4)])
                    TL = cm[:, CM_TL, :]
                    SCL = float(128 ** -0.5)

                    def head_prep(n, h):
                        tsl = slice(n * 128, (n + 1) * 128)
                        Hh, hk = H[h], f"BH{h}"
                        kTh = qkvT[:, 4 + h, tsl]
                        qTh = qkvT[:, h, tsl]
                        ts(Gg[h][:], cm[:, CM_SU, :], gg[:, n, h:h + 1], None, ALU.mult, None, ["cm", "gg"], [f"Gg{h}"])
                        ts(dg[h][:], identb[:], es[:, h:h + 1], None, ALU.mult, None, ["identb", "es"], [f"dg{h}"])
                        yield
                        mm(Hh[:, 0:128], Gg[h][:], TL, True, True, [f"Gg{h}", "cm"], [hk])
                        mm(Hh[:, 128:256], TL, Gg[h][:], True, True, [f"Gg{h}", "cm"], [hk])
                        mm(Hh[:, 256:384], kTh, kTh, True, True, [], [hk])
                        mm(Hh[:, 384:512], kTh, qTh, True, True, [], [hk])
                        yield
                        act(Ed[h][:], Hh[:, 0:256].rearrange("p (a b) -> p a b", a=2), AF.Exp, [hk], [f"Ed{h}"])
                        yield
                        tt(Dm[h][:, 0, :], Ed[h][:, 0, :], cm[:, CM_MCT, :], ALU.mult, [f"Ed{h}", "cm"], [f"DmA{h}"], eng="pool")
                        tt(Dm[h][:, 1, :], Ed[h][:, 1, :], cm[:, CM_MLS, :], ALU.mult, [f"Ed{h}", "cm"], [f"DmB{h}"])
                        yield
                        stt(LN[h][0][:, 0, :], Hh[:, 256:384], beta[:, n, h:h + 1], Dm[h][:, 1, :], ALU.mult, ALU.mult,
                            [hk, "beta", f"DmB{h}"], [f"LN{h}_0"])
                        stt(attnT[:, h, :], Hh[:, 384:512], SCL, Dm[h][:, 0, :], ALU.mult, ALU.mult,
                            [hk, f"DmA{h}"], [f"attnT{h}"])
                        yield
                        mm(Hh[:, 0:128], LN[h][0][:, 0, :], ident, True, True, [f"LN{h}_0", "cm"], [hk])
                        yield
                        act(LN[h][0][:, 1, :], Hh[:, 0:128], AF.Copy, [hk], [f"LN{h}_0"])
                        yield
                        tt(Pm[h][0][:], ident, LN[h][0][:, 1, :], ALU.subtract, ["cm", f"LN{h}_0"], [f"Pm{h}_0"])
                        a, p = 0, 0
                        for lvl in range(1, 6):
                            na = 1 - a
                            Lc, Nc = LN[h][a][:, 0, :], LN[h][a][:, 1, :]
                            mm(Hh[:, 0:128], Nc, Lc, True, True, [f"LN{h}_{a}"], [hk])
                            if lvl < 5:
                                mm(Hh[:, 128:256], Lc, Nc, True, True, [f"LN{h}_{a}"], [hk])
                            yield
                            if lvl < 5:
                                act(LN[h][na][:], Hh[:, 0:256].rearrange("p (a b) -> p a b", a=2), AF.Copy, [hk], [f"LN{h}_{na}"])
                            else:
                                act(LN[h][na][:, 0, :], Hh[:, 0:128], AF.Copy, [hk], [f"LN{h}_{na}"])
                            yield
                            mm(Hh[:, 256:384], LN[h][na][:, 0, :], Pm[h][p][:], True, True, [f"LN{h}_{na}", f"Pm{h}_{p}"], [hk])
                            yield
                            if lvl < 5:
                                tt(Pm[h][1 - p][:], Hh[:, 256:384], Pm[h][p][:], ALU.add, [hk, f"Pm{h}_{p}"], [f"Pm{h}_{1 - p}"])
                            else:
                                tt(TTb[h][:], Hh[:, 256:384], Pm[h][p][:], ALU.add, [hk, f"Pm{h}_{p}"], [f"TTb{h}"])
                            yield
                            a, p = na, 1 - p
                        mm(Hh[:, 0:128], TTb[h][:], vb[:, h, :], True, True, [f"TTb{h}", "vb"], [hk])
                        mm(Hh[:, 128:256], kbg[:, h, :], TTb[h][:], True, True, [f"TTb{h}", "kbg"], [hk])
                        mm(Hh[:, 256:384], onesb[:], dg[h][:], True, True, ["onesb", f"dg{h}"], [hk])
                        yield
                        cp(uu[:, h, :], Hh[:, 0:128], [hk], [f"uu{h}"])
                        stt(qgT[:, h, :], Hh[:, 256:384], SCL, qTh, ALU.mult, ALU.mult, [hk], [f"qgT{h}"])
                        act(wT[:, h, :], Hh[:, 128:256], AF.Copy, [hk], [f"wT{h}"])
                        yield

                    def head_scan(n, h):
                        Hh, hk = H[h], f"BH{h}"
                        for half in range(2):
                            hs = slice(half * 64, (half + 1) * 64)
                            mm(Hh[:, 0:128], wT[:, h, :], S16[:, h, :], True, True, [f"wT{h}", f"S16_{h}"], [hk])
                            yield
                            tt(vnew[hs, h, :], uu[hs, h, :], Hh[hs, 0:128], ALU.subtract, [f"uu{h}", hk], [f"vnew{h}"])
                            yield
                            mm(Hh[:, 128:256], qgT[:, h, :], S16[:, h, :], True, False, [f"qgT{h}", f"S16_{h}"], [hk])
                            mm(Hh[:, 128:256], attnT[hs, h, :], vnew[hs, h, :], False, True, [f"attnT{h}", f"vnew{h}"], [hk])
                            mm(Hh[:, 256:384], kd[hs, h, :], vnew[hs, h, :], True, True, ["kd", f"vnew{h}"], [hk])
                            yield
                            stt(S32[:, h, :], S32[:, h, :], es[:, 8 + 4 * half + h:9 + 4 * half + h], Hh[:, 256:384],
                                ALU.mult, ALU.add, [hk, "es", f"S32_{h}"], [f"S32_{h}"])
                            act(oo[hs, h, :], Hh[hs, 128:256], AF.Copy, [hk], [f"oo{h}"])
                            yield
                            act(S16[:, h, :], S32[:, h, :], AF.Copy, [f"S32_{h}"], [f"S16_{h}"])
                            yield

                    def rr(gens):
                        gens = list(gens)
                        while gens:
                            for g in list(gens):
                                try:
                                    next(g)
                                except StopIteration:
                                    gens.remove(g)

                    G3N = int(os.environ.get("G3N", str(NT)))
                    for n in range(G3N):
                        tsl = slice(n * 128, (n + 1) * 128)
                        mm(B0[:, 0:4], TL, gg[:, n, :], True, True, ["cm", "gg"], ["BG0"])
                        mm(B0[:, 4:8], cm[:, CM_BO, :], gg[:, n, :], True, True, ["cm", "gg"], ["BG0"])
                        mm(B0[:, 8:12], cm[:, CM_SEL0, :], gg[:, n, :], True, True, ["cm", "gg"], ["BG0"])
                        mm(B0[:, 12:16], cm[:, CM_SEL1, :], gg[:, n, :], True, True, ["cm", "gg"], ["BG0"])
                        cp(gs[:], B0[:, 0:16], ["BG0"], ["gs"])
                        tt(gs[:, 4:8], gs[:, 4:8], gs[:, 0:4], ALU.subtract, ["gs"], ["gs"])
                        act(es[:], gs[:], AF.Exp, ["gs"], ["es"])
                        tt(bg[:], es[:, 0:4], beta[:, n, :], ALU.mult, ["es", "beta"], ["bg"])
                        for h in range(4):
                            tr(BT[:, h, :], qkvT[:, 4 + h, tsl], identb[:], ["identb"], ["BGT"])
                            tr(BT[:, 4 + h, :], qkvT[:, 8 + h, tsl], identb[:], ["identb"], ["BGT"])
                        tt(kbg[:], BT[:, 0:4, :], bg[:, :].unsqueeze(2).to_broadcast([128, 4, 128]), ALU.mult, ["BGT", "bg"], ["kbg"])
                        tt(kd[:], BT[:, 0:4, :], es[:, 4:8].unsqueeze(2).to_broadcast([128, 4, 128]), ALU.mult, ["BGT", "es"], ["kd"])
                        tt(vb[:], BT[:, 4:8, :], beta[:, n, :].unsqueeze(2).to_broadcast([128, 4, 128]), ALU.mult, ["BGT", "beta"], ["vb"])
                        rr(head_prep(n, h) for h in range(4))
                        rr(head_scan(n, h) for h in range(4))
                        ook = [f"oo{h}" for h in range(4)]
                        tt(osq[:], oo[:], oo[:], ALU.mult, ook, ["osq"])
                        red(oss[:], osq[:], ALU.add, ["osq"], ["oss"])
                        rsqrt_small(orst[:], oss[:], 1.0 / 128, ["oss"], ["orst"])
                        proj_tm(B0[:], "BG0", lambda c: wz[:, c, :], "wz", n)
                        act(sz[:], B0[:], AF.Silu, ["BG0"], ["sz"])
                        tt(osq[:], oo[:], orst[:, :].unsqueeze(2).to_broadcast([128, 4, 128]), ALU.mult, ook + ["orst"], ["osq"])
                        tt(osq[:], osq[:], gnw[:, :].unsqueeze(1).to_broadcast([128, 4, 128]), ALU.mult, ["osq", "gnw"], ["osq"])
                        tt(ya[:], osq[:].rearrange("p a b -> p (a b)"), sz[:], ALU.mult, ["osq", "sz"], ["ya"])
                        for h in range(4):
                            tr(BT[:, h, :], ya[:, h * 128:(h + 1) * 128], identb[:], ["ya", "identb"], ["BGT"])
                        cp(yT[:, 0:4, tsl], BT[:, 0:4, :], ["BGT"], [f"yT{n}"])
                    S.barrier()
                    S.emit()
                    if stop_after == "G3":
                        return nc

            with ExitStack() as ph:
                rqkT = sbuf(ph, "rqkT", [128, 8, T], BF16)
                retdt = sbuf(ph, "retdt", [128, 4, 128], F32)
                retvec = sbuf(ph, "retvec", [128, 8], F32)
                dma(retdt[:], retdt_d, writes=["retdt"])
                dma(retvec[:], retvec_d, writes=["retvec"])
                with ExitStack() as r1:
                    alloc_wstage(r1)
                    cosT = sbuf(r1, "cosT", [128, T], F32)
                    sinT = sbuf(r1, "sinT", [128, T], F32)
                    t1 = [sbuf(r1, f"t1_{i}", [128, 512], F32) for i in range(2)]
                    t2 = [sbuf(r1, f"t2_{i}", [128, 512], F32) for i in range(2)]
                    pb = [psum(r1, f"pbR{i}", [128, 512], F32) for i in range(8)]
                    dma(cosT[:], cos_d, writes=["cosT"])
                    dma(sinT[:], sin_d, writes=["sinT"])
                    for ch in range(8):
                        if ch % 2 == 0:
                            wt, wkey = load_w(w_in[:, 2056 + ch * 128:2056 + (ch + 2) * 128], 256)
                            wtp, wkeyp = load_w(w_perm[:, ch * 128:(ch + 2) * 128], 256)
                        col = (ch % 2) * 128
                        for blk in range(4):
                            j = blk % 2
                            b1, b2 = 2 * (blk % 4), 2 * (blk % 4) + 1
                            proj_fm(pb[b1][:], f"pbR{b1}", wt, wkey, col, blk)
                            proj_fm(pb[b2][:], f"pbR{b2}", wtp, wkeyp, col, blk)
                            bs = slice(blk * 512, (blk + 1) * 512)
                            tt(t1[j][:], pb[b1][:], cosT[:, bs], ALU.mult, [f"pbR{b1}", "cosT"], [f"t1_{j}"])
                            tt(t2[j][:], pb[b2][:], sinT[:, bs], ALU.mult, [f"pbR{b2}", "sinT"], [f"t2_{j}"])
                            tt(rqkT[:, ch, bs], t1[j][:], t2[j][:], ALU.add, [f"t1_{j}", f"t2_{j}"], [f"rqkT{ch}"], eng="pool")
                    S.barrier()
                    S.emit()
                    if stop_after == "R1":
                        return nc
                with ExitStack() as r2:
                    alloc_wstage(r2)
                    wv = sbuf(r2, "wv", [128, 8, 1024], BF16)
                    wgt = sbuf(r2, "wgt", [128, 8, 1024], BF16)
                    vtok = sbuf(r2, "vtok", [128, 1024], BF16)
                    szr = sbuf(r2, "szr", [128, 1024], F32)
                    kdk = sbuf(r2, "kdk", [128, 4, 128], BF16)
                    sc16 = [sbuf(r2, f"sc16_{h}", [128, 128], BF16) for h in range(4)]
                    R32 = sbuf(r2, "R32", [128, 4, 256], F32)
                    R16 = sbuf(r2, "R16", [128, 4, 256], BF16)
                    otmp = [sbuf(r2, f"otmp{h}", [128, 256], F32) for h in range(4)]
                    ro = sbuf(r2, "ro", [128, 4, 256], F32)
                    rsq = sbuf(r2, "rsq", [128, 4, 256], F32)
                    rss = sbuf(r2, "rss", [128, 4], F32)
                    rrst = sbuf(r2, "rrst", [128, 4], F32)
                    yb = sbuf(r2, "yb", [128, 1024], BF16)
                    Bv = [psum(r2, f"BRv{i}", [128, 512], F32) for i in range(2)]
                    H = [psum(r2, f"BRH{h}", [128, 512], F32) for h in range(4)]
                    BT = psum(r2, "BRT", [128, 8, 128], BF16)
                    for j in range(4):
                        wt2, wkey2 = load_w(w_in[:, 3080 + j * 256:3080 + (j + 1) * 256], 256)
                        cp(wv[:, :, j * 256:(j + 1) * 256], wt2[:, :, 0:256], [wkey2], ["wv"])
                    for j in range(4):
                        wt2, wkey2 = load_w(w_in[:, 4104 + j * 256:4104 + (j + 1) * 256], 256)
                        cp(wgt[:, :, j * 256:(j + 1) * 256], wt2[:, :, 0:256], [wkey2], ["wgt"])
                    memset(R32[:], 0.0, [f"R32_{h}" for h in range(4)])
                    memset(R16[:], 0.0, [f"R16_{h}" for h in range(4)])

                    def ret_head(n, h):
                        tsl = slice(n * 128, (n + 1) * 128)
                        Hh, hk = H[h], f"BRH{h}"
                        qTh = rqkT[:, h, tsl]
                        kTh = rqkT[:, 4 + h, tsl]
                        vh = vtok[:, h * 256:(h + 1) * 256]
                        mm(Hh[:, 0:128], kTh, qTh, True, True, [], [hk])
                        mm(Hh[:, 256:512], kdk[:, h, :], vh, True, True, ["kdk", "vtok"], [hk])
                        yield
                        tt(sc16[h][:], Hh[:, 0:128], retdt[:, h, :], ALU.mult, [hk, "retdt"], [f"sc16_{h}"])
                        stt(R32[:, h, :], R32[:, h, :], float(cdec[h]), Hh[:, 256:512], ALU.mult, ALU.add,
                            [hk, f"R32_{h}"], [f"R32_{h}"])
                        yield
                        mm(Hh[:, 0:256], sc16[h][:], vh, True, True, [f"sc16_{h}", "vtok"], [hk])
                        mm(Hh[:, 256:512], qTh, R16[:, h, :], True, True, [f"R16_{h}"], [hk])
                        yield
                        act(otmp[h][:], Hh[:, 0:256], AF.Copy, [hk], [f"otmp{h}"])
                        yield
                        stt(ro[:, h, :], Hh[:, 256:512], retvec[:, 4 + h:5 + h], otmp[h][:], ALU.mult, ALU.add,
                            [hk, f"otmp{h}", "retvec"], [f"ro{h}"])
                        act(R16[:, h, :], R32[:, h, :], AF.Copy, [f"R32_{h}"], [f"R16_{h}"])
                        yield

                    def rr2(gens):
                        gens = list(gens)
                        while gens:
                            for g in list(gens):
                                try:
                                    next(g)
                                except StopIteration:
                                    gens.remove(g)

                    for n in range(NT):
                        tsl = slice(n * 128, (n + 1) * 128)
                        for j in range(2):
                            proj_tm(Bv[j][:], f"BRv{j}", lambda c, j=j: wv[:, c, j * 512:(j + 1) * 512], "wv", n)
                            act(vtok[:, j * 512:(j + 1) * 512], Bv[j][:], AF.Copy, [f"BRv{j}"], ["vtok"])
                        for h in range(4):
                            tr(BT[:, h, :], rqkT[:, 4 + h, tsl], identb[:], ["identb"], ["BRT"])
                        tt(kdk[:], BT[:, 0:4, :], retvec[:, 0:4].unsqueeze(2).to_broadcast([128, 4, 128]), ALU.mult,
                           ["BRT", "retvec"], ["kdk"])
                        rr2(ret_head(n, h) for h in range(4))
                        for j in range(2):
                            proj_tm(Bv[j][:], f"BRv{j}", lambda c, j=j: wgt[:, c, j * 512:(j + 1) * 512], "wgt", n)
                            act(szr[:, j * 512:(j + 1) * 512], Bv[j][:], AF.Silu, [f"BRv{j}"], ["szr"])
                        rok = [f"ro{h}" for h in range(4)]
                        tt(rsq[:], ro[:], ro[:], ALU.mult, rok, ["rsq"])
                        red(rss[:], rsq[:], ALU.add, ["rsq"], ["rss"])
                        rsqrt_small(rrst[:], rss[:], 1.0 / 256, ["rss"], ["rrst"])
                        tt(rsq[:], ro[:], rrst[:, :].unsqueeze(2).to_broadcast([128, 4, 256]), ALU.mult, rok + ["rrst"], ["rsq"])
                        tt(yb[:], rsq[:].rearrange("p a b -> p (a b)"), szr[:], ALU.mult, ["rsq", "szr"], ["yb"])
                        for c in range(8):
                            tr(BT[:, c, :], yb[:, c * 128:(c + 1) * 128], identb[:], ["yb", "identb"], ["BRT"])
                        cp(yT[:, 4:12, tsl], BT[:], ["BRT"], [f"yT{n}"])
                    S.barrier()
                    S.emit()
                    if stop_after == "R2":
                        return nc
            if debug:
                for c in range(12):
                    dma(dbg["yT"][c * 128:(c + 1) * 128, :], yT[:, c, :], writes=["dbgyT"])
                for c in range(8):
                    dma(dbg["uT"][c * 128:(c + 1) * 128, :], uT[:, c, :], writes=["dbguT"])

            with ExitStack() as ph:
                mT = sbuf(ph, "mT", [128, 8, T], BF16)
                B = [psum(ph, f"BM{i}", [128, 512], F32) for i in range(8)]
                with ExitStack() as ms1:
                    wsm = [sbuf(ms1, f"wsm{i}", [128, 8, 128], F32) for i in range(4)]
                    wsb = [[sbuf(ms1, f"wsb{s_}_{i}", [128, 8, 128], BF16) for i in range(4)] for s_ in range(2)]
                    tA = [sbuf(ms1, f"tA{i}", [128, 512], F32) for i in range(2)]
                    m1 = [sbuf(ms1, f"m1_{i}", [128, 512], F32) for i in range(2)]
                    for ec in range(8):
                        es_ = slice(ec * 128, (ec + 1) * 128)
                        s_ = ec % 2
                        dma(wsm[0][:, 0:4, :], wupa_d[:, es_].rearrange("(c p) n -> p c n", p=128), writes=["wsm0"])
                        dma(wsm[1][:], wupr_d[:, es_].rearrange("(c p) n -> p c n", p=128), writes=["wsm1"])
                        dma(wsm[2][:], w_in[:, 5128 + ec * 128:5128 + (ec + 1) * 128].rearrange("(c p) n -> p c n", p=128), writes=["wsm2"])
                        dma(wsm[3][:], w_in[:, 6152 + ec * 128:6152 + (ec + 1) * 128].rearrange("(c p) n -> p c n", p=128), writes=["wsm3"])
                        cp(wsb[s_][0][:, 0:4, :], wsm[0][:, 0:4, :], ["wsm0"], [f"wsb{s_}_0"], eng="pool")
                        for i in range(1, 4):
                            cp(wsb[s_][i][:], wsm[i][:], [f"wsm{i}"], [f"wsb{s_}_{i}"], eng="pool")
                        for blk in range(4):
                            bs = slice(blk * 512, (blk + 1) * 512)
                            j = blk % 2
                            bA, bB, bMA, bMB = 4 * j, 4 * j + 1, 4 * j + 2, 4 * j + 3
                            for c in range(4):
                                mm(B[bA][:], wsb[s_][0][:, c, :], yT[:, c, bs], c == 0, c == 3, [f"wsb{s_}_0"], [f"BM{bA}"])
                            for c in range(8):
                                mm(B[bB][:], wsb[s_][1][:, c, :], yT[:, 4 + c, bs], c == 0, c == 7, [f"wsb{s_}_1"], [f"BM{bB}"])
                            for c in range(8):
                                mm(B[bMA][:], wsb[s_][2][:, c, :], uT[:, c, bs], c == 0, c == 7, [f"wsb{s_}_2"], [f"BM{bMA}"])
                            for c in range(8):
                                mm(B[bMB][:], wsb[s_][3][:, c, :], uT[:, c, bs], c == 0, c == 7, [f"wsb{s_}_3"], [f"BM{bMB}"])
                            act(tA[j][:], B[bMA][:], AF.Tanh, [f"BM{bMA}"], [f"tA{j}"], scale=0.5)
                            stt(m1[j][:], tA[j][:], 1.0, B[bA][:], ALU.add, ALU.mult, [f"tA{j}", f"BM{bA}"], [f"m1_{j}"])
                            act(tA[j][:], B[bMB][:], AF.Tanh, [f"BM{bMB}"], [f"tA{j}"], scale=0.5)
                            stt(tA[j][:], tA[j][:], 1.0, B[bB][:], ALU.add, ALU.mult, [f"tA{j}", f"BM{bB}"], [f"tA{j}"])
                            tt(mT[:, ec, bs], m1[j][:], tA[j][:], ALU.add, [f"m1_{j}", f"tA{j}"], [f"mT{blk}"], eng="pool")
                    S.barrier()
                    S.emit()
                with ExitStack() as ms2:
                    alloc_wstage(ms2)
                    wo = sbuf(ms2, "wo", [128, 8, 1024], BF16)
                    xr = [sbuf(ms2, f"xr{i}", [128, D], F32) for i in range(2)]
                    ho = [sbuf(ms2, f"ho{i}", [128, D], F32) for i in range(2)]
                    for j in range(4):
                        wt2, wkey2 = load_w(wout_d[:, j * 256:(j + 1) * 256], 256)
                        cp(wo[:, :, j * 256:(j + 1) * 256], wt2[:, :, 0:256], [wkey2], ["wo"])
                    for t in range(NT):
                        i = t % 2
                        dma(xr[i][:], x[t * 128:(t + 1) * 128, :], writes=[f"xr{i}"])
                        for hf in range(2):
                            bk = (2 * t + hf) % 8
                            for c in range(8):
                                mm(B[bk][:], mT[:, c, t * 128:(t + 1) * 128], wo[:, c, hf * 512:(hf + 1) * 512], c == 0, c == 7,
                                   ["wo"], [f"BM{bk}"])
                            stt(ho[i][:, hf * 512:(hf + 1) * 512], B[bk][:], 0.5, xr[i][:, hf * 512:(hf + 1) * 512], ALU.mult, ALU.add,
                                [f"BM{bk}", f"xr{i}"], [f"ho{i}"])
                        dma(h1s[t * 128:(t + 1) * 128, :], ho[i][:], reads=[f"ho{i}"], writes=["h1s"])
                    S.barrier()
                    S.emit()
                    if stop_after == "M":
                        return nc

        TS = 256
        NOVER = 16
        NTILE = 32 + NOVER
        NSLOT = NTILE * TS
        I32 = mybir.dt.int32
        XS = nc.dram_tensor("xs_scr", [NSLOT, D], BF16).ap()
        WS = nc.dram_tensor("ws_scr", [NSLOT, 1], F32).ap()
        YS = nc.dram_tensor("ys_scr", [NSLOT, D], F32).ap()
        TE = nc.dram_tensor("dbg_te" if debug else "te_scr", [128, NOVER], I32, kind="ExternalOutput" if debug else "Internal").ap()
        with ExitStack() as ph:
            hacc = sbuf(ph, "hacc", [128, NT, D], F32)
            s1i = sbuf(ph, "s1i", [128, NT], I32)
            s2i = sbuf(ph, "s2i", [128, NT], I32)
            w1v = sbuf(ph, "w1v", [128, NT], F32)
            w2v = sbuf(ph, "w2v", [128, NT], F32)
            gidx = sbuf(ph, "gidx", [128, NOVER, 8], I32)
            didx = sbuf(ph, "didx", [128, NOVER, 4], I32)
            for t in range(NT):
                dma(hacc[:, t, :], h1s[t * 128:(t + 1) * 128, :], reads=["h1s"], writes=[f"hacc{t}"])
            with ExitStack() as e1:
                xntok = sbuf(e1, "xntok", [128, NT, D], BF16)
                nfrow = sbuf(e1, "nfrow", [128, D], F32)
                misc = sbuf(e1, "misc", [128, 95], F32)
                hs_ = [sbuf(e1, f"hs{i}", [128, D], F32) for i in range(2)]
                sq = sbuf(e1, "sqE", [128, D], F32)
                ssE = sbuf(e1, "ssE", [128, NT], F32)
                rstE = sbuf(e1, "rstE", [128, NT], F32)
                xn32 = [sbuf(e1, f"xn32_{i}", [128, 8, 128], F32) for i in range(2)]
                wr = sbuf(e1, "wr", [128, 8, 36], F32)
                brt = sbuf(e1, "brt", [128, 36], F32)
                lg = sbuf(e1, "lg", [128, NT, 36], F32)
                PT = [psum(e1, f"PTE{i}", [128, 8, 128], F32) for i in range(2)]
                PL = psum(e1, "PLE", [128, 512], F32)
                PS1 = psum(e1, "PTE_rank", [128, 512], F32)
                PS2 = psum(e1, "PTE_cnt", [128, 512], F32)
                dma(wr[:], wr_d.rearrange("(c p) n -> p c n", p=128), writes=["wr"])
                dma(brt[:], br_d.partition_broadcast(128), writes=["brt"])
                dma(nfrow[:], nffnrow_d.partition_broadcast(128), writes=["nfrow"])
                dma(misc[:], misc_d, writes=["misc"])
                memset(ssE[:], 0.0, ["ssE"])
                for t in range(NT):
                    i = t % 2
                    act(sq[:], hacc[:, t, :], AF.Square, ["ssE", f"hacc{t}"], ["sqE", "ssE"], accum_out=ssE[:, t:t + 1])
                    rsqrt_small(rstE[:, t:t + 1], ssE[:, t:t + 1], 1.0 / D, ["ssE"], [f"rstE{t}"])
                    ts(hs_[i][:], hacc[:, t, :], rstE[:, t:t + 1], None, ALU.mult, None, [f"rstE{t}", f"hacc{t}"], [f"hs{i}"])
                    tt(xntok[:, t, :], hs_[i][:], nfrow[:], ALU.mult, [f"hs{i}", "nfrow"], [f"xntok{t}"], eng="pool")
                    for c in range(8):
                        mm(PT[i][:, c, :], hs_[i][:, c * 128:(c + 1) * 128], ident, True, True, [f"hs{i}", "cm"], [f"PTE{i}"])
                    tt(xn32[i][:], PT[i][:], nffn[:, :].unsqueeze(2).to_broadcast([128, 8, 128]), ALU.mult,
                       [f"PTE{i}", "nffn"], [f"xn32_{i}"])
                    for c in range(8):
                        mm(PL[:, 0:36], xn32[i][:, c, :], wr[:, c, :], c == 0, c == 7, [f"xn32_{i}", "wr"], ["PLE"])
                    tt(lg[:, t, :], PL[:, 0:36], brt[:], ALU.add, ["PLE", "brt"], ["lg"])
                gmax = sbuf(e1, "gmax", [128, NT], F32)
                ohg = sbuf(e1, "ohg", [128, NT, 4], F32)
                sh4 = sbuf(e1, "sh4", [128, NT, 4], F32)
                gw = sbuf(e1, "gw", [128, NT], F32)
                M32 = sbuf(e1, "M32", [128, NT, 32], F32)
                oh1 = sbuf(e1, "oh1", [128, NT, 32], F32)
                oh2 = sbuf(e1, "oh2", [128, NT, 32], F32)
                m1v = sbuf(e1, "m1v", [128, NT], F32)
                m2v = sbuf(e1, "m2v", [128, NT], F32)
                L4 = lg[:, :, 0:4]
                L32 = lg[:, :, 4:36]
                bc4 = lambda a: a.unsqueeze(2).to_broadcast([128, NT, 4])
                bc32 = lambda a: a.unsqueeze(2).to_broadcast([128, NT, 32])
                red(gmax[:], L4, ALU.max, ["lg"], ["gmax"])
                tt(ohg[:], L4, bc4(gmax[:, :]), ALU.is_equal, ["lg", "gmax"], ["ohg"])
                tt(sh4[:], L4, bc4(gmax[:, :]), ALU.subtract, ["lg", "gmax"], ["sh4"])
                act(sh4[:], sh4[:], AF.Exp, ["sh4"], ["sh4"])
                red(gw[:], sh4[:], ALU.add, ["sh4"], ["gw"])
                recip(gw[:], gw[:], ["gw"], ["gw"])
                ts(ohg[:], ohg[:], BIG, -BIG, ALU.mult, ALU.add, ["ohg"], ["ohg"])
                tt(M32[:].rearrange("p t (g e) -> p t g e", g=4), L32.rearrange("p t (g e) -> p t g e", g=4),
                   ohg[:, :, :].unsqueeze(3).to_broadcast([128, NT, 4, 8]), ALU.add, ["lg", "ohg"], ["M32"])
                red(m1v[:], M32[:], ALU.max, ["M32"], ["m1v"])
                tt(oh1[:], M32[:], bc32(m1v[:, :]), ALU.is_equal, ["M32", "m1v"], ["oh1"])
                stt(M32[:], oh1[:], -BIG, M32[:], ALU.mult, ALU.add, ["oh1", "M32"], ["M32"])
                red(m2v[:], M32[:], ALU.max, ["M32"], ["m2v"])
                tt(oh2[:], M32[:], bc32(m2v[:, :]), ALU.is_equal, ["M32", "m2v"], ["oh2"])
                tt(w2v[:], m2v[:], m1v[:], ALU.subtract, ["m1v", "m2v"], ["w2v"])
                act(w2v[:], w2v[:], AF.Exp, ["w2v"], ["w2v"])
                ts(w1v[:], w2v[:], 1.0, None, ALU.add, None, ["w2v"], ["w1v"])
                recip(w1v[:], w1v[:], ["w1v"], ["w1v"])
                tt(w2v[:], w2v[:], w1v[:], ALU.mult, ["w2v", "w1v"], ["w2v"])
                tt(w1v[:], w1v[:], gw[:], ALU.mult, ["w1v", "gw"], ["w1v"])
                tt(w2v[:], w2v[:], gw[:], ALU.mult, ["w2v", "gw"], ["w2v"])
                sel = sbuf(e1, "sel", [128, NT, 32], F32)
                tcs = sbuf(e1, "tcs", [128, NT, 32], F32)
                off = sbuf(e1, "off", [128, NT, 32], F32)
                slot = sbuf(e1, "slot", [128, NT, 32], F32)
                cnt = sbuf(e1, "cnt", [128, 32], F32)
                cmp3 = sbuf(e1, "cmp3", [128, 32, 7], F32)
                pfa = sbuf(e1, "pfa", [128, 32], F32)
                pfb = sbuf(e1, "pfb", [128, 32], F32)
                nov = sbuf(e1, "nov", [128, 32], F32)
                ost = sbuf(e1, "ost", [128, 32], F32)
                oen = sbuf(e1, "oen", [128, 32], F32)
                dlt = sbuf(e1, "dlt", [128, 32], F32)
                isov = sbuf(e1, "isov", [128, NT, 32], F32)
                s1f = sbuf(e1, "s1f", [128, NT], F32)
                s2f = sbuf(e1, "s2f", [128, NT], F32)
                A1 = sbuf(e1, "A1", [128, NOVER, 32], F32)
                A2 = sbuf(e1, "A2", [128, NOVER, 32], F32)
                tef = sbuf(e1, "tef", [128, NOVER], F32)
                tei = sbuf(e1, "tei", [128, NOVER], I32)
                bgf = sbuf(e1, "bgf", [128, NOVER], F32)
                gidxf = sbuf(e1, "gidxf", [128, NOVER, 8], F32)
                didxf = sbuf(e1, "didxf", [128, NOVER, 4], F32)
                thr3 = misc[:, 0:7]
                kk8 = misc[:, 7:23]
                eio = misc[:, 23:55]
                pc8 = misc[:, 55:63]
                e512 = misc[:, 63:95]
                flat = lambda a: a.rearrange("p t e -> p (t e)")
                tt(sel[:], oh1[:], oh2[:], ALU.add, ["oh1", "oh2"], ["sel"])
                mm(PS1[:], cm[:, CM_UT, :], flat(sel[:]), True, True, ["cm", "sel"], ["PTE_rank"])
                mm(PS2[:], cm[:, CM_ONES, :], flat(sel[:]), True, True, ["cm", "sel"], ["PTE_cnt"])
                cp(flat(tcs[:]), PS2[:], ["PTE_cnt"], ["tcs"])
                memset(off[:, 0, :], 0.0, ["off"])
                for t in range(1, NT):
                    tt(off[:, t, :], off[:, t - 1, :], tcs[:, t - 1, :], ALU.add, ["off", "tcs"], ["off"])
                tt(cnt[:], off[:, NT - 1, :], tcs[:, NT - 1, :], ALU.add, ["off", "tcs"], ["cnt"])
                tt(cmp3[:], cnt[:, :].unsqueeze(2).to_broadcast([128, 32, 7]), thr3.unsqueeze(1).to_broadcast([128, 32, 7]),
                   ALU.is_gt, ["cnt", "misc"], ["cmp3"])
                red(nov[:], cmp3[:], ALU.add, ["cmp3"], ["nov"])
                cp(pfa[:], nov[:], ["nov"], ["pfa"])
                cur, nxt, ck, nk = pfa, pfb, "pfa", "pfb"
                for dd in (1, 2, 4, 8, 16):
                    cp(nxt[:], cur[:], [ck], [nk])
                    tt(nxt[:, dd:32], cur[:, dd:32], cur[:, 0:32 - dd], ALU.add, [ck, nk], [nk])
                    cur, nxt, ck, nk = nxt, cur, nk, ck
                incl, ik = cur, ck
                ts(oen[:], incl[:], 32.0, None, ALU.add, None, [ik], ["oen"])
                tt(ost[:], oen[:], nov[:], ALU.subtract, ["oen", "nov"], ["ost"])
                ts(dlt[:], ost[:], float(TS), -float(TS), ALU.mult, ALU.add, ["ost"], ["dlt"])
                tt(dlt[:], dlt[:], e512, ALU.subtract, ["dlt", "misc"], ["dlt"])
                tt(flat(slot[:]), PS1[:], flat(off[:]), ALU.add, ["PTE_rank", "off"], ["slot"])
                ts(isov[:], slot[:], float(TS), None, ALU.is_ge, None, ["slot"], ["isov"])
                tt(isov[:], isov[:], dlt[:, :].unsqueeze(1).to_broadcast([128, NT, 32]), ALU.mult, ["isov", "dlt"], ["isov"])
                tt(slot[:], slot[:], e512.unsqueeze(1).to_broadcast([128, NT, 32]), ALU.add, ["slot", "misc"], ["slot"])
                tt(slot[:], slot[:], isov[:], ALU.add, ["slot", "isov"], ["slot"])
                tt(sel[:], oh1[:], slot[:], ALU.mult, ["oh1", "slot"], ["sel"])
                red(s1f[:], sel[:], ALU.add, ["sel"], ["s1f"])
                tt(sel[:], oh2[:], slot[:], ALU.mult, ["oh2", "slot"], ["sel"])
                red(s2f[:], sel[:], ALU.add, ["sel"], ["s2f"])
                cp(s1i[:], s1f[:], ["s1f"], ["s1i"])
                cp(s2i[:], s2f[:], ["s2f"], ["s2i"])
                kkb = kk8.unsqueeze(2).to_broadcast([128, NOVER, 32])
                tt(A1[:], kkb, ost[:, :].unsqueeze(1).to_broadcast([128, NOVER, 32]), ALU.is_ge, ["misc", "ost"], ["A1"])
                tt(A2[:], kkb, oen[:, :].unsqueeze(1).to_broadcast([128, NOVER, 32]), ALU.is_lt, ["misc", "oen"], ["A2"])
                tt(A1[:], A1[:], A2[:], ALU.mult, ["A1", "A2"], ["A1"])
                tt(A1[:], A1[:], eio.unsqueeze(1).to_broadcast([128, NOVER, 32]), ALU.mult, ["A1", "misc"], ["A1"])
                red(tef[:], A1[:], ALU.add, ["A1"], ["tef"])
                cp(tei[:], tef[:], ["tef"], ["tei"])
                dma(TE, tei[:], reads=["tei"], writes=["TE"])
                ts(bgf[:], tef[:], 1024.0, None, ALU.mult, None, ["tef"], ["bgf"])
                tt(gidxf[:], bgf[:, :].unsqueeze(2).to_broadcast([128, NOVER, 8]), pc8.unsqueeze(1).to_broadcast([128, NOVER, 8]),
                   ALU.add, ["bgf", "misc"], ["gidxf"])
                ts(bgf[:], tef[:], 512.0, None, ALU.mult, None, ["tef"], ["bgf"])
                tt(didxf[:], bgf[:, :].unsqueeze(2).to_broadcast([128, NOVER, 4]), pc8[:, 0:4].unsqueeze(1).to_broadcast([128, NOVER, 4]),
                   ALU.add, ["bgf", "misc"], ["didxf"])
                cp(gidx[:], gidxf[:], ["gidxf"], ["gidx"])
                cp(didx[:], didxf[:], ["didxf"], ["didx"])
                for t in range(NT):
                    for (si, wv_, nm) in ((s1i, w1v, "a"), (s2i, w2v, "b")):
                        S.add("pool", lambda e, t=t, si=si: e.indirect_dma_start(
                            out=XS[:, :], out_offset=bass.IndirectOffsetOnAxis(ap=si[:, t:t + 1], axis=0),
                            in_=xntok[:, t, :], in_offset=None),
                            reads=[f"xntok{t}", "s1i", "s2i"], writes=["XS"], dma=True)
                        S.add("pool", lambda e, t=t, si=si, wv_=wv_: e.indirect_dma_start(
                            out=WS[:, :], out_offset=bass.IndirectOffsetOnAxis(ap=si[:, t:t + 1], axis=0),
                            in_=wv_[:, t:t + 1], in_offset=None),
                            reads=["w1v", "w2v", "s1i", "s2i"], writes=["WS"], dma=True)
                if debug:
                    dbg["s12"] = nc.dram_tensor("dbg_s12", [128, 2 * NT], I32, kind="ExternalOutput").ap()
                    dma(dbg["s12"][:, 0:NT], s1i[:], reads=["s1i"], writes=["dbgs12"])
                    dma(dbg["s12"][:, NT:2 * NT], s2i[:], reads=["s2i"], writes=["dbgs12"])
                S.barrier()
                S.emit()
                if stop_after == "router":
                    return nc
            with ExitStack() as e2:
                NB = 3
                NH = TS // 128
                ewg = [sbuf(e2, f"ewg{i}", [128, 8, 512], BF16) for i in range(NB)]
                ewu = [sbuf(e2, f"ewu{i}", [128, 8, 512], BF16) for i in range(NB)]
                ewd = [sbuf(e2, f"ewd{i}", [128, 4, 1024], BF16) for i in range(NB)]
                xst = [sbuf(e2, f"xst{i}", [128, NH, D], BF16) for i in range(2)]
                wst_ = [sbuf(e2, f"wsl{i}", [128, NH], F32) for i in range(2)]
                xT = [sbuf(e2, f"xT{i}", [128, 8, TS], BF16) for i in range(2)]
                hidT = [sbuf(e2, f"hidT{i}", [128, 4, TS], BF16) for i in range(2)]
                sg = [sbuf(e2, f"sg{i}", [128, TS], BF16) for i in range(2)]
                yt = [sbuf(e2, f"yt{i}", [128, NH, D], F32) for i in range(2)]
                PTk = [psum(e2, f"BEt{i}", [128, 8, 128], BF16) for i in range(2)]
                Bgu = [psum(e2, f"BEg{i}", [128, 512], F32) for i in range(4)]
                Bd = [psum(e2, f"BEd{i}", [128, 512], F32) for i in range(2)]
                wg_flat = wg_d.rearrange("e d n -> (e d) n")
                wu_flat = wu_d.rearrange("e d n -> (e d) n")
                wd_flat = wd_d.rearrange("e f n -> (e f) n")

                def gather_w(dst, src_flat, idx_ap, key):
                    S.add("pool", lambda e: e.indirect_dma_start(
                        out=dst, out_offset=None, in_=src_flat[:, :],
                        in_offset=bass.IndirectOffsetOnAxis(ap=idx_ap, axis=0)),
                        reads=["gidx", "didx"], writes=[key], dma=True)

                dcount = [0]
                for k in range(NTILE):
                    b3 = k % NB
                    b2 = k % 2
                    rs = slice(k * TS, (k + 1) * TS)
                    if k < 32:
                        dma(ewg[b3][:], wg_d[k].rearrange("(c p) n -> p c n", p=128), writes=[f"ewg{b3}_{c}" for c in range(8)], q="pool")
                        dma(ewu[b3][:], wu_d[k].rearrange("(c p) n -> p c n", p=128), writes=[f"ewu{b3}_{c}" for c in range(8)], q="pool")
                        dma(ewd[b3][:], wd_d[k].rearrange("(c p) n -> p c n", p=128), writes=[f"ewd{b3}_{c}" for c in range(4)], q="pool")
                    else:
                        ko = k - 32
                        for c in range(8):
                            gather_w(ewg[b3][:, c, :], wg_flat, gidx[:, ko, c:c + 1], f"ewg{b3}_{c}")
                        for c in range(8):
                            gather_w(ewu[b3][:, c, :], wu_flat, gidx[:, ko, c:c + 1], f"ewu{b3}_{c}")
                        for c in range(4):
                            gather_w(ewd[b3][:, c, :], wd_flat, didx[:, ko, c:c + 1], f"ewd{b3}_{c}")
                    dma(xst[b2][:], XS[rs, :].rearrange("(h p) d -> p h d", p=128), reads=["XS"], writes=[f"xst{b2}"])
                    dma(wst_[b2][:], WS[rs, :].rearrange("(h p) o -> p (h o)", p=128), reads=["WS"], writes=[f"wsl{b2}"],
                        allow_slow_non_contiguous=True)
                    for hh in range(NH):
                        pj = hh % 2
                        for c in range(8):
                            tr(PTk[pj][:, c, :], xst[b2][:, hh, c * 128:(c + 1) * 128], identb[:], [f"xst{b2}", "identb"], [f"BEt{pj}"])
                        if pj == 0:
                            cp(xT[b2][:, :, hh * 128:(hh + 1) * 128], PTk[pj][:], [f"BEt{pj}"], [f"xT{b2}"])
                        else:
                            act(xT[b2][:, :, hh * 128:(hh + 1) * 128], PTk[pj][:], AF.Copy, [f"BEt{pj}"], [f"xT{b2}"])
                    for f in range(4):
                        j = f % 2
                        bg_, bu_ = Bgu[2 * j], Bgu[2 * j + 1]
                        for c in range(8):
                            mm(bg_[:, 0:TS], ewg[b3][:, c, f * 128:(f + 1) * 128], xT[b2][:, c, :], c == 0, c == 7,
                               [f"ewg{b3}_{c}", f"xT{b2}"], [f"BEg{2 * j}"])
                        for c in range(8):
                            mm(bu_[:, 0:TS], ewu[b3][:, c, f * 128:(f + 1) * 128], xT[b2][:, c, :], c == 0, c == 7,
                               [f"ewu{b3}_{c}", f"xT{b2}"], [f"BEg{2 * j + 1}"])
                        act(sg[j][:], bg_[:, 0:TS], AF.Silu, [f"BEg{2 * j}"], [f"sg{j}"])
                        tt(hidT[b2][:, f, :], bu_[:, 0:TS], sg[j][:], ALU.mult, [f"BEg{2 * j + 1}", f"sg{j}"], [f"hidT{b2}"])
                    for hh in range(NH):
                        for cc in range(2):
                            bk = dcount[0] % 2
                            dcount[0] += 1
                            for f in range(4):
                                mm(Bd[bk][:], hidT[b2][:, f, hh * 128:(hh + 1) * 128], ewd[b3][:, f, cc * 512:(cc + 1) * 512],
                                   f == 0, f == 3, [f"ewd{b3}_{f}", f"hidT{b2}"], [f"BEd{bk}"])
                            if bk == 0:
                                ts(yt[b2][:, hh, cc * 512:(cc + 1) * 512], Bd[bk][:], wst_[b2][:, hh:hh + 1], None, ALU.mult, None,
                                   [f"BEd{bk}", f"wsl{b2}"], [f"yt{b2}"])
                            else:
                                act(yt[b2][:, hh, cc * 512:(cc + 1) * 512], Bd[bk][:], AF.Copy, [f"BEd{bk}", f"wsl{b2}"], [f"yt{b2}"],
                                    scale=wst_[b2][:, hh:hh + 1])
                    dma(YS[rs, :].rearrange("(h p) d -> p h d", p=128), yt[b2][:], reads=[f"yt{b2}"], writes=["YS"])
                S.barrier()
                S.emit()
                if stop_after == "experts":
                    return nc
            with ExitStack() as e3:
                nfw = sbuf(e3, "nfw", [128, D], F32)
                sq = sbuf(e3, "sqF", [128, D], F32)
                ssF = sbuf(e3, "ssF", [128, NT], F32)
                rstF = sbuf(e3, "rstF", [128, NT], F32)
                y1 = [sbuf(e3, f"y1_{i}", [128, D], F32) for i in range(2)]
                y2 = [sbuf(e3, f"y2_{i}", [128, D], F32) for i in range(2)]
                ob = [sbuf(e3, f"ob{i}", [128, D], F32) for i in range(2)]
                dma(nfw[:], nfin_d.partition_broadcast(128), writes=["nfw"])
                memset(ssF[:], 0.0, ["ssF"])
                for t in range(NT):
                    i = t % 2
                    S.add("pool", lambda e, t=t, i=i: e.indirect_dma_start(
                        out=y1[i][:, :], out_offset=None, in_=YS[:, :],
                        in_offset=bass.IndirectOffsetOnAxis(ap=s1i[:, t:t + 1], axis=0)),
                        reads=["YS", "s1i"], writes=[f"y1_{i}"], dma=True)
                    S.add("pool", lambda e, t=t, i=i: e.indirect_dma_start(
                        out=y2[i][:, :], out_offset=None, in_=YS[:, :],
                        in_offset=bass.IndirectOffsetOnAxis(ap=s2i[:, t:t + 1], axis=0)),
                        reads=["YS", "s2i"], writes=[f"y2_{i}"], dma=True)
                    tt(y1[i][:], y1[i][:], y2[i][:], ALU.add, [f"y1_{i}", f"y2_{i}"], [f"y1_{i}"], eng="pool")
                    tt(hacc[:, t, :], hacc[:, t, :], y1[i][:], ALU.add, [f"y1_{i}", f"hacc{t}"], [f"hacc{t}"])
                    act(sq[:], hacc[:, t, :], AF.Square, ["ssF", f"hacc{t}"], ["sqF", "ssF"], accum_out=ssF[:, t:t + 1])
                    rsqrt_small(rstF[:, t:t + 1], ssF[:, t:t + 1], 1.0 / D, ["ssF"], [f"rstF{t}"])
                    stt(ob[i][:], hacc[:, t, :], rstF[:, t:t + 1], nfw[:], ALU.mult, ALU.mult, [f"rstF{t}", "nfw", f"hacc{t}"], [f"ob{i}"])
                    dma(out[t * 128:(t + 1) * 128, :], ob[i][:], reads=[f"ob{i}"], writes=["out"])
                S.barrier()
                S.emit()
    return nc


_CACHE = {}


def _host_inputs(inputs):
    f = lambda a: np.ascontiguousarray(np.asarray(a, dtype=np.float32))
    w_in = f(inputs["w_in"][0])
    perm = np.arange(1024) ^ 1
    w_perm = np.ascontiguousarray(w_in[:, 2056:3080][:, perm])
    cw = np.ascontiguousarray(f(inputs["conv_w"][0]).T.reshape(12, 128, 4).transpose(1, 0, 2))
    cm, ret_dt, retvec, cdec, cosT, sinT = make_consts()
    shared = {
        "w_in": w_in, "w_perm": w_perm, "cw": cw,
        "A_log": f(inputs["A_log"][0]), "dt_bias": f(inputs["dt_bias"][0]),
        "gdn_norm_w": f(inputs["gdn_norm_w"][0]),
        "w_up_gdn": f(inputs["w_up_gdn"][0]), "w_up_ret": f(inputs["w_up_ret"][0]), "w_out": f(inputs["w_out"][0]),
        "nmix": np.ascontiguousarray(f(inputs["norm_mix_w"][0]).reshape(8, 128).T),
        "nffn": np.ascontiguousarray(f(inputs["norm_ffn_w"][0]).reshape(8, 128).T),
        "w_router": np.ascontiguousarray(np.concatenate([f(inputs["w_group"][0]), f(inputs["w_expert"][0])], axis=1)),
        "b_router": np.ascontiguousarray(np.concatenate([f(inputs["b_group"][0]), f(inputs["b_expert"][0])], axis=0)),
        "w_gate": f(inputs["w_gate"][0]), "w_up": f(inputs["w_up"][0]), "w_down": f(inputs["w_down"][0]),
        "norm_final_w": f(inputs["norm_final_w"]),
        "misc": np.ascontiguousarray(np.concatenate([
            np.broadcast_to(np.concatenate([256.0 * np.arange(1, 8), 32.0 + np.arange(16), np.arange(32)]).astype(np.float32)[None, :], (128, 55)),
            (np.arange(8)[None, :] * 128 + np.arange(128)[:, None]).astype(np.float32),
            np.broadcast_to((256.0 * np.arange(32)).astype(np.float32)[None, :], (128, 32))], axis=1)),
        "nffn_row": f(inputs["norm_ffn_w"][0]),
        "cm": cm, "ret_dt": ret_dt, "retvec": retvec, "cosT": cosT, "sinT": sinT,
    }
    return shared


def kernel(**inputs):
    x = np.asarray(inputs["x"], dtype=np.float32)
    nb = x.shape[0]
    shared = _host_inputs(inputs)
    nc = build()
    in_maps = []
    for b in range(nb):
        m = dict(shared)
        m["x"] = np.ascontiguousarray(x[b])
        in_maps.append(m)
    res = run_bass_kernel_spmd(nc, in_maps, core_ids=list(range(nb)))
    return np.stack([np.asarray(r["out"], dtype=np.float32) for r in res.results], axis=0)
```

```python
import os
from contextlib import ExitStack
import numpy as np
import concourse.bass as bass
import concourse.mybir as mybir
from concourse.bass_utils import run_bass_kernel_spmd

F32 = mybir.dt.float32
BF16 = mybir.dt.bfloat16
AF = mybir.ActivationFunctionType
ALU = mybir.AluOpType
AX = mybir.AxisListType

T = 2048
D = 1024
NT = 16
EPS = 1e-6
ENGINES = ("pe", "act", "dve", "pool", "sp")
N_DMA_SEMS = 12
BIG = 1.0e30


class Sched:
    def __init__(self, nc, stack):
        self.nc = nc
        self.ops = []
        self.last_writer = {}
        self.readers = {}
        self.sem = {e: stack.enter_context(nc.semaphore("s_" + e)) for e in ENGINES}
        self.dma_sems = {}
        for q in ("sp", "act", "pool"):
            self.dma_sems[q] = [stack.enter_context(nc.semaphore(f"d_{q}{i}")) for i in range(N_DMA_SEMS)]
        self.n_dma = {q: 0 for q in self.dma_sems}
        self.cnt = {e: 0 for e in ENGINES}
        self.waited = {e: {} for e in ENGINES}

    PSUM_PREFIXES = ("ptA", "pbG", "pab", "BG", "BH", "BSC", "pbR", "BR", "BM", "PTE", "PLE", "BE")

    def add(self, eng, fn, reads=(), writes=(), dma=False):
        ex = [k for k in reads if k.startswith(self.PSUM_PREFIXES)]
        if ex:
            reads = [k for k in reads if k not in ex]
            writes = list(writes) + ex
        idx = len(self.ops)
        deps = set()
        for k in reads:
            w = self.last_writer.get(k)
            if w is not None:
                deps.add((w, "raw"))
        for k in writes:
            w = self.last_writer.get(k)
            if w is not None:
                deps.add((w, "waw"))
            for r in self.readers.get(k, ()):
                deps.add((r, "war"))
        op = dict(eng=eng, fn=fn, deps=deps, dma=dma, idx=idx, signal=False, ticket=None, dsem=None)
        if dma:
            n = self.n_dma[eng]
            self.n_dma[eng] = n + 1
            op["dsem"] = (eng, n % N_DMA_SEMS, 16 * (n // N_DMA_SEMS + 1))
        self.ops.append(op)
        for k in reads:
            self.readers.setdefault(k, []).append(idx)
        for k in writes:
            self.last_writer[k] = idx
            self.readers[k] = []
        return idx

    def barrier(self):
        keys = set(self.last_writer) | set(self.readers)
        keys.add("__bar__")
        for e in ENGINES:
            self.add(e, None, reads=(), writes=tuple(keys))

    def emit(self):
        ops = self.ops
        for op in ops:
            for (d, kind) in op["deps"]:
                dop = ops[d]
                if dop["dma"]:
                    continue
                if dop["eng"] == op["eng"]:
                    if op["eng"] in ("pe", "sp") or kind == "war":
                        continue
                dop["signal"] = True
        for op in ops:
            if op["dma"]:
                continue
            if op["signal"]:
                self.cnt[op["eng"]] += 1
                op["ticket"] = self.cnt[op["eng"]]
        per_eng = {e: [op for op in ops if op["eng"] == e] for e in ENGINES}
        sem = self.sem
        dma_sems = self.dma_sems

        def run(e, engobj):
            waited = self.waited[e]
            for op in per_eng[e]:
                waits = {}
                for (d, kind) in op["deps"]:
                    dop = ops[d]
                    if dop["dma"]:
                        q, si, val = dop["dsem"]
                        key = ("d", q, si)
                        waits[key] = max(waits.get(key, 0), val)
                        continue
                    if dop["eng"] == e:
                        if e in ("pe", "sp") or kind == "war":
                            continue
                    if dop["ticket"] is None:
                        continue
                    key = ("e", dop["eng"])
                    waits[key] = max(waits.get(key, 0), dop["ticket"])
                if op["dma"]:
                    q, si, val = op["dsem"]
                    if val > 16:
                        key = ("d", q, si)
                        waits[key] = max(waits.get(key, 0), val - 16)
                for key, val in waits.items():
                    if waited.get(key, 0) >= val:
                        continue
                    waited[key] = val
                    s = sem[key[1]] if key[0] == "e" else dma_sems[key[1]][key[2]]
                    engobj.wait_ge(s, val)
                if op["fn"] is None:
                    if op["signal"]:
                        engobj.nop().then_inc(sem[e], 1)
                    continue
                ins = op["fn"](engobj)
                if op["dma"]:
                    q, si, val = op["dsem"]
                    ins.then_inc(dma_sems[q][si], 16)
                elif op["signal"]:
                    ins.then_inc(sem[e], 1)

        with self.nc.Block() as block:
            @block.tensor
            def _(eng):
                run("pe", eng)

            @block.scalar
            def _(eng):
                run("act", eng)

            @block.vector
            def _(eng):
                run("dve", eng)

            @block.gpsimd
            def _(eng):
                run("pool", eng)

            @block.sync
            def _(eng):
                run("sp", eng)
        self.ops = []
        self.last_writer = {}
        self.readers = {}


CM_ID, CM_TL, CM_SU, CM_MLS, CM_MCT, CM_SEL0, CM_SEL1, CM_BO, CM_ONES, CM_UT = range(10)


def make_consts():
    i = np.arange(128)
    same = (i[:, None] // 64) == (i[None, :] // 64)
    cm = np.zeros((10, 128, 128), np.float32)
    cm[CM_ID] = np.eye(128)
    cm[CM_TL] = same & (i[:, None] <= i[None, :])
    cm[CM_SU] = same & (i[:, None] > i[None, :])
    cm[CM_MLS] = same & (i[:, None] > i[None, :])
    cm[CM_MCT] = same & (i[None, :] >= i[:, None])
    cm[CM_SEL0] = (i[:, None] < 64) & np.ones((1, 128), bool)
    cm[CM_SEL1] = (i[:, None] >= 64) & np.ones((1, 128), bool)
    cm[CM_BO] = same
    cm[CM_ONES] = 1.0
    cm[CM_UT] = i[:, None] < i[None, :]
    cm = np.ascontiguousarray(cm.transpose(1, 0, 2))
    h = np.arange(4, dtype=np.float64)
    lg = np.log(1.0 - 2.0 ** (-5.0 - h))
    idx = np.arange(128, dtype=np.float64)
    rel = idx[None, :] - idx[:, None]
    dt = np.where(rel[None] >= 0, np.exp(np.maximum(rel[None], 0) * lg[:, None, None]), 0.0) * (128 ** -0.5)
    ret_dt = np.ascontiguousarray(dt.transpose(1, 0, 2)).astype(np.float32)
    kdec = np.exp(lg[None, :] * (127.0 - idx)[:, None]) * (128 ** -0.5)
    qdec = np.exp(lg[None, :] * (idx + 1.0)[:, None])
    retvec = np.concatenate([kdec, qdec], axis=1).astype(np.float32)
    cdec = [float(np.exp(lg[k] * 128.0)) for k in range(4)]
    inv_freq = 1.0 / (10000.0 ** np.linspace(0.0, 1.0, 64).astype(np.float32).astype(np.float64))
    ang = np.arange(T, dtype=np.float64)[None, :] * np.repeat(inv_freq, 2)[:, None].astype(np.float32).astype(np.float64)
    ang32 = (np.arange(T, dtype=np.float32)[None, :] * np.repeat(inv_freq.astype(np.float32), 2)[:, None]).astype(np.float64)
    cosT = np.cos(ang32).astype(np.float32)
    sgn = np.where(np.arange(128) % 2 == 0, -1.0, 1.0)[:, None]
    sinT = (np.sin(ang32) * sgn).astype(np.float32)
    return cm, ret_dt, retvec, cdec, cosT, sinT


def build(debug=False, stop_after=None):
    nc = bass.Bass("TRN2", target_bir_lowering=False)
    cdec = make_consts()[3]

    def din(name, shape):
        return nc.dram_tensor(name, list(shape), F32, kind="ExternalInput").ap()

    x = din("x", [T, D])
    w_in = din("w_in", [D, 7176])
    w_perm = din("w_perm", [D, 1024])
    cw_d = din("cw", [128, 12, 4])
    alog_d = din("A_log", [4])
    dtb_d = din("dt_bias", [4])
    gnw_d = din("gdn_norm_w", [128])
    wupa_d = din("w_up_gdn", [512, D])
    wupr_d = din("w_up_ret", [D, D])
    wout_d = din("w_out", [D, D])
    nmix_d = din("nmix", [128, 8])
    nffn_d = din("nffn", [128, 8])
    wr_d = din("w_router", [D, 36])
    br_d = din("b_router", [36])
    wg_d = din("w_gate", [32, D, 512])
    wu_d = din("w_up", [32, D, 512])
    wd_d = din("w_down", [32, 512, D])
    nfin_d = din("norm_final_w", [D])
    cm_d = din("cm", [128, 10, 128])
    misc_d = din("misc", [128, 96])
    nffnrow_d = din("nffn_row", [D])
    retdt_d = din("ret_dt", [128, 4, 128])
    retvec_d = din("retvec", [128, 8])
    cos_d = din("cosT", [128, T])
    sin_d = din("sinT", [128, T])
    out = nc.dram_tensor("out", [T, D], F32, kind="ExternalOutput").ap()
    h1s = nc.dram_tensor("dbg_h1" if debug else "h1s", [T, D], F32, kind="ExternalOutput" if debug else "Internal").ap()
    dbg = {}
    if debug:
        dbg["yT"] = nc.dram_tensor("dbg_yT", [12 * 128, T], BF16, kind="ExternalOutput").ap()
        dbg["uT"] = nc.dram_tensor("dbg_uT", [8 * 128, T], BF16, kind="ExternalOutput").ap()

    with ExitStack() as st:
        S = Sched(nc, st)

        def sbuf(stk, name, shape, dt):
            return stk.enter_context(nc.sbuf_tensor("sb_" + name, list(shape), dt))

        def psum(stk, name, shape, dt):
            return stk.enter_context(nc.psum_tensor("ps_" + name, list(shape), dt))

        def dma(out_ap, in_ap, reads=(), writes=(), q="sp", **kw):
            S.add(q, lambda e: e.dma_start(out=out_ap, in_=in_ap, **kw), reads, writes, dma=True)

        def mm(o, lhsT, rhs, start, stop, reads, writes):
            S.add("pe", lambda e: e.matmul(o, lhsT=lhsT, rhs=rhs, start=start, stop=stop), reads, writes)

        def tr(o, in_, ident, reads, writes):
            S.add("pe", lambda e: e.transpose(o, in_, ident), reads, writes)

        def act(o, in_, func, reads, writes, **kw):
            S.add("act", lambda e: e.activation(out=o, in_=in_, func=func, **kw), reads, writes)

        def tt(o, a, b, op, reads, writes, eng="dve"):
            S.add(eng, lambda e: e.tensor_tensor(o, a, b, op), reads, writes)

        def ts(o, a, s1, s2, op0, op1, reads, writes, eng="dve"):
            if op1 is None:
                S.add(eng, lambda e: e.tensor_scalar(o, a, s1, None, op0), reads, writes)
            else:
                S.add(eng, lambda e: e.tensor_scalar(o, a, s1, s2, op0, op1), reads, writes)

        def stt(o, a, s, b, op0, op1, reads, writes, eng="dve"):
            S.add(eng, lambda e: e.scalar_tensor_tensor(o, a, s, b, op0, op1), reads, writes)

        def cp(o, a, reads, writes, eng="dve"):
            S.add(eng, lambda e: e.tensor_copy(o, a), reads, writes)

        def red(o, a, op, reads, writes, eng="dve"):
            S.add(eng, lambda e: e.tensor_reduce(o, a, AX.X, op), reads, writes)

        def memset(o, v, writes, eng="dve"):
            S.add(eng, lambda e: e.memset(o, v), (), writes)

        def recip(o, a, reads, writes):
            S.add("dve", lambda e: e.reciprocal(o, a), reads, writes)

        def rsqrt_small(o, a, scale, reads, writes):
            act(o, a, AF.Ln, list(reads) + ["epsb"], writes, scale=scale, bias=epsb[:])
            act(o, o, AF.Exp, writes, writes, scale=-0.5)

        cm = sbuf(st, "cm", [128, 10, 128], F32)
        identb = sbuf(st, "identb", [128, 128], BF16)
        onesb = sbuf(st, "onesb", [128, 128], BF16)
        epsb = sbuf(st, "epsb", [128, 1], F32)
        nmix = sbuf(st, "nmix", [128, 8], F32)
        nffn = sbuf(st, "nffn", [128, 8], F32)
        wstage = {}
        wcnt = [0]

        def alloc_wstage(stk):
            wstage["st"] = [sbuf(stk, f"wst{i}_{wcnt[0]}", [128, 8, 256], F32) for i in range(2)]
            wstage["bf"] = [sbuf(stk, f"wbf{i}_{wcnt[0]}", [128, 8, 256], BF16) for i in range(2)]

        dma(cm[:], cm_d, writes=["cm"])
        dma(nmix[:], nmix_d, writes=["nmix"])
        dma(nffn[:], nffn_d, writes=["nffn"])
        cp(identb[:], cm[:, CM_ID, :], ["cm"], ["identb"])
        cp(onesb[:], cm[:, CM_ONES, :], ["cm"], ["onesb"])
        memset(epsb[:], EPS, ["epsb"])
        ident = cm[:, CM_ID, :]

        def load_w(src_ap, ncols):
            wst, wbf = wstage["st"], wstage["bf"]
            i = wcnt[0] % 2
            wcnt[0] += 1
            dma(wst[i][:, :, 0:ncols], src_ap.rearrange("(c p) n -> p c n", p=128), writes=[f"wst{i}"])
            cp(wbf[i][:, :, 0:ncols], wst[i][:, :, 0:ncols], [f"wst{i}"], [f"wbf{i}"], eng="pool")
            return wbf[i], f"wbf{i}"

        with ExitStack() as mix:
            uT = sbuf(mix, "uT", [128, 8, T], BF16)
            yT = sbuf(mix, "yT", [128, 12, T], BF16)

            with ExitStack() as ph:
                xt = [sbuf(ph, f"xt{i}", [128, D], F32) for i in range(2)]
                sq = sbuf(ph, "sq", [128, D], F32)
                xs = [sbuf(ph, f"xs{i}", [128, D], BF16) for i in range(2)]
                ss = sbuf(ph, "ssA", [128, NT], F32)
                rstd = sbuf(ph, "rstdA", [128, NT], F32)
                pt = [psum(ph, f"ptA{i}", [128, 8, 128], BF16) for i in range(2)]
                memset(ss[:], 0.0, ["ssA"])
                for t in range(NT):
                    i = t % 2
                    dma(xt[i][:], x[t * 128:(t + 1) * 128, :], writes=[f"xt{i}"])
                    act(sq[:], xt[i][:], AF.Square, [f"xt{i}", "ssA"], ["sq", "ssA"], accum_out=ss[:, t:t + 1])
                    rsqrt_small(rstd[:, t:t + 1], ss[:, t:t + 1], 1.0 / D, ["ssA"], [f"rstdA{t}"])
                    ts(xs[i][:], xt[i][:], rstd[:, t:t + 1], None, ALU.mult, None, [f"xt{i}", f"rstdA{t}"], [f"xs{i}"])
                    for c in range(8):
                        tr(pt[i][:, c, :], xs[i][:, c * 128:(c + 1) * 128], identb[:], [f"xs{i}", "identb"], [f"ptA{i}"])
                    tt(uT[:, :, t * 128:(t + 1) * 128], pt[i][:], nmix[:, :].unsqueeze(2).to_broadcast([128, 8, 128]),
                       ALU.mult, [f"ptA{i}", "nmix"], [f"uT{t}"])
                S.barrier()
                S.emit()
                if stop_after == "A":
                    return nc
            uT_keys = [f"uT{t}" for t in range(NT)]

            def proj_fm(bank, bkey, wt, wkey, col, blk):
                for c in range(8):
                    mm(bank, wt[:, c, col:col + 128], uT[:, c, blk * 512:(blk + 1) * 512], c == 0, c == 7,
                       [wkey], [bkey])

            def proj_tm(bank_ap, bkey, wt_ap_fn, wkey, t):
                for c in range(8):
                    mm(bank_ap, uT[:, c, t * 128:(t + 1) * 128], wt_ap_fn(c), c == 0, c == 7, [wkey], [bkey])

            with ExitStack() as ph:
                qkvT = sbuf(ph, "qkvT", [128, 12, T], BF16)
                cwt = sbuf(ph, "cwt", [128, 12, 4], F32)
                dma(cwt[:], cw_d, writes=["cwt"])
                with ExitStack() as g1:
                    alloc_wstage(g1)
                    xc = [sbuf(g1, f"xc{i}", [128, 3 + T], BF16) for i in range(2)]
                    diag = [sbuf(g1, f"diag{i}", [128, 4, 128], BF16) for i in range(2)]
                    s16 = [sbuf(g1, f"s16_{i}", [128, 512], BF16) for i in range(2)]
                    sq16 = [sbuf(g1, f"sq16_{i}", [128, 512], BF16) for i in range(2)]
                    rn = [sbuf(g1, f"rn{i}", [128, 512], F32) for i in range(2)]
                    pb = [psum(g1, f"pbG{i}", [128, 512], F32) for i in range(8)]
                    for i in range(2):
                        memset(xc[i][:, 0:3], 0.0, [f"xc{i}"])
                    for ch in range(12):
                        i = ch % 2
                        if ch % 2 == 0:
                            wt, wkey = load_w(w_in[:, ch * 128:(ch + 2) * 128], 256)
                        col = (ch % 2) * 128
                        for blk in range(4):
                            proj_fm(pb[blk][:], f"pbG{blk}", wt, wkey, col, blk)
                            act(xc[i][:, 3 + blk * 512:3 + (blk + 1) * 512], pb[blk][:], AF.Copy, [f"pbG{blk}"], [f"xc{i}"])
                        for k in range(4):
                            ts(diag[i][:, k, :], identb[:], cwt[:, ch, k:k + 1], None, ALU.mult, None,
                               ["identb", "cwt"], [f"diag{i}"])
                        for blk in range(4):
                            b2 = 4 + (blk % 2)
                            j = blk % 2
                            for k in range(4):
                                mm(pb[b2][:], diag[i][:, k, :], xc[i][:, blk * 512 + k:blk * 512 + k + 512], k == 0, k == 3,
                                   [f"diag{i}", f"xc{i}"], [f"pbG{b2}"])
                            dst = qkvT[:, ch, blk * 512:(blk + 1) * 512]
                            act(dst, pb[b2][:], AF.Silu, [f"pbG{b2}"], [f"qkvT{ch}_{blk}"])
                    for ch in range(8):
                        for blk in range(4):
                            j = blk % 2
                            b3 = 6 + j
                            dst = qkvT[:, ch, blk * 512:(blk + 1) * 512]
                            tt(sq16[j][:], dst, dst, ALU.mult, [f"qkvT{ch}_{blk}"], [f"sq16_{j}"])
                            mm(pb[b3][:], onesb[:], sq16[j][:], True, True, ["onesb", f"sq16_{j}"], [f"pbG{b3}"])
                            act(rn[j][:], pb[b3][:], AF.Ln, [f"pbG{b3}", "epsb"], [f"rn{j}"], bias=epsb[:])
                            act(rn[j][:], rn[j][:], AF.Exp, [f"rn{j}"], [f"rn{j}"], scale=-0.5)
                            tt(dst, dst, rn[j][:], ALU.mult, [f"qkvT{ch}_{blk}", f"rn{j}"], [f"qkvT{ch}_{blk}"])
                    S.barrier()
                    S.emit()
                    if stop_after == "G1":
                        return nc
                gg = sbuf(ph, "gg", [128, NT, 4], F32)
                beta = sbuf(ph, "beta", [128, NT, 4], F32)
                wz = sbuf(ph, "wz", [128, 8, 512], BF16)
                gnw = sbuf(ph, "gnw", [128, 128], F32)
                with ExitStack() as g2:
                    alloc_wstage(g2)
                    ab = sbuf(g2, "ab", [128, NT, 8], F32)
                    tmp4 = sbuf(g2, "tmp4", [128, NT, 4], F32)
                    alog = sbuf(g2, "alog", [128, 4], F32)
                    dtb = sbuf(g2, "dtb", [128, 4], F32)
                    pab = psum(g2, "pab", [128, NT, 8], F32)
                    dma(alog[:], alog_d.partition_broadcast(128), writes=["alog"])
                    dma(dtb[:], dtb_d.partition_broadcast(128), writes=["dtb"])
                    dma(gnw[:], gnw_d.partition_broadcast(128), writes=["gnw"])
                    wt, wkey = load_w(w_in[:, 1536:1544], 8)
                    for t in range(NT):
                        proj_tm(pab[:, t, :], "pab", lambda c: wt[:, c, 0:8], wkey, t)
                    cp(ab[:], pab[:], ["pab"], ["ab"])
                    for j in range(2):
                        wt2, wkey2 = load_w(w_in[:, 1544 + j * 256:1544 + (j + 1) * 256], 256)
                        cp(wz[:, :, j * 256:(j + 1) * 256], wt2[:, :, 0:256], [wkey2], ["wz"])
                    tt(tmp4[:], ab[:, :, 0:4], dtb[:, :].unsqueeze(1).to_broadcast([128, NT, 4]), ALU.add, ["ab", "dtb"], ["tmp4"])
                    act(tmp4[:], tmp4[:], AF.Exp, ["tmp4"], ["tmp4"])
                    act(tmp4[:], tmp4[:], AF.Ln, ["tmp4"], ["tmp4"], bias=1.0)
                    act(alog[:], alog[:], AF.Exp, ["alog"], ["alog"])
                    stt(gg[:], tmp4[:], -1.0, alog[:, :].unsqueeze(1).to_broadcast([128, NT, 4]), ALU.mult, ALU.mult,
                        ["tmp4", "alog"], ["gg"])
                    act(beta[:], ab[:, :, 4:8], AF.Exp, ["ab"], ["beta"], scale=-1.0)
                    ts(beta[:], beta[:], 1.0, None, ALU.add, None, ["beta"], ["beta"])
                    recip(beta[:], beta[:], ["beta"], ["beta"])
                    S.barrier()
                    S.emit()
                    if stop_after == "G2":
                        return nc
                with ExitStack() as g3:
                    B0 = psum(g3, "BG0", [128, 512], F32)
                    BT = psum(g3, "BGT", [128, 8, 128], BF16)
                    H = [psum(g3, f"BH{h}", [128, 512], F32) for h in range(4)]
                    SC = [psum(g3, f"BSC{i}", [128, 512], F32) for i in range(2)]
                    gs = sbuf(g3, "gs", [128, 16], F32)
                    es = sbuf(g3, "es", [128, 16], F32)
                    bg = sbuf(g3, "bg", [128, 4], F32)
                    kbg = sbuf(g3, "kbg", [128, 4, 128], BF16)
                    kd = sbuf(g3, "kd", [128, 4, 128], BF16)
                    vb = sbuf(g3, "vb", [128, 4, 128], BF16)
                    Gg = [sbuf(g3, f"Gg{h}", [128, 128], F32) for h in range(4)]
                    Ed = [sbuf(g3, f"Ed{h}", [128, 2, 128], F32) for h in range(4)]
                    Dm = [sbuf(g3, f"Dm{h}", [128, 2, 128], F32) for h in range(4)]
                    LN = [[sbuf(g3, f"LN{h}_{i}", [128, 2, 128], F32) for i in range(2)] for h in range(4)]
                    Pm = [[sbuf(g3, f"Pm{h}_{i}", [128, 128], F32) for i in range(2)] for h in range(4)]
                    TTb = [sbuf(g3, f"TTb{h}", [128, 128], BF16) for h in range(4)]
                    dg = [sbuf(g3, f"dg{h}", [128, 128], BF16) for h in range(4)]
                    attnT = sbuf(g3, "attnT", [128, 4, 128], BF16)
                    qgT = sbuf(g3, "qgT", [128, 4, 128], BF16)
                    wT = sbuf(g3, "wT", [128, 4, 128], BF16)
                    uu = sbuf(g3, "uu", [128, 4, 128], F32)
                    vnew = sbuf(g3, "vnew", [128, 4, 128], BF16)
                    S32 = sbuf(g3, "S32", [128, 4, 128], F32)
                    S16 = sbuf(g3, "S16", [128, 4, 128], BF16)
                    oo = sbuf(g3, "oo", [128, 4, 128], F32)
                    osq = sbuf(g3, "osq", [128, 4, 128], F32)
                    oss = sbuf(g3, "oss", [128, 4], F32)
                    orst = sbuf(g3, "orst", [128, 4], F32)
                    sz = sbuf(g3, "sz", [128, 512], F32)
                    ya = sbuf(g3, "ya", [128, 512], BF16)
                    memset(S32[:], 0.0, [f"S32_{h}" for h in range(4)])
                    memset(S16[:], 0.0, [f"S16_{h}" for h in range(4)])
                    TL = cm[:, CM_TL, :]
                    SCL = float(128 ** -0.5)

                    def head_prep(n, h):
                        tsl = slice(n * 128, (n + 1) * 128)
                        Hh, hk = H[h], f"BH{h}"
                        kTh = qkvT[:, 4 + h, tsl]
                        qTh = qkvT[:, h, tsl]
                        ts(Gg[h][:], cm[:, CM_SU, :], gg[:, n, h:h + 1], None, ALU.mult, None, ["cm", "gg"], [f"Gg{h}"])
                        ts(dg[h][:], identb[:], es[:, h:h + 1], None, ALU.mult, None, ["identb", "es"], [f"dg{h}"])
                        yield
                        mm(Hh[:, 0:128], Gg[h][:], TL, True, True, [f"Gg{h}", "cm"], [hk])
                        mm(Hh[:, 128:256], TL, Gg[h][:], True, True, [f"Gg{h}", "cm"], [hk])
                        mm(Hh[:, 256:384], kTh, kTh, True, True, [], [hk])
                        mm(Hh[:, 384:512], kTh, qTh, True, True, [], [hk])
                        yield
                        act(Ed[h][:], Hh[:, 0:256].rearrange("p (a b) -> p a b", a=2), AF.Exp, [hk], [f"Ed{h}"])
                        yield
                        tt(Dm[h][:, 0, :], Ed[h][:, 0, :], cm[:, CM_MCT, :], ALU.mult, [f"Ed{h}", "cm"], [f"DmA{h}"], eng="pool")
                        tt(Dm[h][:, 1, :], Ed[h][:, 1, :], cm[:, CM_MLS, :], ALU.mult, [f"Ed{h}", "cm"], [f"DmB{h}"])
                        yield
                        stt(LN[h][0][:, 0, :], Hh[:, 256:384], beta[:, n, h:h + 1], Dm[h][:, 1, :], ALU.mult, ALU.mult,
                            [hk, "beta", f"DmB{h}"], [f"LN{h}_0"])
                        stt(attnT[:, h, :], Hh[:, 384:512], SCL, Dm[h][:, 0, :], ALU.mult, ALU.mult,
                            [hk, f"DmA{h}"], [f"attnT{h}"])
                        yield
                        mm(Hh[:, 0:128], LN[h][0][:, 0, :], ident, True, True, [f"LN{h}_0", "cm"], [hk])
                        yield
                        act(LN[h][0][:, 1, :], Hh[:, 0:128], AF.Copy, [hk], [f"LN{h}_0"])
                        yield
                        tt(Pm[h][0][:], ident, LN[h][0][:, 1, :], ALU.subtract, ["cm", f"LN{h}_0"], [f"Pm{h}_0"])
                        a, p = 0, 0
                        for lvl in range(1, 6):
                            na = 1 - a
                            Lc, Nc = LN[h][a][:, 0, :], LN[h][a][:, 1, :]
                            mm(Hh[:, 0:128], Nc, Lc, True, True, [f"LN{h}_{a}"], [hk])
                            if lvl < 5:
                                mm(Hh[:, 128:256], Lc, Nc, True, True, [f"LN{h}_{a}"], [hk])
                            yield
                            if lvl < 5:
                                act(LN[h][na][:], Hh[:, 0:256].rearrange("p (a b) -> p a b", a=2), AF.Copy, [hk], [f"LN{h}_{na}"])
                            else:
                                act(LN[h][na][:, 0, :], Hh[:, 0:128], AF.Copy, [hk], [f"LN{h}_{na}"])
                            yield
                            mm(Hh[:, 256:384], LN[h][na][:, 0, :], Pm[h][p][:], True, True, [f"LN{h}_{na}", f"Pm{h}_{p}"], [hk])
                            yield
                            if lvl < 5:
                                tt(Pm[h][1 - p][:], Hh[:, 256:384], Pm[h][p][:], ALU.add, [hk, f"Pm{h}_{p}"], [f"Pm{h}_{1 - p}"])
                            else:
                                tt(TTb[h][:], Hh[:, 256:384], Pm[h][p][:], ALU.add, [hk, f"Pm{h}_{p}"], [f"TTb{h}"])
                            yield
                            a, p = na, 1 - p
                        mm(Hh[:, 0:128], TTb[h][:], vb[:, h, :], True, True, [f"TTb{h}", "vb"], [hk])
                        mm(Hh[:, 128:256], kbg[:, h, :], TTb[h][:], True, True, [f"TTb{h}", "kbg"], [hk])
                        mm(Hh[:, 256:384], onesb[:], dg[h][:], True, True, ["onesb", f"dg{h}"], [hk])
                        yield
                        cp(uu[:, h, :], Hh[:, 0:128], [hk], [f"uu{h}"])
                        stt(qgT[:, h, :], Hh[:, 256:384], SCL, qTh, ALU.mult, ALU.mult, [hk], [f"qgT{h}"])
                        act(wT[:, h, :], Hh[:, 128:256], AF.Copy, [hk], [f"wT{h}"])
                        yield

                    def head_scan(n, h):
                        Hh, hk = H[h], f"BH{h}"
                        for half in range(2):
                            hs = slice(half * 64, (half + 1) * 64)
                            mm(Hh[:, 0:128], wT[:, h, :], S16[:, h, :], True, True, [f"wT{h}", f"S16_{h}"], [hk])
                            yield
                            tt(vnew[hs, h, :], uu[hs, h, :], Hh[hs, 0:128], ALU.subtract, [f"uu{h}", hk], [f"vnew{h}"])
                            yield
                            mm(Hh[:, 128:256], qgT[:, h, :], S16[:, h, :], True, False, [f"qgT{h}", f"S16_{h}"], [hk])
                            mm(Hh[:, 128:256], attnT[hs, h, :], vnew[hs, h, :], False, True, [f"attnT{h}", f"vnew{h}"], [hk])
                            mm(Hh[:, 256:384], kd[hs, h, :], vnew[hs, h, :], True, True, ["kd", f"vnew{h}"], [hk])
                            yield
                            stt(S32[:, h, :], S32[:, h, :], es[:, 8 + 4 * half + h:9 + 4 * half + h], Hh[:, 256:384],
                                ALU.mult, ALU.add, [hk, "es", f"S32_{h}"], [f"S32_{h}"])
                            act(oo[hs, h, :], Hh[hs, 128:256], AF.Copy, [hk], [f"oo{h}"])
                            yield
                            act(S16[:, h, :], S32[:, h, :], AF.Copy, [f"S32_{h}"], [f"S16_{h}"])
                            yield

                    def rr(gens):
                        gens = list(gens)
                        while gens:
                            for g in list(gens):
                                try:
                                    next(g)
                                except StopIteration:
                                    gens.remove(g)

                    G3N = int(os.environ.get("G3N", str(NT)))
                    for n in range(G3N):
                        tsl = slice(n * 128, (n + 1) * 128)
                        mm(B0[:, 0:4], TL, gg[:, n, :], True, True, ["cm", "gg"], ["BG0"])
                        mm(B0[:, 4:8], cm[:, CM_BO, :], gg[:, n, :], True, True, ["cm", "gg"], ["BG0"])
                        mm(B0[:, 8:12], cm[:, CM_SEL0, :], gg[:, n, :], True, True, ["cm", "gg"], ["BG0"])
                        mm(B0[:, 12:16], cm[:, CM_SEL1, :], gg[:, n, :], True, True, ["cm", "gg"], ["BG0"])
                        cp(gs[:], B0[:, 0:16], ["BG0"], ["gs"])
                        tt(gs[:, 4:8], gs[:, 4:8], gs[:, 0:4], ALU.subtract, ["gs"], ["gs"])
                        act(es[:], gs[:], AF.Exp, ["gs"], ["es"])
                        tt(bg[:], es[:, 0:4], beta[:, n, :], ALU.mult, ["es", "beta"], ["bg"])
                        for h in range(4):
                            tr(BT[:, h, :], qkvT[:, 4 + h, tsl], identb[:], ["identb"], ["BGT"])
                            tr(BT[:, 4 + h, :], qkvT[:, 8 + h, tsl], identb[:], ["identb"], ["BGT"])
                        tt(kbg[:], BT[:, 0:4, :], bg[:, :].unsqueeze(2).to_broadcast([128, 4, 128]), ALU.mult, ["BGT", "bg"], ["kbg"])
                        tt(kd[:], BT[:, 0:4, :], es[:, 4:8].unsqueeze(2).to_broadcast([128, 4, 128]), ALU.mult, ["BGT", "es"], ["kd"])
                        tt(vb[:], BT[:, 4:8, :], beta[:, n, :].unsqueeze(2).to_broadcast([128, 4, 128]), ALU.mult, ["BGT", "beta"], ["vb"])
                        rr(head_prep(n, h) for h in range(4))
                        rr(head_scan(n, h) for h in range(4))
                        ook = [f"oo{h}" for h in range(4)]
                        tt(osq[:], oo[:], oo[:], ALU.mult, ook, ["osq"])
                        red(oss[:], osq[:], ALU.add, ["osq"], ["oss"])
                        rsqrt_small(orst[:], oss[:], 1.0 / 128, ["oss"], ["orst"])
                        proj_tm(B0[:], "BG0", lambda c: wz[:, c, :], "wz", n)
                        act(sz[:], B0[:], AF.Silu, ["BG0"], ["sz"])
                        tt(osq[:], oo[:], orst[:, :].unsqueeze(2).to_broadcast([128, 4, 128]), ALU.mult, ook + ["orst"], ["osq"])
                        tt(osq[:], osq[:], gnw[:, :].unsqueeze(1).to_broadcast([128, 4, 128]), ALU.mult, ["osq", "gnw"], ["osq"])
                        tt(ya[:], osq[:].rearrange("p a b -> p (a b)"), sz[:], ALU.mult, ["osq", "sz"], ["ya"])
                        for h in range(4):
                            tr(BT[:, h, :], ya[:, h * 128:(h + 1) * 128], identb[:], ["ya", "identb"], ["BGT"])
                        cp(yT[:, 0:4, tsl], BT[:, 0:4, :], ["BGT"], [f"yT{n}"])
                    S.barrier()
                    S.emit()
                    if stop_after == "G3":
                        return nc

            with ExitStack() as ph:
                rqkT = sbuf(ph, "rqkT", [128, 8, T], BF16)
                retdt = sbuf(ph, "retdt", [128, 4, 128], F32)
                retvec = sbuf(ph, "retvec", [128, 8], F32)
                dma(retdt[:], retdt_d, writes=["retdt"])
                dma(retvec[:], retvec_d, writes=["retvec"])
                with ExitStack() as r1:
                    alloc_wstage(r1)
                    cosT = sbuf(r1, "cosT", [128, T], F32)
                    sinT = sbuf(r1, "sinT", [128, T], F32)
                    t1 = [sbuf(r1, f"t1_{i}", [128, 512], F32) for i in range(2)]
                    t2 = [sbuf(r1, f"t2_{i}", [128, 512], F32) for i in range(2)]
                    pb = [psum(r1, f"pbR{i}", [128, 512], F32) for i in range(8)]
                    dma(cosT[:], cos_d, writes=["cosT"])
                    dma(sinT[:], sin_d, writes=["sinT"])
                    for ch in range(8):
                        if ch % 2 == 0:
                            wt, wkey = load_w(w_in[:, 2056 + ch * 128:2056 + (ch + 2) * 128], 256)
                            wtp, wkeyp = load_w(w_perm[:, ch * 128:(ch + 2) * 128], 256)
                        col = (ch % 2) * 128
                        for blk in range(4):
                            j = blk % 2
                            b1, b2 = 2 * (blk % 4), 2 * (blk % 4) + 1
                            proj_fm(pb[b1][:], f"pbR{b1}", wt, wkey, col, blk)
                            proj_fm(pb[b2][:], f"pbR{b2}", wtp, wkeyp, col, blk)
                            bs = slice(blk * 512, (blk + 1) * 512)
                            tt(t1[j][:], pb[b1][:], cosT[:, bs], ALU.mult, [f"pbR{b1}", "cosT"], [f"t1_{j}"])
                            tt(t2[j][:], pb[b2][:], sinT[:, bs], ALU.mult, [f"pbR{b2}", "sinT"], [f"t2_{j}"])
                            tt(rqkT[:, ch, bs], t1[j][:], t2[j][:], ALU.add, [f"t1_{j}", f"t2_{j}"], [f"rqkT{ch}"], eng="pool")
                    S.barrier()
                    S.emit()
                    if stop_after == "R1":
                        return nc
                with ExitStack() as r2:
                    alloc_wstage(r2)
                    wv = sbuf(r2, "wv", [128, 8, 1024], BF16)
                    wgt = sbuf(r2, "wgt", [128, 8, 1024], BF16)
                    vtok = sbuf(r2, "vtok", [128, 1024], BF16)
                    szr = sbuf(r2, "szr", [128, 1024], F32)
                    kdk = sbuf(r2, "kdk", [128, 4, 128], BF16)
                    sc16 = [sbuf(r2, f"sc16_{h}", [128, 128], BF16) for h in range(4)]
                    R32 = sbuf(r2, "R32", [128, 4, 256], F32)
                    R16 = sbuf(r2, "R16", [128, 4, 256], BF16)
                    otmp = [sbuf(r2, f"otmp{h}", [128, 256], F32) for h in range(4)]
                    ro = sbuf(r2, "ro", [128, 4, 256], F32)
                    rsq = sbuf(r2, "rsq", [128, 4, 256], F32)
                    rss = sbuf(r2, "rss", [128, 4], F32)
                    rrst = sbuf(r2, "rrst", [128, 4], F32)
                    yb = sbuf(r2, "yb", [128, 1024], BF16)
                    Bv = [psum(r2, f"BRv{i}", [128, 512], F32) for i in range(2)]
                    H = [psum(r2, f"BRH{h}", [128, 512], F32) for h in range(4)]
                    BT = psum(r2, "BRT", [128, 8, 128], BF16)
                    for j in range(4):
                        wt2, wkey2 = load_w(w_in[:, 3080 + j * 256:3080 + (j + 1) * 256], 256)
                        cp(wv[:, :, j * 256:(j + 1) * 256], wt2[:, :, 0:256], [wkey2], ["wv"])
                    for j in range(4):
                        wt2, wkey2 = load_w(w_in[:, 4104 + j * 256:4104 + (j + 1) * 256], 256)
                        cp(wgt[:, :, j * 256:(j + 1) * 256], wt2[:, :, 0:256], [wkey2], ["wgt"])
                    memset(R32[:], 0.0, [f"R32_{h}" for h in range(4)])
                    memset(R16[:], 0.0, [f"R16_{h}" for h in range(4)])

                    def ret_head(n, h):
                        tsl = slice(n * 128, (n + 1) * 128)
                        Hh, hk = H[h], f"BRH{h}"
                        qTh = rqkT[:, h, tsl]
                        kTh = rqkT[:, 4 + h, tsl]
                        vh = vtok[:, h * 256:(h + 1) * 256]
                        mm(Hh[:, 0:128], kTh, qTh, True, True, [], [hk])
                        mm(Hh[:, 256:512], kdk[:, h, :], vh, True, True, ["kdk", "vtok"], [hk])
                        yield
                        tt(sc16[h][:], Hh[:, 0:128], retdt[:, h, :], ALU.mult, [hk, "retdt"], [f"sc16_{h}"])
                        stt(R32[:, h, :], R32[:, h, :], float(cdec[h]), Hh[:, 256:512], ALU.mult, ALU.add,
                            [hk, f"R32_{h}"], [f"R32_{h}"])
                        yield
                        mm(Hh[:, 0:256], sc16[h][:], vh, True, True, [f"sc16_{h}", "vtok"], [hk])
                        mm(Hh[:, 256:512], qTh, R16[:, h, :], True, True, [f"R16_{h}"], [hk])
                        yield
                        act(otmp[h][:], Hh[:, 0:256], AF.Copy, [hk], [f"otmp{h}"])
                        yield
                        stt(ro[:, h, :], Hh[:, 256:512], retvec[:, 4 + h:5 + h], otmp[h][:], ALU.mult, ALU.add,
                            [hk, f"otmp{h}", "retvec"], [f"ro{h}"])
                        act(R16[:, h, :], R32[:, h, :], AF.Copy, [f"R32_{h}"], [f"R16_{h}"])
                        yield

                    def rr2(gens):
                        gens = list(gens)
                        while gens:
                            for g in list(gens):
                                try:
                                    next(g)
                                except StopIteration:
                                    gens.remove(g)

                    for n in range(NT):
                        tsl = slice(n * 128, (n + 1) * 128)
                        for j in range(2):
                            proj_tm(Bv[j][:], f"BRv{j}", lambda c, j=j: wv[:, c, j * 512:(j + 1) * 512], "wv", n)
                            act(vtok[:, j * 512:(j + 1) * 512], Bv[j][:], AF.Copy, [f"BRv{j}"], ["vtok"])
                        for h in range(4):
                            tr(BT[:, h, :], rqkT[:, 4 + h, tsl], identb[:], ["identb"], ["BRT"])
                        tt(kdk[:], BT[:, 0:4, :], retvec[:, 0:4].unsqueeze(2).to_broadcast([128, 4, 128]), ALU.mult,
                           ["BRT", "retvec"], ["kdk"])
                        rr2(ret_head(n, h) for h in range(4))
                        for j in range(2):
                            proj_tm(Bv[j][:], f"BRv{j}", lambda c, j=j: wgt[:, c, j * 512:(j + 1) * 512], "wgt", n)
                            act(szr[:, j * 512:(j + 1) * 512], Bv[j][:], AF.Silu, [f"BRv{j}"], ["szr"])
                        rok = [f"ro{h}" for h in range(4)]
                        tt(rsq[:], ro[:], ro[:], ALU.mult, rok, ["rsq"])
                        red(rss[:], rsq[:], ALU.add, ["rsq"], ["rss"])
                        rsqrt_small(rrst[:], rss[:], 1.0 / 256, ["rss"], ["rrst"])
                        tt(rsq[:], ro[:], rrst[:, :].unsqueeze(2).to_broadcast([128, 4, 256]), ALU.mult, rok + ["rrst"], ["rsq"])
                        tt(yb[:], rsq[:].rearrange("p a b -> p (a b)"), szr[:], ALU.mult, ["rsq", "szr"], ["yb"])
                        for c in range(8):
                            tr(BT[:, c, :], yb[:, c * 128:(c + 1) * 128], identb[:], ["yb", "identb"], ["BRT"])
                        cp(yT[:, 4:12, tsl], BT[:], ["BRT"], [f"yT{n}"])
                    S.barrier()
                    S.emit()
                    if stop_after == "R2":
                        return nc
            if debug:
                for c in range(12):
                    dma(dbg["yT"][c * 128:(c + 1) * 128, :], yT[:, c, :], writes=["dbgyT"])
                for c in range(8):
                    dma(dbg["uT"][c * 128:(c + 1) * 128, :], uT[:, c, :], writes=["dbguT"])

            with ExitStack() as ph:
                mT = sbuf(ph, "mT", [128, 8, T], BF16)
                B = [psum(ph, f"BM{i}", [128, 512], F32) for i in range(8)]
                with ExitStack() as ms1:
                    wsm = [sbuf(ms1, f"wsm{i}", [128, 8, 128], F32) for i in range(4)]
                    wsb = [[sbuf(ms1, f"wsb{s_}_{i}", [128, 8, 128], BF16) for i in range(4)] for s_ in range(2)]
                    tA = [sbuf(ms1, f"tA{i}", [128, 512], F32) for i in range(2)]
                    m1 = [sbuf(ms1, f"m1_{i}", [128, 512], F32) for i in range(2)]
                    for ec in range(8):
                        es_ = slice(ec * 128, (ec + 1) * 128)
                        s_ = ec % 2
                        dma(wsb[s_][0][:, 0:4, :], wupa_d[:, es_].rearrange("(c p) n -> p c n", p=128), writes=[f"wsb{s_}_0"], q="pool")
                        dma(wsb[s_][1][:], wupr_d[:, es_].rearrange("(c p) n -> p c n", p=128), writes=[f"wsb{s_}_1"], q="pool")
                        dma(wsb[s_][2][:], w_in[:, 5128 + ec * 128:5128 + (ec + 1) * 128].rearrange("(c p) n -> p c n", p=128), writes=[f"wsb{s_}_2"], q="pool")
                        dma(wsb[s_][3][:], w_in[:, 6152 + ec * 128:6152 + (ec + 1) * 128].rearrange("(c p) n -> p c n", p=128), writes=[f"wsb{s_}_3"], q="pool")
                        for blk in range(4):
                            bs = slice(blk * 512, (blk + 1) * 512)
                            j = blk % 2
                            bA, bB, bMA, bMB = 4 * j, 4 * j + 1, 4 * j + 2, 4 * j + 3
                            for c in range(4):
                                mm(B[bA][:], wsb[s_][0][:, c, :], yT[:, c, bs], c == 0, c == 3, [f"wsb{s_}_0"], [f"BM{bA}"])
                            for c in range(8):
                                mm(B[bB][:], wsb[s_][1][:, c, :], yT[:, 4 + c, bs], c == 0, c == 7, [f"wsb{s_}_1"], [f"BM{bB}"])
                            for c in range(8):
                                mm(B[bMA][:], wsb[s_][2][:, c, :], uT[:, c, bs], c == 0, c == 7, [f"wsb{s_}_2"], [f"BM{bMA}"])
                            for c in range(8):
                                mm(B[bMB][:], wsb[s_][3][:, c, :], uT[:, c, bs], c == 0, c == 7, [f"wsb{s_}_3"], [f"BM{bMB}"])
                            act(tA[j][:], B[bMA][:], AF.Tanh, [f"BM{bMA}"], [f"tA{j}"], scale=0.5)
                            stt(m1[j][:], tA[j][:], 1.0, B[bA][:], ALU.add, ALU.mult, [f"tA{j}", f"BM{bA}"], [f"m1_{j}"])
                            act(tA[j][:], B[bMB][:], AF.Tanh, [f"BM{bMB}"], [f"tA{j}"], scale=0.5)
                            stt(tA[j][:], tA[j][:], 1.0, B[bB][:], ALU.add, ALU.mult, [f"tA{j}", f"BM{bB}"], [f"tA{j}"])
                            tt(mT[:, ec, bs], m1[j][:], tA[j][:], ALU.add, [f"m1_{j}", f"tA{j}"], [f"mT{blk}"], eng="pool")
                    S.barrier()
                    S.emit()
                with ExitStack() as ms2:
                    alloc_wstage(ms2)
                    wo = sbuf(ms2, "wo", [128, 8, 1024], BF16)
                    xr = [sbuf(ms2, f"xr{i}", [128, D], F32) for i in range(2)]
                    ho = [sbuf(ms2, f"ho{i}", [128, D], F32) for i in range(2)]
                    dma(wo[:], wout_d.rearrange("(c p) n -> p c n", p=128), writes=["wo"], q="pool")
                    for t in range(NT):
                        i = t % 2
                        dma(xr[i][:], x[t * 128:(t + 1) * 128, :], writes=[f"xr{i}"])
                        for hf in range(2):
                            bk = (2 * t + hf) % 8
                            for c in range(8):
                                mm(B[bk][:], mT[:, c, t * 128:(t + 1) * 128], wo[:, c, hf * 512:(hf + 1) * 512], c == 0, c == 7,
                                   ["wo"], [f"BM{bk}"])
                            stt(ho[i][:, hf * 512:(hf + 1) * 512], B[bk][:], 0.5, xr[i][:, hf * 512:(hf + 1) * 512], ALU.mult, ALU.add,
                                [f"BM{bk}", f"xr{i}"], [f"ho{i}"])
                        dma(h1s[t * 128:(t + 1) * 128, :], ho[i][:], reads=[f"ho{i}"], writes=["h1s"])
                    S.barrier()
                    S.emit()
                    if stop_after == "M":
                        return nc

        NTILE = 48
        NSLOT = NTILE * 256
        I32 = mybir.dt.int32
        XS = nc.dram_tensor("xs_scr", [NSLOT, D], BF16).ap()
        WS = nc.dram_tensor("ws_scr", [NSLOT, 1], F32).ap()
        YS = nc.dram_tensor("ys_scr", [NSLOT, D], F32).ap()
        TE = nc.dram_tensor("dbg_te" if debug else "te_scr", [128, NTILE], I32, kind="ExternalOutput" if debug else "Internal").ap()
        with ExitStack() as ph:
            hacc = sbuf(ph, "hacc", [128, NT, D], F32)
            s1i = sbuf(ph, "s1i", [128, NT], I32)
            s2i = sbuf(ph, "s2i", [128, NT], I32)
            w1v = sbuf(ph, "w1v", [128, NT], F32)
            w2v = sbuf(ph, "w2v", [128, NT], F32)
            gidx = sbuf(ph, "gidx", [128, NTILE, 8], I32)
            didx = sbuf(ph, "didx", [128, NTILE, 4], I32)
            for t in range(NT):
                dma(hacc[:, t, :], h1s[t * 128:(t + 1) * 128, :], reads=["h1s"], writes=[f"hacc{t}"])
            with ExitStack() as e1:
                xntok = sbuf(e1, "xntok", [128, NT, D], BF16)
                nfrow = sbuf(e1, "nfrow", [128, D], F32)
                misc = sbuf(e1, "misc", [128, 96], F32)
                hs_ = [sbuf(e1, f"hs{i}", [128, D], F32) for i in range(2)]
                sq = sbuf(e1, "sqE", [128, D], F32)
                ssE = sbuf(e1, "ssE", [128, NT], F32)
                rstE = sbuf(e1, "rstE", [128, NT], F32)
                xn32 = [sbuf(e1, f"xn32_{i}", [128, 8, 128], F32) for i in range(2)]
                wr = sbuf(e1, "wr", [128, 8, 36], F32)
                brt = sbuf(e1, "brt", [128, 36], F32)
                lg = sbuf(e1, "lg", [128, NT, 36], F32)
                PT = [psum(e1, f"PTE{i}", [128, 8, 128], F32) for i in range(2)]
                PL = psum(e1, "PLE", [128, 512], F32)
                PS1 = psum(e1, "PTE_rank", [128, 512], F32)
                PS2 = psum(e1, "PTE_cnt", [128, 512], F32)
                dma(wr[:], wr_d.rearrange("(c p) n -> p c n", p=128), writes=["wr"])
                dma(brt[:], br_d.partition_broadcast(128), writes=["brt"])
                dma(nfrow[:], nffnrow_d.partition_broadcast(128), writes=["nfrow"])
                dma(misc[:], misc_d, writes=["misc"])
                memset(ssE[:], 0.0, ["ssE"])
                for t in range(NT):
                    i = t % 2
                    act(sq[:], hacc[:, t, :], AF.Square, ["ssE", f"hacc{t}"], ["sqE", "ssE"], accum_out=ssE[:, t:t + 1])
                    rsqrt_small(rstE[:, t:t + 1], ssE[:, t:t + 1], 1.0 / D, ["ssE"], [f"rstE{t}"])
                    ts(hs_[i][:], hacc[:, t, :], rstE[:, t:t + 1], None, ALU.mult, None, [f"rstE{t}", f"hacc{t}"], [f"hs{i}"])
                    tt(xntok[:, t, :], hs_[i][:], nfrow[:], ALU.mult, [f"hs{i}", "nfrow"], [f"xntok{t}"], eng="pool")
                    for c in range(8):
                        mm(PT[i][:, c, :], hs_[i][:, c * 128:(c + 1) * 128], ident, True, True, [f"hs{i}", "cm"], [f"PTE{i}"])
                    tt(xn32[i][:], PT[i][:], nffn[:, :].unsqueeze(2).to_broadcast([128, 8, 128]), ALU.mult,
                       [f"PTE{i}", "nffn"], [f"xn32_{i}"])
                    for c in range(8):
                        mm(PL[:, 0:36], xn32[i][:, c, :], wr[:, c, :], c == 0, c == 7, [f"xn32_{i}", "wr"], ["PLE"])
                    tt(lg[:, t, :], PL[:, 0:36], brt[:], ALU.add, ["PLE", "brt"], ["lg"])
                gmax = sbuf(e1, "gmax", [128, NT], F32)
                ohg = sbuf(e1, "ohg", [128, NT, 4], F32)
                sh4 = sbuf(e1, "sh4", [128, NT, 4], F32)
                gw = sbuf(e1, "gw", [128, NT], F32)
                M32 = sbuf(e1, "M32", [128, NT, 32], F32)
                oh1 = sbuf(e1, "oh1", [128, NT, 32], F32)
                oh2 = sbuf(e1, "oh2", [128, NT, 32], F32)
                m1v = sbuf(e1, "m1v", [128, NT], F32)
                m2v = sbuf(e1, "m2v", [128, NT], F32)
                L4 = lg[:, :, 0:4]
                L32 = lg[:, :, 4:36]
                bc4 = lambda a: a.unsqueeze(2).to_broadcast([128, NT, 4])
                bc32 = lambda a: a.unsqueeze(2).to_broadcast([128, NT, 32])
                red(gmax[:], L4, ALU.max, ["lg"], ["gmax"])
                tt(ohg[:], L4, bc4(gmax[:, :]), ALU.is_equal, ["lg", "gmax"], ["ohg"])
                tt(sh4[:], L4, bc4(gmax[:, :]), ALU.subtract, ["lg", "gmax"], ["sh4"])
                act(sh4[:], sh4[:], AF.Exp, ["sh4"], ["sh4"])
                red(gw[:], sh4[:], ALU.add, ["sh4"], ["gw"])
                recip(gw[:], gw[:], ["gw"], ["gw"])
                ts(ohg[:], ohg[:], BIG, -BIG, ALU.mult, ALU.add, ["ohg"], ["ohg"])
                tt(M32[:].rearrange("p t (g e) -> p t g e", g=4), L32.rearrange("p t (g e) -> p t g e", g=4),
                   ohg[:, :, :].unsqueeze(3).to_broadcast([128, NT, 4, 8]), ALU.add, ["lg", "ohg"], ["M32"])
                red(m1v[:], M32[:], ALU.max, ["M32"], ["m1v"])
                tt(oh1[:], M32[:], bc32(m1v[:, :]), ALU.is_equal, ["M32", "m1v"], ["oh1"])
                stt(M32[:], oh1[:], -BIG, M32[:], ALU.mult, ALU.add, ["oh1", "M32"], ["M32"])
                red(m2v[:], M32[:], ALU.max, ["M32"], ["m2v"])
                tt(oh2[:], M32[:], bc32(m2v[:, :]), ALU.is_equal, ["M32", "m2v"], ["oh2"])
                tt(w2v[:], m2v[:], m1v[:], ALU.subtract, ["m1v", "m2v"], ["w2v"])
                act(w2v[:], w2v[:], AF.Exp, ["w2v"], ["w2v"])
                ts(w1v[:], w2v[:], 1.0, None, ALU.add, None, ["w2v"], ["w1v"])
                recip(w1v[:], w1v[:], ["w1v"], ["w1v"])
                tt(w2v[:], w2v[:], w1v[:], ALU.mult, ["w2v", "w1v"], ["w2v"])
                tt(w1v[:], w1v[:], gw[:], ALU.mult, ["w1v", "gw"], ["w1v"])
                tt(w2v[:], w2v[:], gw[:], ALU.mult, ["w2v", "gw"], ["w2v"])
                sel = sbuf(e1, "sel", [128, NT, 32], F32)
                tcs = sbuf(e1, "tcs", [128, NT, 32], F32)
                off = sbuf(e1, "off", [128, NT, 32], F32)
                slot = sbuf(e1, "slot", [128, NT, 32], F32)
                cnt = sbuf(e1, "cnt", [128, 32], F32)
                cmp8 = sbuf(e1, "cmp8", [128, 32, 8], F32)
                pfa = sbuf(e1, "pfa", [128, 32], F32)
                pfb = sbuf(e1, "pfb", [128, 32], F32)
                ntl = sbuf(e1, "ntl", [128, 32], F32)
                stt_ = sbuf(e1, "stt_", [128, 32], F32)
                s1f = sbuf(e1, "s1f", [128, NT], F32)
                s2f = sbuf(e1, "s2f", [128, NT], F32)
                A1 = sbuf(e1, "A1", [128, NTILE, 32], F32)
                A2 = sbuf(e1, "A2", [128, NTILE, 32], F32)
                tef = sbuf(e1, "tef", [128, NTILE], F32)
                tei = sbuf(e1, "tei", [128, NTILE], I32)
                thr8 = misc[:, 0:8]
                kk48 = misc[:, 8:56]
                eio = misc[:, 56:88]
                flat = lambda a: a.rearrange("p t e -> p (t e)")
                tt(sel[:], oh1[:], oh2[:], ALU.add, ["oh1", "oh2"], ["sel"])
                mm(PS1[:], cm[:, CM_UT, :], flat(sel[:]), True, True, ["cm", "sel"], ["PTE_rank"])
                mm(PS2[:], cm[:, CM_ONES, :], flat(sel[:]), True, True, ["cm", "sel"], ["PTE_cnt"])
                cp(flat(tcs[:]), PS2[:], ["PTE_cnt"], ["tcs"])
                memset(off[:, 0, :], 0.0, ["off"])
                for t in range(1, NT):
                    tt(off[:, t, :], off[:, t - 1, :], tcs[:, t - 1, :], ALU.add, ["off", "tcs"], ["off"])
                tt(cnt[:], off[:, NT - 1, :], tcs[:, NT - 1, :], ALU.add, ["off", "tcs"], ["cnt"])
                tt(cmp8[:], cnt[:, :].unsqueeze(2).to_broadcast([128, 32, 8]), thr8.unsqueeze(1).to_broadcast([128, 32, 8]),
                   ALU.is_gt, ["cnt", "misc"], ["cmp8"])
                red(ntl[:], cmp8[:], ALU.add, ["cmp8"], ["ntl"])
                cp(pfa[:], ntl[:], ["ntl"], ["pfa"])
                cur, nxt, ck, nk = pfa, pfb, "pfa", "pfb"
                for dd in (1, 2, 4, 8, 16):
                    cp(nxt[:], cur[:], [ck], [nk])
                    tt(nxt[:, dd:32], cur[:, dd:32], cur[:, 0:32 - dd], ALU.add, [ck, nk], [nk])
                    cur, nxt, ck, nk = nxt, cur, nk, ck
                incl, ik = cur, ck
                tt(stt_[:], incl[:], ntl[:], ALU.subtract, [ik, "ntl"], ["stt_"])
                ts(cnt[:], stt_[:], 256.0, None, ALU.mult, None, ["stt_"], ["cnt"])
                tt(off[:], off[:], cnt[:, :].unsqueeze(1).to_broadcast([128, NT, 32]), ALU.add, ["off", "cnt"], ["off"])
                tt(flat(slot[:]), PS1[:], flat(off[:]), ALU.add, ["PTE_rank", "off"], ["slot"])
                tt(sel[:], oh1[:], slot[:], ALU.mult, ["oh1", "slot"], ["sel"])
                red(s1f[:], sel[:], ALU.add, ["sel"], ["s1f"])
                tt(sel[:], oh2[:], slot[:], ALU.mult, ["oh2", "slot"], ["sel"])
                red(s2f[:], sel[:], ALU.add, ["sel"], ["s2f"])
                cp(s1i[:], s1f[:], ["s1f"], ["s1i"])
                cp(s2i[:], s2f[:], ["s2f"], ["s2i"])
                kkb = kk48.unsqueeze(2).to_broadcast([128, NTILE, 32])
                tt(A1[:], kkb, stt_[:, :].unsqueeze(1).to_broadcast([128, NTILE, 32]), ALU.is_ge, ["misc", "stt_"], ["A1"])
                tt(A2[:], kkb, incl[:, :].unsqueeze(1).to_broadcast([128, NTILE, 32]), ALU.is_lt, ["misc", ik], ["A2"])
                tt(A1[:], A1[:], A2[:], ALU.mult, ["A1", "A2"], ["A1"])
                valf = sbuf(e1, "valf", [128, NTILE], F32)
                bgf = sbuf(e1, "bgf", [128, NTILE], F32)
                bdf = sbuf(e1, "bdf", [128, NTILE], F32)
                gidxf = sbuf(e1, "gidxf", [128, NTILE, 8], F32)
                didxf = sbuf(e1, "didxf", [128, NTILE, 4], F32)
                pc8 = misc[:, 88:96]
                red(valf[:], A1[:], ALU.add, ["A1"], ["valf"])
                tt(A1[:], A1[:], eio.unsqueeze(1).to_broadcast([128, NTILE, 32]), ALU.mult, ["A1", "misc"], ["A1"])
                red(tef[:], A1[:], ALU.add, ["A1"], ["tef"])
                cp(tei[:], tef[:], ["tef"], ["tei"])
                dma(TE, tei[:], reads=["tei"], writes=["TE"])
                ts(valf[:], valf[:], 0.0, None, ALU.mult, None, ["valf"], ["valf"])
                stt(bgf[:], tef[:], 1024.0, valf[:], ALU.mult, ALU.add, ["tef", "valf"], ["bgf"])
                stt(bdf[:], tef[:], 512.0, valf[:], ALU.mult, ALU.add, ["tef", "valf"], ["bdf"])
                tt(gidxf[:], bgf[:, :].unsqueeze(2).to_broadcast([128, NTILE, 8]), pc8.unsqueeze(1).to_broadcast([128, NTILE, 8]),
                   ALU.add, ["bgf", "misc"], ["gidxf"])
                tt(didxf[:], bdf[:, :].unsqueeze(2).to_broadcast([128, NTILE, 4]), pc8[:, 0:4].unsqueeze(1).to_broadcast([128, NTILE, 4]),
                   ALU.add, ["bdf", "misc"], ["didxf"])
                cp(gidx[:], gidxf[:], ["gidxf"], ["gidx"])
                cp(didx[:], didxf[:], ["didxf"], ["didx"])
                for t in range(NT):
                    for (si, wv_, nm) in ((s1i, w1v, "a"), (s2i, w2v, "b")):
                        S.add("pool", lambda e, t=t, si=si: e.indirect_dma_start(
                            out=XS[:, :], out_offset=bass.IndirectOffsetOnAxis(ap=si[:, t:t + 1], axis=0),
                            in_=xntok[:, t, :], in_offset=None),
                            reads=[f"xntok{t}", "s1i", "s2i"], writes=["XS"], dma=True)
                if debug:
                    dbg["s12"] = nc.dram_tensor("dbg_s12", [128, 2 * NT], I32, kind="ExternalOutput").ap()
                    dma(dbg["s12"][:, 0:NT], s1i[:], reads=["s1i"], writes=["dbgs12"])
                    dma(dbg["s12"][:, NT:2 * NT], s2i[:], reads=["s2i"], writes=["dbgs12"])
                S.barrier()
                S.emit()
                if stop_after == "router":
                    return nc
            with ExitStack() as e2:
                NB = 3
                ewg = [sbuf(e2, f"ewg{i}", [128, 8, 512], BF16) for i in range(NB)]
                ewu = [sbuf(e2, f"ewu{i}", [128, 8, 512], BF16) for i in range(NB)]
                ewd = [sbuf(e2, f"ewd{i}", [128, 4, 1024], BF16) for i in range(NB)]
                xst = [sbuf(e2, f"xst{i}", [128, 2, D], BF16) for i in range(2)]
                wst_ = [sbuf(e2, f"wsl{i}", [128, 2], F32) for i in range(2)]
                xT = [sbuf(e2, f"xT{i}", [128, 8, 256], BF16) for i in range(2)]
                hidT = [sbuf(e2, f"hidT{i}", [128, 4, 256], BF16) for i in range(2)]
                sg = [sbuf(e2, f"sg{i}", [128, 256], BF16) for i in range(2)]
                yt = [sbuf(e2, f"yt{i}", [128, 2, D], F32) for i in range(2)]
                PTk = [psum(e2, f"BEt{i}", [128, 8, 128], BF16) for i in range(2)]
                Bgu = [psum(e2, f"BEg{i}", [128, 512], F32) for i in range(4)]
                Bd = [psum(e2, f"BEd{i}", [128, 512], F32) for i in range(2)]
                wg_flat = wg_d.rearrange("e d n -> (e d) n")
                wu_flat = wu_d.rearrange("e d n -> (e d) n")
                wd_flat = wd_d.rearrange("e f n -> (e f) n")

                def gather_w(dst, src_flat, idx_ap, bound, key):
                    S.add("pool", lambda e: e.indirect_dma_start(
                        out=dst, out_offset=None, in_=src_flat[:, :],
                        in_offset=bass.IndirectOffsetOnAxis(ap=idx_ap, axis=0)),
                        reads=["gidx", "didx"], writes=[key], dma=True)

                dcount = [0]
                for k in range(NTILE):
                    b3 = k % NB
                    b2 = k % 2
                    rs = slice(k * 256, (k + 1) * 256)
                    for c in range(8):
                        gather_w(ewg[b3][:, c, :], wg_flat, gidx[:, k, c:c + 1], 32 * 1024 - 1, f"ewg{b3}_{c}")
                    for c in range(8):
                        gather_w(ewu[b3][:, c, :], wu_flat, gidx[:, k, c:c + 1], 32 * 1024 - 1, f"ewu{b3}_{c}")
                    for c in range(4):
                        gather_w(ewd[b3][:, c, :], wd_flat, didx[:, k, c:c + 1], 32 * 512 - 1, f"ewd{b3}_{c}")
                    dma(xst[b2][:], XS[rs, :].rearrange("(h p) d -> p h d", p=128), reads=["XS"], writes=[f"xst{b2}"])
                    for hh in range(2):
                        for c in range(8):
                            tr(PTk[hh][:, c, :], xst[b2][:, hh, c * 128:(c + 1) * 128], identb[:], [f"xst{b2}", "identb"], [f"BEt{hh}"])
                        if hh == 0:
                            cp(xT[b2][:, :, 0:128], PTk[hh][:], [f"BEt{hh}"], [f"xT{b2}"])
                        else:
                            act(xT[b2][:, :, 128:256], PTk[hh][:], AF.Copy, [f"BEt{hh}"], [f"xT{b2}"])
                    for f in range(4):
                        j = f % 2
                        bg_, bu_ = Bgu[2 * j], Bgu[2 * j + 1]
                        for c in range(8):
                            mm(bg_[:, 0:256], ewg[b3][:, c, f * 128:(f + 1) * 128], xT[b2][:, c, :], c == 0, c == 7,
                               [f"ewg{b3}_{c}", f"xT{b2}"], [f"BEg{2 * j}"])
                        for c in range(8):
                            mm(bu_[:, 0:256], ewu[b3][:, c, f * 128:(f + 1) * 128], xT[b2][:, c, :], c == 0, c == 7,
                               [f"ewu{b3}_{c}", f"xT{b2}"], [f"BEg{2 * j + 1}"])
                        act(sg[j][:], bg_[:, 0:256], AF.Silu, [f"BEg{2 * j}"], [f"sg{j}"])
                        tt(hidT[b2][:, f, :], bu_[:, 0:256], sg[j][:], ALU.mult, [f"BEg{2 * j + 1}", f"sg{j}"], [f"hidT{b2}"])
                    for hh in range(2):
                        for cc in range(2):
                            bk = dcount[0] % 2
                            dcount[0] += 1
                            for f in range(4):
                                mm(Bd[bk][:], hidT[b2][:, f, hh * 128:(hh + 1) * 128], ewd[b3][:, f, cc * 512:(cc + 1) * 512],
                                   f == 0, f == 3, [f"ewd{b3}_{f}", f"hidT{b2}"], [f"BEd{bk}"])
                            if bk == 0:
                                cp(yt[b2][:, hh, cc * 512:(cc + 1) * 512], Bd[bk][:], [f"BEd{bk}"], [f"yt{b2}"])
                            else:
                                act(yt[b2][:, hh, cc * 512:(cc + 1) * 512], Bd[bk][:], AF.Copy, [f"BEd{bk}"], [f"yt{b2}"])
                    dma(YS[rs, :].rearrange("(h p) d -> p h d", p=128), yt[b2][:], reads=[f"yt{b2}"], writes=["YS"])
                S.barrier()
                S.emit()
                if stop_after == "experts":
                    return nc
            with ExitStack() as e3:
                nfw = sbuf(e3, "nfw", [128, D], F32)
                sq = sbuf(e3, "sqF", [128, D], F32)
                ssF = sbuf(e3, "ssF", [128, NT], F32)
                rstF = sbuf(e3, "rstF", [128, NT], F32)
                y1 = [sbuf(e3, f"y1_{i}", [128, D], F32) for i in range(2)]
                y2 = [sbuf(e3, f"y2_{i}", [128, D], F32) for i in range(2)]
                ob = [sbuf(e3, f"ob{i}", [128, D], F32) for i in range(2)]
                dma(nfw[:], nfin_d.partition_broadcast(128), writes=["nfw"])
                memset(ssF[:], 0.0, ["ssF"])
                for t in range(NT):
                    i = t % 2
                    S.add("pool", lambda e, t=t, i=i: e.indirect_dma_start(
                        out=y1[i][:, :], out_offset=None, in_=YS[:, :],
                        in_offset=bass.IndirectOffsetOnAxis(ap=s1i[:, t:t + 1], axis=0)),
                        reads=["YS", "s1i"], writes=[f"y1_{i}"], dma=True)
                    S.add("pool", lambda e, t=t, i=i: e.indirect_dma_start(
                        out=y2[i][:, :], out_offset=None, in_=YS[:, :],
                        in_offset=bass.IndirectOffsetOnAxis(ap=s2i[:, t:t + 1], axis=0)),
                        reads=["YS", "s2i"], writes=[f"y2_{i}"], dma=True)
                    stt(hacc[:, t, :], y1[i][:], w1v[:, t:t + 1], hacc[:, t, :], ALU.mult, ALU.add, [f"y1_{i}", f"hacc{t}", "w1v"], [f"hacc{t}"])
                    stt(hacc[:, t, :], y2[i][:], w2v[:, t:t + 1], hacc[:, t, :], ALU.mult, ALU.add, [f"y2_{i}", f"hacc{t}", "w2v"], [f"hacc{t}"])
                    act(sq[:], hacc[:, t, :], AF.Square, ["ssF", f"hacc{t}"], ["sqF", "ssF"], accum_out=ssF[:, t:t + 1])
                    rsqrt_small(rstF[:, t:t + 1], ssF[:, t:t + 1], 1.0 / D, ["ssF"], [f"rstF{t}"])
                    stt(ob[i][:], hacc[:, t, :], rstF[:, t:t + 1], nfw[:], ALU.mult, ALU.mult, [f"rstF{t}", "nfw", f"hacc{t}"], [f"ob{i}"])
                    dma(out[t * 128:(t + 1) * 128, :], ob[i][:], reads=[f"ob{i}"], writes=["out"])
                S.barrier()
                S.emit()
    return nc


_CACHE = {}


def _host_inputs(inputs):
    f = lambda a: np.ascontiguousarray(np.asarray(a, dtype=np.float32))
    w_in = f(inputs["w_in"][0])
    perm = np.arange(1024) ^ 1
    w_perm = np.ascontiguousarray(w_in[:, 2056:3080][:, perm])
    cw = np.ascontiguousarray(f(inputs["conv_w"][0]).T.reshape(12, 128, 4).transpose(1, 0, 2))
    cm, ret_dt, retvec, cdec, cosT, sinT = make_consts()
    shared = {
        "w_in": w_in, "w_perm": w_perm, "cw": cw,
        "A_log": f(inputs["A_log"][0]), "dt_bias": f(inputs["dt_bias"][0]),
        "gdn_norm_w": f(inputs["gdn_norm_w"][0]),
        "w_up_gdn": f(inputs["w_up_gdn"][0]), "w_up_ret": f(inputs["w_up_ret"][0]), "w_out": f(inputs["w_out"][0]),
        "nmix": np.ascontiguousarray(f(inputs["norm_mix_w"][0]).reshape(8, 128).T),
        "nffn": np.ascontiguousarray(f(inputs["norm_ffn_w"][0]).reshape(8, 128).T),
        "w_router": np.ascontiguousarray(np.concatenate([f(inputs["w_group"][0]), f(inputs["w_expert"][0])], axis=1)),
        "b_router": np.ascontiguousarray(np.concatenate([f(inputs["b_group"][0]), f(inputs["b_expert"][0])], axis=0)),
        "w_gate": f(inputs["w_gate"][0]), "w_up": f(inputs["w_up"][0]), "w_down": f(inputs["w_down"][0]),
        "norm_final_w": f(inputs["norm_final_w"]),
        "misc": np.ascontiguousarray(np.concatenate([
            np.broadcast_to(np.concatenate([256.0 * np.arange(8), np.arange(48), np.arange(32)]).astype(np.float32)[None, :], (128, 88)),
            (np.arange(8)[None, :] * 128 + np.arange(128)[:, None]).astype(np.float32)], axis=1)),
        "nffn_row": f(inputs["norm_ffn_w"][0]),
        "cm": cm, "ret_dt": ret_dt, "retvec": retvec, "cosT": cosT, "sinT": sinT,
    }
    return shared


def kernel(**inputs):
    x = np.asarray(inputs["x"], dtype=np.float32)
    nb = x.shape[0]
    shared = _host_inputs(inputs)
    nc = build()
    in_maps = []
    for b in range(nb):
        m = dict(shared)
        m["x"] = np.ascontiguousarray(x[b])
        in_maps.append(m)
    res = run_bass_kernel_spmd(nc, in_maps, core_ids=list(range(nb)))
    return np.stack([np.asarray(r["out"], dtype=np.float32) for r in res.results], axis=0)
```

```python
import os
from contextlib import ExitStack
import numpy as np
import concourse.bass as bass
import concourse.mybir as mybir
from concourse.bass_utils import run_bass_kernel_spmd

F32 = mybir.dt.float32
BF16 = mybir.dt.bfloat16
AF = mybir.ActivationFunctionType
ALU = mybir.AluOpType
AX = mybir.AxisListType

T = 2048
D = 1024
NT = 16
EPS = 1e-6
ENGINES = ("pe", "act", "dve", "pool", "sp")
N_DMA_SEMS = 12
BIG = 1.0e30


class Sched:
    def __init__(self, nc, stack):
        self.nc = nc
        self.ops = []
        self.last_writer = {}
        self.readers = {}
        self.sem = {e: stack.enter_context(nc.semaphore("s_" + e)) for e in ENGINES}
        self.dma_sems = {}
        for q in ("sp", "act", "pool"):
            self.dma_sems[q] = [stack.enter_context(nc.semaphore(f"d_{q}{i}")) for i in range(N_DMA_SEMS)]
        self.n_dma = {q: 0 for q in self.dma_sems}
        self.cnt = {e: 0 for e in ENGINES}
        self.waited = {e: {} for e in ENGINES}

    PSUM_PREFIXES = ("ptA", "pbG", "pab", "BG", "BH", "BSC", "pbR", "BR", "BM", "PTE", "PLE", "BE")

    def add(self, eng, fn, reads=(), writes=(), dma=False):
        ex = [k for k in reads if k.startswith(self.PSUM_PREFIXES)]
        if ex:
            reads = [k for k in reads if k not in ex]
            writes = list(writes) + ex
        idx = len(self.ops)
        deps = set()
        for k in reads:
            w = self.last_writer.get(k)
            if w is not None:
                deps.add((w, "raw"))
        for k in writes:
            w = self.last_writer.get(k)
            if w is not None:
                deps.add((w, "waw"))
            for r in self.readers.get(k, ()):
                deps.add((r, "war"))
        op = dict(eng=eng, fn=fn, deps=deps, dma=dma, idx=idx, signal=False, ticket=None, dsem=None)
        if dma:
            n = self.n_dma[eng]
            self.n_dma[eng] = n + 1
            op["dsem"] = (eng, n % N_DMA_SEMS, 16 * (n // N_DMA_SEMS + 1))
        self.ops.append(op)
        for k in reads:
            self.readers.setdefault(k, []).append(idx)
        for k in writes:
            self.last_writer[k] = idx
            self.readers[k] = []
        return idx

    def barrier(self):
        keys = set(self.last_writer) | set(self.readers)
        keys.add("__bar__")
        for e in ENGINES:
            self.add(e, None, reads=(), writes=tuple(keys))

    def emit(self):
        ops = self.ops
        for op in ops:
            for (d, kind) in op["deps"]:
                dop = ops[d]
                if dop["dma"]:
                    continue
                if dop["eng"] == op["eng"]:
                    if op["eng"] in ("pe", "sp") or kind == "war":
                        continue
                dop["signal"] = True
        for op in ops:
            if op["dma"]:
                continue
            if op["signal"]:
                self.cnt[op["eng"]] += 1
                op["ticket"] = self.cnt[op["eng"]]
        per_eng = {e: [op for op in ops if op["eng"] == e] for e in ENGINES}
        sem = self.sem
        dma_sems = self.dma_sems

        def run(e, engobj):
            waited = self.waited[e]
            for op in per_eng[e]:
                waits = {}
                for (d, kind) in op["deps"]:
                    dop = ops[d]
                    if dop["dma"]:
                        q, si, val = dop["dsem"]
                        key = ("d", q, si)
                        waits[key] = max(waits.get(key, 0), val)
                        continue
                    if dop["eng"] == e:
                        if e in ("pe", "sp") or kind == "war":
                            continue
                    if dop["ticket"] is None:
                        continue
                    key = ("e", dop["eng"])
                    waits[key] = max(waits.get(key, 0), dop["ticket"])
                if op["dma"]:
                    q, si, val = op["dsem"]
                    if val > 16:
                        key = ("d", q, si)
                        waits[key] = max(waits.get(key, 0), val - 16)
                for key, val in waits.items():
                    if waited.get(key, 0) >= val:
                        continue
                    waited[key] = val
                    s = sem[key[1]] if key[0] == "e" else dma_sems[key[1]][key[2]]
                    engobj.wait_ge(s, val)
                if op["fn"] is None:
                    if op["signal"]:
                        engobj.nop().then_inc(sem[e], 1)
                    continue
                ins = op["fn"](engobj)
                if op["dma"]:
                    q, si, val = op["dsem"]
                    ins.then_inc(dma_sems[q][si], 16)
                elif op["signal"]:
                    ins.then_inc(sem[e], 1)

        with self.nc.Block() as block:
            @block.tensor
            def _(eng):
                run("pe", eng)

            @block.scalar
            def _(eng):
                run("act", eng)

            @block.vector
            def _(eng):
                run("dve", eng)

            @block.gpsimd
            def _(eng):
                run("pool", eng)

            @block.sync
            def _(eng):
                run("sp", eng)
        self.ops = []
        self.last_writer = {}
        self.readers = {}


CM_ID, CM_TL, CM_SU, CM_MLS, CM_MCT, CM_SEL0, CM_SEL1, CM_BO, CM_ONES, CM_UT = range(10)


def make_consts():
    i = np.arange(128)
    same = (i[:, None] // 64) == (i[None, :] // 64)
    cm = np.zeros((10, 128, 128), np.float32)
    cm[CM_ID] = np.eye(128)
    cm[CM_TL] = same & (i[:, None] <= i[None, :])
    cm[CM_SU] = same & (i[:, None] > i[None, :])
    cm[CM_MLS] = same & (i[:, None] > i[None, :])
    cm[CM_MCT] = same & (i[None, :] >= i[:, None])
    cm[CM_SEL0] = (i[:, None] < 64) & np.ones((1, 128), bool)
    cm[CM_SEL1] = (i[:, None] >= 64) & np.ones((1, 128), bool)
    cm[CM_BO] = same
    cm[CM_ONES] = 1.0
    cm[CM_UT] = i[:, None] < i[None, :]
    cm = np.ascontiguousarray(cm.transpose(1, 0, 2))
    h = np.arange(4, dtype=np.float64)
    lg = np.log(1.0 - 2.0 ** (-5.0 - h))
    idx = np.arange(128, dtype=np.float64)
    rel = idx[None, :] - idx[:, None]
    dt = np.where(rel[None] >= 0, np.exp(np.maximum(rel[None], 0) * lg[:, None, None]), 0.0) * (128 ** -0.5)
    ret_dt = np.ascontiguousarray(dt.transpose(1, 0, 2)).astype(np.float32)
    kdec = np.exp(lg[None, :] * (127.0 - idx)[:, None]) * (128 ** -0.5)
    qdec = np.exp(lg[None, :] * (idx + 1.0)[:, None])
    retvec = np.concatenate([kdec, qdec], axis=1).astype(np.float32)
    cdec = [float(np.exp(lg[k] * 128.0)) for k in range(4)]
    inv_freq = 1.0 / (10000.0 ** np.linspace(0.0, 1.0, 64).astype(np.float32).astype(np.float64))
    ang = np.arange(T, dtype=np.float64)[None, :] * np.repeat(inv_freq, 2)[:, None].astype(np.float32).astype(np.float64)
    ang32 = (np.arange(T, dtype=np.float32)[None, :] * np.repeat(inv_freq.astype(np.float32), 2)[:, None]).astype(np.float64)
    cosT = np.cos(ang32).astype(np.float32)
    sgn = np.where(np.arange(128) % 2 == 0, -1.0, 1.0)[:, None]
    sinT = (np.sin(ang32) * sgn).astype(np.float32)
    return cm, ret_dt, retvec, cdec, cosT, sinT


def build(debug=False, stop_after=None):
    nc = bass.Bass("TRN2", target_bir_lowering=False)
    cdec = make_consts()[3]

    def din(name, shape):
        return nc.dram_tensor(name, list(shape), F32, kind="ExternalInput").ap()

    x = din("x", [T, D])
    w_in = din("w_in", [D, 7176])
    w_perm = din("w_perm", [D, 1024])
    cw_d = din("cw", [128, 12, 4])
    alog_d = din("A_log", [4])
    dtb_d = din("dt_bias", [4])
    gnw_d = din("gdn_norm_w", [128])
    wupa_d = din("w_up_gdn", [512, D])
    wupr_d = din("w_up_ret", [D, D])
    wout_d = din("w_out", [D, D])
    nmix_d = din("nmix", [128, 8])
    nffn_d = din("nffn", [128, 8])
    wr_d = din("w_router", [D, 36])
    br_d = din("b_router", [36])
    wg_d = din("w_gate", [32, D, 512])
    wu_d = din("w_up", [32, D, 512])
    wd_d = din("w_down", [32, 512, D])
    nfin_d = din("norm_final_w", [D])
    cm_d = din("cm", [128, 10, 128])
    misc_d = din("misc", [128, 96])
    nffnrow_d = din("nffn_row", [D])
    retdt_d = din("ret_dt", [128, 4, 128])
    retvec_d = din("retvec", [128, 8])
    cos_d = din("cosT", [128, T])
    sin_d = din("sinT", [128, T])
    out = nc.dram_tensor("out", [T, D], F32, kind="ExternalOutput").ap()
    h1s = nc.dram_tensor("dbg_h1" if debug else "h1s", [T, D], F32, kind="ExternalOutput" if debug else "Internal").ap()
    dbg = {}
    if debug:
        dbg["yT"] = nc.dram_tensor("dbg_yT", [12 * 128, T], BF16, kind="ExternalOutput").ap()
        dbg["uT"] = nc.dram_tensor("dbg_uT", [8 * 128, T], BF16, kind="ExternalOutput").ap()

    with ExitStack() as st:
        S = Sched(nc, st)

        def sbuf(stk, name, shape, dt):
            return stk.enter_context(nc.sbuf_tensor("sb_" + name, list(shape), dt))

        def psum(stk, name, shape, dt):
            return stk.enter_context(nc.psum_tensor("ps_" + name, list(shape), dt))

        def dma(out_ap, in_ap, reads=(), writes=(), q="sp", **kw):
            S.add(q, lambda e: e.dma_start(out=out_ap, in_=in_ap, **kw), reads, writes, dma=True)

        def mm(o, lhsT, rhs, start, stop, reads, writes):
            S.add("pe", lambda e: e.matmul(o, lhsT=lhsT, rhs=rhs, start=start, stop=stop), reads, writes)

        def tr(o, in_, ident, reads, writes):
            S.add("pe", lambda e: e.transpose(o, in_, ident), reads, writes)

        def act(o, in_, func, reads, writes, **kw):
            S.add("act", lambda e: e.activation(out=o, in_=in_, func=func, **kw), reads, writes)

        def tt(o, a, b, op, reads, writes, eng="dve"):
            S.add(eng, lambda e: e.tensor_tensor(o, a, b, op), reads, writes)

        def ts(o, a, s1, s2, op0, op1, reads, writes, eng="dve"):
            if op1 is None:
                S.add(eng, lambda e: e.tensor_scalar(o, a, s1, None, op0), reads, writes)
            else:
                S.add(eng, lambda e: e.tensor_scalar(o, a, s1, s2, op0, op1), reads, writes)

        def stt(o, a, s, b, op0, op1, reads, writes, eng="dve"):
            S.add(eng, lambda e: e.scalar_tensor_tensor(o, a, s, b, op0, op1), reads, writes)

        def cp(o, a, reads, writes, eng="dve"):
            S.add(eng, lambda e: e.tensor_copy(o, a), reads, writes)

        def red(o, a, op, reads, writes, eng="dve"):
            S.add(eng, lambda e: e.tensor_reduce(o, a, AX.X, op), reads, writes)

        def memset(o, v, writes, eng="dve"):
            S.add(eng, lambda e: e.memset(o, v), (), writes)

        def recip(o, a, reads, writes):
            S.add("dve", lambda e: e.reciprocal(o, a), reads, writes)

        def rsqrt_small(o, a, scale, reads, writes):
            act(o, a, AF.Ln, list(reads) + ["epsb"], writes, scale=scale, bias=epsb[:])
            act(o, o, AF.Exp, writes, writes, scale=-0.5)

        cm = sbuf(st, "cm", [128, 10, 128], F32)
        identb = sbuf(st, "identb", [128, 128], BF16)
        onesb = sbuf(st, "onesb", [128, 128], BF16)
        epsb = sbuf(st, "epsb", [128, 1], F32)
        nmix = sbuf(st, "nmix", [128, 8], F32)
        nffn = sbuf(st, "nffn", [128, 8], F32)
        wstage = {}
        wcnt = [0]

        def alloc_wstage(stk):
            wstage["st"] = [sbuf(stk, f"wst{i}_{wcnt[0]}", [128, 8, 256], F32) for i in range(2)]
            wstage["bf"] = [sbuf(stk, f"wbf{i}_{wcnt[0]}", [128, 8, 256], BF16) for i in range(2)]

        dma(cm[:], cm_d, writes=["cm"])
        dma(nmix[:], nmix_d, writes=["nmix"])
        dma(nffn[:], nffn_d, writes=["nffn"])
        cp(identb[:], cm[:, CM_ID, :], ["cm"], ["identb"])
        cp(onesb[:], cm[:, CM_ONES, :], ["cm"], ["onesb"])
        memset(epsb[:], EPS, ["epsb"])
        ident = cm[:, CM_ID, :]

        def load_w(src_ap, ncols):
            wst, wbf = wstage["st"], wstage["bf"]
            i = wcnt[0] % 2
            wcnt[0] += 1
            dma(wst[i][:, :, 0:ncols], src_ap.rearrange("(c p) n -> p c n", p=128), writes=[f"wst{i}"])
            cp(wbf[i][:, :, 0:ncols], wst[i][:, :, 0:ncols], [f"wst{i}"], [f"wbf{i}"], eng="pool")
            return wbf[i], f"wbf{i}"

        with ExitStack() as mix:
            uT = sbuf(mix, "uT", [128, 8, T], BF16)
            yT = sbuf(mix, "yT", [128, 12, T], BF16)

            with ExitStack() as ph:
                xt = [sbuf(ph, f"xt{i}", [128, D], F32) for i in range(2)]
                sq = sbuf(ph, "sq", [128, D], F32)
                xs = [sbuf(ph, f"xs{i}", [128, D], BF16) for i in range(2)]
                ss = sbuf(ph, "ssA", [128, NT], F32)
                rstd = sbuf(ph, "rstdA", [128, NT], F32)
                pt = [psum(ph, f"ptA{i}", [128, 8, 128], BF16) for i in range(2)]
                memset(ss[:], 0.0, ["ssA"])
                for t in range(NT):
                    i = t % 2
                    dma(xt[i][:], x[t * 128:(t + 1) * 128, :], writes=[f"xt{i}"])
                    act(sq[:], xt[i][:], AF.Square, [f"xt{i}", "ssA"], ["sq", "ssA"], accum_out=ss[:, t:t + 1])
                    rsqrt_small(rstd[:, t:t + 1], ss[:, t:t + 1], 1.0 / D, ["ssA"], [f"rstdA{t}"])
                    ts(xs[i][:], xt[i][:], rstd[:, t:t + 1], None, ALU.mult, None, [f"xt{i}", f"rstdA{t}"], [f"xs{i}"])
                    for c in range(8):
                        tr(pt[i][:, c, :], xs[i][:, c * 128:(c + 1) * 128], identb[:], [f"xs{i}", "identb"], [f"ptA{i}"])
                    tt(uT[:, :, t * 128:(t + 1) * 128], pt[i][:], nmix[:, :].unsqueeze(2).to_broadcast([128, 8, 128]),
                       ALU.mult, [f"ptA{i}", "nmix"], [f"uT{t}"])
                S.barrier()
                S.emit()
                if stop_after == "A":
                    return nc
            uT_keys = [f"uT{t}" for t in range(NT)]

            def proj_fm(bank, bkey, wt, wkey, col, blk):
                for c in range(8):
                    mm(bank, wt[:, c, col:col + 128], uT[:, c, blk * 512:(blk + 1) * 512], c == 0, c == 7,
                       [wkey], [bkey])

            def proj_tm(bank_ap, bkey, wt_ap_fn, wkey, t):
                for c in range(8):
                    mm(bank_ap, uT[:, c, t * 128:(t + 1) * 128], wt_ap_fn(c), c == 0, c == 7, [wkey], [bkey])

            with ExitStack() as ph:
                qkvT = sbuf(ph, "qkvT", [128, 12, T], BF16)
                cwt = sbuf(ph, "cwt", [128, 12, 4], F32)
                dma(cwt[:], cw_d, writes=["cwt"])
                with ExitStack() as g1:
                    alloc_wstage(g1)
                    xc = [sbuf(g1, f"xc{i}", [128, 3 + T], BF16) for i in range(2)]
                    diag = [sbuf(g1, f"diag{i}", [128, 4, 128], BF16) for i in range(2)]
                    s16 = [sbuf(g1, f"s16_{i}", [128, 512], BF16) for i in range(2)]
                    sq16 = [sbuf(g1, f"sq16_{i}", [128, 512], BF16) for i in range(2)]
                    rn = [sbuf(g1, f"rn{i}", [128, 512], F32) for i in range(2)]
                    pb = [psum(g1, f"pbG{i}", [128, 512], F32) for i in range(8)]
                    for i in range(2):
                        memset(xc[i][:, 0:3], 0.0, [f"xc{i}"])
                    for ch in range(12):
                        i = ch % 2
                        if ch % 2 == 0:
                            wt, wkey = load_w(w_in[:, ch * 128:(ch + 2) * 128], 256)
                        col = (ch % 2) * 128
                        for blk in range(4):
                            proj_fm(pb[blk][:], f"pbG{blk}", wt, wkey, col, blk)
                            act(xc[i][:, 3 + blk * 512:3 + (blk + 1) * 512], pb[blk][:], AF.Copy, [f"pbG{blk}"], [f"xc{i}"])
                        for k in range(4):
                            ts(diag[i][:, k, :], identb[:], cwt[:, ch, k:k + 1], None, ALU.mult, None,
                               ["identb", "cwt"], [f"diag{i}"])
                        for blk in range(4):
                            b2 = 4 + (blk % 2)
                            j = blk % 2
                            for k in range(4):
                                mm(pb[b2][:], diag[i][:, k, :], xc[i][:, blk * 512 + k:blk * 512 + k + 512], k == 0, k == 3,
                                   [f"diag{i}", f"xc{i}"], [f"pbG{b2}"])
                            dst = qkvT[:, ch, blk * 512:(blk + 1) * 512]
                            act(dst, pb[b2][:], AF.Silu, [f"pbG{b2}"], [f"qkvT{ch}_{blk}"])
                    for ch in range(8):
                        for blk in range(4):
                            j = blk % 2
                            b3 = 6 + j
                            dst = qkvT[:, ch, blk * 512:(blk + 1) * 512]
                            tt(sq16[j][:], dst, dst, ALU.mult, [f"qkvT{ch}_{blk}"], [f"sq16_{j}"])
                            mm(pb[b3][:], onesb[:], sq16[j][:], True, True, ["onesb", f"sq16_{j}"], [f"pbG{b3}"])
                            act(rn[j][:], pb[b3][:], AF.Ln, [f"pbG{b3}", "epsb"], [f"rn{j}"], bias=epsb[:])
                            act(rn[j][:], rn[j][:], AF.Exp, [f"rn{j}"], [f"rn{j}"], scale=-0.5)
                            tt(dst, dst, rn[j][:], ALU.mult, [f"qkvT{ch}_{blk}", f"rn{j}"], [f"qkvT{ch}_{blk}"])
                    S.barrier()
                    S.emit()
                    if stop_after == "G1":
                        return nc
                gg = sbuf(ph, "gg", [128, NT, 4], F32)
                beta = sbuf(ph, "beta", [128, NT, 4], F32)
                wz = sbuf(ph, "wz", [128, 8, 512], BF16)
                gnw = sbuf(ph, "gnw", [128, 128], F32)
                with ExitStack() as g2:
                    alloc_wstage(g2)
                    ab = sbuf(g2, "ab", [128, NT, 8], F32)
                    tmp4 = sbuf(g2, "tmp4", [128, NT, 4], F32)
                    alog = sbuf(g2, "alog", [128, 4], F32)
                    dtb = sbuf(g2, "dtb", [128, 4], F32)
                    pab = psum(g2, "pab", [128, NT, 8], F32)
                    dma(alog[:], alog_d.partition_broadcast(128), writes=["alog"])
                    dma(dtb[:], dtb_d.partition_broadcast(128), writes=["dtb"])
                    dma(gnw[:], gnw_d.partition_broadcast(128), writes=["gnw"])
                    wt, wkey = load_w(w_in[:, 1536:1544], 8)
                    for t in range(NT):
                        proj_tm(pab[:, t, :], "pab", lambda c: wt[:, c, 0:8], wkey, t)
                    cp(ab[:], pab[:], ["pab"], ["ab"])
                    for j in range(2):
                        wt2, wkey2 = load_w(w_in[:, 1544 + j * 256:1544 + (j + 1) * 256], 256)
                        cp(wz[:, :, j * 256:(j + 1) * 256], wt2[:, :, 0:256], [wkey2], ["wz"])
                    tt(tmp4[:], ab[:, :, 0:4], dtb[:, :].unsqueeze(1).to_broadcast([128, NT, 4]), ALU.add, ["ab", "dtb"], ["tmp4"])
                    act(tmp4[:], tmp4[:], AF.Exp, ["tmp4"], ["tmp4"])
                    act(tmp4[:], tmp4[:], AF.Ln, ["tmp4"], ["tmp4"], bias=1.0)
                    act(alog[:], alog[:], AF.Exp, ["alog"], ["alog"])
                    stt(gg[:], tmp4[:], -1.0, alog[:, :].unsqueeze(1).to_broadcast([128, NT, 4]), ALU.mult, ALU.mult,
                        ["tmp4", "alog"], ["gg"])
                    act(beta[:], ab[:, :, 4:8], AF.Exp, ["ab"], ["beta"], scale=-1.0)
                    ts(beta[:], beta[:], 1.0, None, ALU.add, None, ["beta"], ["beta"])
                    recip(beta[:], beta[:], ["beta"], ["beta"])
                    S.barrier()
                    S.emit()
                    if stop_after == "G2":
                        return nc
                with ExitStack() as g3:
                    B0 = psum(g3, "BG0", [128, 512], F32)
                    BT = psum(g3, "BGT", [128, 8, 128], BF16)
                    H = [psum(g3, f"BH{h}", [128, 512], F32) for h in range(4)]
                    SC = [psum(g3, f"BSC{i}", [128, 512], F32) for i in range(2)]
                    gs = sbuf(g3, "gs", [128, 16], F32)
                    es2 = [sbuf(g3, f"es{i}", [128, 16], F32) for i in range(2)]
                    bg = sbuf(g3, "bg", [128, 4], F32)
                    kbg = sbuf(g3, "kbg", [128, 4, 128], BF16)
                    kd2 = [sbuf(g3, f"kd{i}", [128, 4, 128], BF16) for i in range(2)]
                    vb = sbuf(g3, "vb", [128, 4, 128], BF16)
                    Gg = [sbuf(g3, f"Gg{h}", [128, 128], F32) for h in range(4)]
                    Ed = [sbuf(g3, f"Ed{h}", [128, 2, 128], F32) for h in range(4)]
                    Dm = [sbuf(g3, f"Dm{h}", [128, 2, 128], F32) for h in range(4)]
                    LN = [[sbuf(g3, f"LN{h}_{i}", [128, 2, 128], F32) for i in range(2)] for h in range(4)]
                    Pm = [[sbuf(g3, f"Pm{h}_{i}", [128, 128], F32) for i in range(2)] for h in range(4)]
                    TTb = [sbuf(g3, f"TTb{h}", [128, 128], BF16) for h in range(4)]
                    dg = [sbuf(g3, f"dg{h}", [128, 128], BF16) for h in range(4)]
                    attnT2 = [sbuf(g3, f"attnT{i}", [128, 4, 128], BF16) for i in range(2)]
                    qgT2 = [sbuf(g3, f"qgT{i}", [128, 4, 128], BF16) for i in range(2)]
                    wT2 = [sbuf(g3, f"wT{i}", [128, 4, 128], BF16) for i in range(2)]
                    uu2 = [sbuf(g3, f"uu{i}", [128, 4, 128], F32) for i in range(2)]
                    vnew = sbuf(g3, "vnew", [128, 4, 128], BF16)
                    S32 = sbuf(g3, "S32", [128, 4, 128], F32)
                    S16 = sbuf(g3, "S16", [128, 4, 128], BF16)
                    oo = sbuf(g3, "oo", [128, 4, 128], F32)
                    osq = sbuf(g3, "osq", [128, 4, 128], F32)
                    oss = sbuf(g3, "oss", [128, 4], F32)
                    orst = sbuf(g3, "orst", [128, 4], F32)
                    sz = sbuf(g3, "sz", [128, 512], F32)
                    ya = sbuf(g3, "ya", [128, 512], BF16)
                    memset(S32[:], 0.0, [f"S32_{h}" for h in range(4)])
                    memset(S16[:], 0.0, [f"S16_{h}" for h in range(4)])
                    TL = cm[:, CM_TL, :]
                    SCL = float(128 ** -0.5)

                    def head_prep(n, h):
                        tsl = slice(n * 128, (n + 1) * 128)
                        Hh, hk = H[h], f"BH{h}"
                        q_ = n % 2
                        es, attnT, qgT, wT, uu = es2[q_], attnT2[q_], qgT2[q_], wT2[q_], uu2[q_]
                        esk = f"es{q_}"
                        kTh = qkvT[:, 4 + h, tsl]
                        qTh = qkvT[:, h, tsl]
                        ts(Gg[h][:], cm[:, CM_SU, :], gg[:, n, h:h + 1], None, ALU.mult, None, ["cm", "gg"], [f"Gg{h}"])
                        ts(dg[h][:], identb[:], es[:, h:h + 1], None, ALU.mult, None, ["identb", esk], [f"dg{h}"])
                        yield
                        mm(Hh[:, 0:128], Gg[h][:], TL, True, True, [f"Gg{h}", "cm"], [hk])
                        mm(Hh[:, 128:256], TL, Gg[h][:], True, True, [f"Gg{h}", "cm"], [hk])
                        mm(Hh[:, 256:384], kTh, kTh, True, True, [], [hk])
                        mm(Hh[:, 384:512], kTh, qTh, True, True, [], [hk])
                        yield
                        act(Ed[h][:], Hh[:, 0:256].rearrange("p (a b) -> p a b", a=2), AF.Exp, [hk], [f"Ed{h}"])
                        yield
                        tt(Dm[h][:, 0, :], Ed[h][:, 0, :], cm[:, CM_MCT, :], ALU.mult, [f"Ed{h}", "cm"], [f"DmA{h}"], eng="pool")
                        tt(Dm[h][:, 1, :], Ed[h][:, 1, :], cm[:, CM_MLS, :], ALU.mult, [f"Ed{h}", "cm"], [f"DmB{h}"])
                        yield
                        stt(LN[h][0][:, 0, :], Hh[:, 256:384], beta[:, n, h:h + 1], Dm[h][:, 1, :], ALU.mult, ALU.mult,
                            [hk, "beta", f"DmB{h}"], [f"LN{h}_0"])
                        stt(attnT[:, h, :], Hh[:, 384:512], SCL, Dm[h][:, 0, :], ALU.mult, ALU.mult,
                            [hk, f"DmA{h}"], [f"attnT{q_}_{h}"])
                        yield
                        mm(Hh[:, 0:128], LN[h][0][:, 0, :], ident, True, True, [f"LN{h}_0", "cm"], [hk])
                        yield
                        act(LN[h][0][:, 1, :], Hh[:, 0:128], AF.Copy, [hk], [f"LN{h}_0"])
                        yield
                        tt(Pm[h][0][:], ident, LN[h][0][:, 1, :], ALU.subtract, ["cm", f"LN{h}_0"], [f"Pm{h}_0"])
                        a, p = 0, 0
                        for lvl in range(1, 6):
                            na = 1 - a
                            Lc, Nc = LN[h][a][:, 0, :], LN[h][a][:, 1, :]
                            mm(Hh[:, 0:128], Nc, Lc, True, True, [f"LN{h}_{a}"], [hk])
                            if lvl < 5:
                                mm(Hh[:, 128:256], Lc, Nc, True, True, [f"LN{h}_{a}"], [hk])
                            yield
                            if lvl < 5:
                                act(LN[h][na][:], Hh[:, 0:256].rearrange("p (a b) -> p a b", a=2), AF.Copy, [hk], [f"LN{h}_{na}"])
                            else:
                                act(LN[h][na][:, 0, :], Hh[:, 0:128], AF.Copy, [hk], [f"LN{h}_{na}"])
                            yield
                            mm(Hh[:, 256:384], LN[h][na][:, 0, :], Pm[h][p][:], True, True, [f"LN{h}_{na}", f"Pm{h}_{p}"], [hk])
                            yield
                            if lvl < 5:
                                tt(Pm[h][1 - p][:], Hh[:, 256:384], Pm[h][p][:], ALU.add, [hk, f"Pm{h}_{p}"], [f"Pm{h}_{1 - p}"])
                            else:
                                tt(TTb[h][:], Hh[:, 256:384], Pm[h][p][:], ALU.add, [hk, f"Pm{h}_{p}"], [f"TTb{h}"])
                            yield
                            a, p = na, 1 - p
                        mm(Hh[:, 0:128], TTb[h][:], vb[:, h, :], True, True, [f"TTb{h}", "vb"], [hk])
                        mm(Hh[:, 128:256], kbg[:, h, :], TTb[h][:], True, True, [f"TTb{h}", "kbg"], [hk])
                        mm(Hh[:, 256:384], onesb[:], dg[h][:], True, True, ["onesb", f"dg{h}"], [hk])
                        yield
                        cp(uu[:, h, :], Hh[:, 0:128], [hk], [f"uu{q_}_{h}"])
                        stt(qgT[:, h, :], Hh[:, 256:384], SCL, qTh, ALU.mult, ALU.mult, [hk], [f"qgT{q_}_{h}"])
                        act(wT[:, h, :], Hh[:, 128:256], AF.Copy, [hk], [f"wT{q_}_{h}"])
                        yield

                    def head_scan(n, h):
                        Hh, hk = SC[h % 2], f"BSC{h % 2}"
                        cb = (h // 2) * 256
                        RA = slice(cb, cb + 128)
                        RB = slice(cb + 128, cb + 256)
                        q_ = n % 2
                        es, kd, attnT, qgT, wT, uu = es2[q_], kd2[q_], attnT2[q_], qgT2[q_], wT2[q_], uu2[q_]
                        for half in range(2):
                            hs = slice(half * 64, (half + 1) * 64)
                            mm(Hh[:, RA], wT[:, h, :], S16[:, h, :], True, True, [f"wT{q_}_{h}", f"S16_{h}"], [hk])
                            yield
                            tt(vnew[hs, h, :], uu[hs, h, :], Hh[hs, RA], ALU.subtract, [f"uu{q_}_{h}", hk], [f"vnew{h}"])
                            yield
                            mm(Hh[:, RB], qgT[:, h, :], S16[:, h, :], True, False, [f"qgT{q_}_{h}", f"S16_{h}"], [hk])
                            mm(Hh[:, RB], attnT[hs, h, :], vnew[hs, h, :], False, True, [f"attnT{q_}_{h}", f"vnew{h}"], [hk])
                            mm(Hh[:, RA], kd[hs, h, :], vnew[hs, h, :], True, True, [f"kd{q_}", f"vnew{h}"], [hk])
                            yield
                            stt(S32[:, h, :], S32[:, h, :], es[:, 8 + 4 * half + h:9 + 4 * half + h], Hh[:, RA],
                                ALU.mult, ALU.add, [hk, f"es{q_}", f"S32_{h}"], [f"S32_{h}"])
                            act(oo[hs, h, :], Hh[hs, RB], AF.Copy, [hk], [f"oo{h}"])
                            yield
                            act(S16[:, h, :], S32[:, h, :], AF.Copy, [f"S32_{h}"], [f"S16_{h}"])
                            yield

                    def rr(gens):
                        gens = list(gens)
                        while gens:
                            for g in list(gens):
                                try:
                                    next(g)
                                except StopIteration:
                                    gens.remove(g)

                    def pre_tile(n):
                        tsl = slice(n * 128, (n + 1) * 128)
                        q_ = n % 2
                        es, kd, esk = es2[q_], kd2[q_], f"es{q_}"
                        mm(B0[:, 0:4], TL, gg[:, n, :], True, True, ["cm", "gg"], ["BG0"])
                        mm(B0[:, 4:8], cm[:, CM_BO, :], gg[:, n, :], True, True, ["cm", "gg"], ["BG0"])
                        mm(B0[:, 8:12], cm[:, CM_SEL0, :], gg[:, n, :], True, True, ["cm", "gg"], ["BG0"])
                        mm(B0[:, 12:16], cm[:, CM_SEL1, :], gg[:, n, :], True, True, ["cm", "gg"], ["BG0"])
                        cp(gs[:], B0[:, 0:16], ["BG0"], ["gs"])
                        tt(gs[:, 4:8], gs[:, 4:8], gs[:, 0:4], ALU.subtract, ["gs"], ["gs"])
                        act(es[:], gs[:], AF.Exp, ["gs"], [esk])
                        tt(bg[:], es[:, 0:4], beta[:, n, :], ALU.mult, [esk, "beta"], ["bg"])
                        for h in range(4):
                            tr(BT[:, h, :], qkvT[:, 4 + h, tsl], identb[:], ["identb"], ["BGT"])
                            tr(BT[:, 4 + h, :], qkvT[:, 8 + h, tsl], identb[:], ["identb"], ["BGT"])
                        tt(kbg[:], BT[:, 0:4, :], bg[:, :].unsqueeze(2).to_broadcast([128, 4, 128]), ALU.mult, ["BGT", "bg"], ["kbg"])
                        tt(kd[:], BT[:, 0:4, :], es[:, 4:8].unsqueeze(2).to_broadcast([128, 4, 128]), ALU.mult, ["BGT", esk], [f"kd{q_}"])
                        tt(vb[:], BT[:, 4:8, :], beta[:, n, :].unsqueeze(2).to_broadcast([128, 4, 128]), ALU.mult, ["BGT", "beta"], ["vb"])

                    def out_tile(n):
                        tsl = slice(n * 128, (n + 1) * 128)
                        ook = [f"oo{h}" for h in range(4)]
                        tt(osq[:], oo[:], oo[:], ALU.mult, ook, ["osq"])
                        red(oss[:], osq[:], ALU.add, ["osq"], ["oss"])
                        rsqrt_small(orst[:], oss[:], 1.0 / 128, ["oss"], ["orst"])
                        proj_tm(B0[:], "BG0", lambda c: wz[:, c, :], "wz", n)
                        act(sz[:], B0[:], AF.Silu, ["BG0"], ["sz"])
                        tt(osq[:], oo[:], orst[:, :].unsqueeze(2).to_broadcast([128, 4, 128]), ALU.mult, ook + ["orst"], ["osq"])
                        tt(osq[:], osq[:], gnw[:, :].unsqueeze(1).to_broadcast([128, 4, 128]), ALU.mult, ["osq", "gnw"], ["osq"])
                        tt(ya[:], osq[:].rearrange("p a b -> p (a b)"), sz[:], ALU.mult, ["osq", "sz"], ["ya"])
                        for h in range(4):
                            tr(BT[:, h, :], ya[:, h * 128:(h + 1) * 128], identb[:], ["ya", "identb"], ["BGT"])
                        cp(yT[:, 0:4, tsl], BT[:, 0:4, :], ["BGT"], [f"yT{n}"])

                    for n in range(NT + 1):
                        gens = []
                        if n < NT:
                            pre_tile(n)
                            gens += [head_prep(n, h) for h in range(4)]
                        if n >= 1:
                            gens += [head_scan(n - 1, h) for h in range(4)]
                        rr(gens)
                        if n >= 1:
                            out_tile(n - 1)
                    S.barrier()
                    S.emit()
                    if stop_after == "G3":
                        return nc

            with ExitStack() as ph:
                rqkT = sbuf(ph, "rqkT", [128, 8, T], BF16)
                retdt = sbuf(ph, "retdt", [128, 4, 128], F32)
                retvec = sbuf(ph, "retvec", [128, 8], F32)
                dma(retdt[:], retdt_d, writes=["retdt"])
                dma(retvec[:], retvec_d, writes=["retvec"])
                with ExitStack() as r1:
                    alloc_wstage(r1)
                    cosT = sbuf(r1, "cosT", [128, T], F32)
                    sinT = sbuf(r1, "sinT", [128, T], F32)
                    t1 = [sbuf(r1, f"t1_{i}", [128, 512], F32) for i in range(2)]
                    t2 = [sbuf(r1, f"t2_{i}", [128, 512], F32) for i in range(2)]
                    pb = [psum(r1, f"pbR{i}", [128, 512], F32) for i in range(8)]
                    dma(cosT[:], cos_d, writes=["cosT"])
                    dma(sinT[:], sin_d, writes=["sinT"])
                    for ch in range(8):
                        if ch % 2 == 0:
                            wt, wkey = load_w(w_in[:, 2056 + ch * 128:2056 + (ch + 2) * 128], 256)
                            wtp, wkeyp = load_w(w_perm[:, ch * 128:(ch + 2) * 128], 256)
                        col = (ch % 2) * 128
                        for blk in range(4):
                            j = blk % 2
                            b1, b2 = 2 * (blk % 4), 2 * (blk % 4) + 1
                            proj_fm(pb[b1][:], f"pbR{b1}", wt, wkey, col, blk)
                            proj_fm(pb[b2][:], f"pbR{b2}", wtp, wkeyp, col, blk)
                            bs = slice(blk * 512, (blk + 1) * 512)
                            tt(t1[j][:], pb[b1][:], cosT[:, bs], ALU.mult, [f"pbR{b1}", "cosT"], [f"t1_{j}"])
                            tt(t2[j][:], pb[b2][:], sinT[:, bs], ALU.mult, [f"pbR{b2}", "sinT"], [f"t2_{j}"])
                            tt(rqkT[:, ch, bs], t1[j][:], t2[j][:], ALU.add, [f"t1_{j}", f"t2_{j}"], [f"rqkT{ch}"], eng="pool")
                    S.barrier()
                    S.emit()
                    if stop_after == "R1":
                        return nc
                with ExitStack() as r2:
                    alloc_wstage(r2)
                    wv = sbuf(r2, "wv", [128, 8, 1024], BF16)
                    wgt = sbuf(r2, "wgt", [128, 8, 1024], BF16)
                    vtok = sbuf(r2, "vtok", [128, 1024], BF16)
                    szr = sbuf(r2, "szr", [128, 1024], F32)
                    kdk = sbuf(r2, "kdk", [128, 4, 128], BF16)
                    sc16 = [sbuf(r2, f"sc16_{h}", [128, 128], BF16) for h in range(4)]
                    R32 = sbuf(r2, "R32", [128, 4, 256], F32)
                    R16 = sbuf(r2, "R16", [128, 4, 256], BF16)
                    otmp = [sbuf(r2, f"otmp{h}", [128, 256], F32) for h in range(4)]
                    ro = sbuf(r2, "ro", [128, 4, 256], F32)
                    rsq = sbuf(r2, "rsq", [128, 4, 256], F32)
                    rss = sbuf(r2, "rss", [128, 4], F32)
                    rrst = sbuf(r2, "rrst", [128, 4], F32)
                    yb = sbuf(r2, "yb", [128, 1024], BF16)
                    Bv = [psum(r2, f"BRv{i}", [128, 512], F32) for i in range(2)]
                    H = [psum(r2, f"BRH{h}", [128, 512], F32) for h in range(4)]
                    BT = psum(r2, "BRT", [128, 8, 128], BF16)
                    for j in range(4):
                        wt2, wkey2 = load_w(w_in[:, 3080 + j * 256:3080 + (j + 1) * 256], 256)
                        cp(wv[:, :, j * 256:(j + 1) * 256], wt2[:, :, 0:256], [wkey2], ["wv"])
                    for j in range(4):
                        wt2, wkey2 = load_w(w_in[:, 4104 + j * 256:4104 + (j + 1) * 256], 256)
                        cp(wgt[:, :, j * 256:(j + 1) * 256], wt2[:, :, 0:256], [wkey2], ["wgt"])
                    memset(R32[:], 0.0, [f"R32_{h}" for h in range(4)])
                    memset(R16[:], 0.0, [f"R16_{h}" for h in range(4)])

                    def ret_head(n, h):
                        tsl = slice(n * 128, (n + 1) * 128)
                        Hh, hk = H[h], f"BRH{h}"
                        qTh = rqkT[:, h, tsl]
                        kTh = rqkT[:, 4 + h, tsl]
                        vh = vtok[:, h * 256:(h + 1) * 256]
                        mm(Hh[:, 0:128], kTh, qTh, True, True, [], [hk])
                        mm(Hh[:, 256:512], kdk[:, h, :], vh, True, True, ["kdk", "vtok"], [hk])
                        yield
                        tt(sc16[h][:], Hh[:, 0:128], retdt[:, h, :], ALU.mult, [hk, "retdt"], [f"sc16_{h}"])
                        stt(R32[:, h, :], R32[:, h, :], float(cdec[h]), Hh[:, 256:512], ALU.mult, ALU.add,
                            [hk, f"R32_{h}"], [f"R32_{h}"])
                        yield
                        mm(Hh[:, 0:256], sc16[h][:], vh, True, True, [f"sc16_{h}", "vtok"], [hk])
                        mm(Hh[:, 256:512], qTh, R16[:, h, :], True, True, [f"R16_{h}"], [hk])
                        yield
                        act(otmp[h][:], Hh[:, 0:256], AF.Copy, [hk], [f"otmp{h}"])
                        yield
                        stt(ro[:, h, :], Hh[:, 256:512], retvec[:, 4 + h:5 + h], otmp[h][:], ALU.mult, ALU.add,
                            [hk, f"otmp{h}", "retvec"], [f"ro{h}"])
                        act(R16[:, h, :], R32[:, h, :], AF.Copy, [f"R32_{h}"], [f"R16_{h}"])
                        yield

                    def rr2(gens):
                        gens = list(gens)
                        while gens:
                            for g in list(gens):
                                try:
                                    next(g)
                                except StopIteration:
                                    gens.remove(g)

                    for n in range(NT):
                        tsl = slice(n * 128, (n + 1) * 128)
                        for j in range(2):
                            proj_tm(Bv[j][:], f"BRv{j}", lambda c, j=j: wv[:, c, j * 512:(j + 1) * 512], "wv", n)
                            act(vtok[:, j * 512:(j + 1) * 512], Bv[j][:], AF.Copy, [f"BRv{j}"], ["vtok"])
                        for h in range(4):
                            tr(BT[:, h, :], rqkT[:, 4 + h, tsl], identb[:], ["identb"], ["BRT"])
                        tt(kdk[:], BT[:, 0:4, :], retvec[:, 0:4].unsqueeze(2).to_broadcast([128, 4, 128]), ALU.mult,
                           ["BRT", "retvec"], ["kdk"])
                        rr2(ret_head(n, h) for h in range(4))
                        for j in range(2):
                            proj_tm(Bv[j][:], f"BRv{j}", lambda c, j=j: wgt[:, c, j * 512:(j + 1) * 512], "wgt", n)
                            act(szr[:, j * 512:(j + 1) * 512], Bv[j][:], AF.Silu, [f"BRv{j}"], ["szr"])
                        rok = [f"ro{h}" for h in range(4)]
                        tt(rsq[:], ro[:], ro[:], ALU.mult, rok, ["rsq"])
                        red(rss[:], rsq[:], ALU.add, ["rsq"], ["rss"])
                        rsqrt_small(rrst[:], rss[:], 1.0 / 256, ["rss"], ["rrst"])
                        tt(rsq[:], ro[:], rrst[:, :].unsqueeze(2).to_broadcast([128, 4, 256]), ALU.mult, rok + ["rrst"], ["rsq"])
                        tt(yb[:], rsq[:].rearrange("p a b -> p (a b)"), szr[:], ALU.mult, ["rsq", "szr"], ["yb"])
                        for c in range(8):
                            tr(BT[:, c, :], yb[:, c * 128:(c + 1) * 128], identb[:], ["yb", "identb"], ["BRT"])
                        cp(yT[:, 4:12, tsl], BT[:], ["BRT"], [f"yT{n}"])
                    S.barrier()
                    S.emit()
                    if stop_after == "R2":
                        return nc
            if debug:
                for c in range(12):
                    dma(dbg["yT"][c * 128:(c + 1) * 128, :], yT[:, c, :], writes=["dbgyT"])
                for c in range(8):
                    dma(dbg["uT"][c * 128:(c + 1) * 128, :], uT[:, c, :], writes=["dbguT"])

            with ExitStack() as ph:
                mT = sbuf(ph, "mT", [128, 8, T], BF16)
                B = [psum(ph, f"BM{i}", [128, 512], F32) for i in range(8)]
                with ExitStack() as ms1:
                    wsm = [sbuf(ms1, f"wsm{i}", [128, 8, 128], F32) for i in range(4)]
                    wsb = [[sbuf(ms1, f"wsb{s_}_{i}", [128, 8, 128], BF16) for i in range(4)] for s_ in range(2)]
                    tA = [sbuf(ms1, f"tA{i}", [128, 512], F32) for i in range(2)]
                    m1 = [sbuf(ms1, f"m1_{i}", [128, 512], F32) for i in range(2)]
                    for ec in range(8):
                        es_ = slice(ec * 128, (ec + 1) * 128)
                        s_ = ec % 2
                        dma(wsb[s_][0][:, 0:4, :], wupa_d[:, es_].rearrange("(c p) n -> p c n", p=128), writes=[f"wsb{s_}_0"], q="pool")
                        dma(wsb[s_][1][:], wupr_d[:, es_].rearrange("(c p) n -> p c n", p=128), writes=[f"wsb{s_}_1"], q="pool")
                        dma(wsb[s_][2][:], w_in[:, 5128 + ec * 128:5128 + (ec + 1) * 128].rearrange("(c p) n -> p c n", p=128), writes=[f"wsb{s_}_2"], q="pool")
                        dma(wsb[s_][3][:], w_in[:, 6152 + ec * 128:6152 + (ec + 1) * 128].rearrange("(c p) n -> p c n", p=128), writes=[f"wsb{s_}_3"], q="pool")
                        for blk in range(4):
                            bs = slice(blk * 512, (blk + 1) * 512)
                            j = blk % 2
                            bA, bB, bMA, bMB = 4 * j, 4 * j + 1, 4 * j + 2, 4 * j + 3
                            for c in range(4):
                                mm(B[bA][:], wsb[s_][0][:, c, :], yT[:, c, bs], c == 0, c == 3, [f"wsb{s_}_0"], [f"BM{bA}"])
                            for c in range(8):
                                mm(B[bB][:], wsb[s_][1][:, c, :], yT[:, 4 + c, bs], c == 0, c == 7, [f"wsb{s_}_1"], [f"BM{bB}"])
                            for c in range(8):
                                mm(B[bMA][:], wsb[s_][2][:, c, :], uT[:, c, bs], c == 0, c == 7, [f"wsb{s_}_2"], [f"BM{bMA}"])
                            for c in range(8):
                                mm(B[bMB][:], wsb[s_][3][:, c, :], uT[:, c, bs], c == 0, c == 7, [f"wsb{s_}_3"], [f"BM{bMB}"])
                            act(tA[j][:], B[bMA][:], AF.Tanh, [f"BM{bMA}"], [f"tA{j}"], scale=0.5)
                            stt(m1[j][:], tA[j][:], 1.0, B[bA][:], ALU.add, ALU.mult, [f"tA{j}", f"BM{bA}"], [f"m1_{j}"])
                            act(tA[j][:], B[bMB][:], AF.Tanh, [f"BM{bMB}"], [f"tA{j}"], scale=0.5)
                            stt(tA[j][:], tA[j][:], 1.0, B[bB][:], ALU.add, ALU.mult, [f"tA{j}", f"BM{bB}"], [f"tA{j}"])
                            tt(mT[:, ec, bs], m1[j][:], tA[j][:], ALU.add, [f"m1_{j}", f"tA{j}"], [f"mT{blk}"], eng="pool")
                    S.barrier()
                    S.emit()
                with ExitStack() as ms2:
                    alloc_wstage(ms2)
                    wo = sbuf(ms2, "wo", [128, 8, 1024], BF16)
                    xr = [sbuf(ms2, f"xr{i}", [128, D], F32) for i in range(2)]
                    ho = [sbuf(ms2, f"ho{i}", [128, D], F32) for i in range(2)]
                    dma(wo[:], wout_d.rearrange("(c p) n -> p c n", p=128), writes=["wo"], q="pool")
                    for t in range(NT):
                        i = t % 2
                        dma(xr[i][:], x[t * 128:(t + 1) * 128, :], writes=[f"xr{i}"])
                        for hf in range(2):
                            bk = (2 * t + hf) % 8
                            for c in range(8):
                                mm(B[bk][:], mT[:, c, t * 128:(t + 1) * 128], wo[:, c, hf * 512:(hf + 1) * 512], c == 0, c == 7,
                                   ["wo"], [f"BM{bk}"])
                            stt(ho[i][:, hf * 512:(hf + 1) * 512], B[bk][:], 0.5, xr[i][:, hf * 512:(hf + 1) * 512], ALU.mult, ALU.add,
                                [f"BM{bk}", f"xr{i}"], [f"ho{i}"])
                        dma(h1s[t * 128:(t + 1) * 128, :], ho[i][:], reads=[f"ho{i}"], writes=["h1s"])
                    S.barrier()
                    S.emit()
                    if stop_after == "M":
                        return nc

        NTILE = 48
        NSLOT = NTILE * 256
        I32 = mybir.dt.int32
        XS = nc.dram_tensor("xs_scr", [NSLOT, D], BF16).ap()
        WS = nc.dram_tensor("ws_scr", [NSLOT, 1], F32).ap()
        YS = nc.dram_tensor("ys_scr", [NSLOT, D], F32).ap()
        TE = nc.dram_tensor("dbg_te" if debug else "te_scr", [128, NTILE], I32, kind="ExternalOutput" if debug else "Internal").ap()
        with ExitStack() as ph:
            hacc = sbuf(ph, "hacc", [128, NT, D], F32)
            s1i = sbuf(ph, "s1i", [128, NT], I32)
            s2i = sbuf(ph, "s2i", [128, NT], I32)
            w1v = sbuf(ph, "w1v", [128, NT], F32)
            w2v = sbuf(ph, "w2v", [128, NT], F32)
            gidx = sbuf(ph, "gidx", [128, NTILE, 8], I32)
            didx = sbuf(ph, "didx", [128, NTILE, 4], I32)
            for t in range(NT):
                dma(hacc[:, t, :], h1s[t * 128:(t + 1) * 128, :], reads=["h1s"], writes=[f"hacc{t}"])
            with ExitStack() as e1:
                xntok = sbuf(e1, "xntok", [128, NT, D], BF16)
                nfrow = sbuf(e1, "nfrow", [128, D], F32)
                misc = sbuf(e1, "misc", [128, 96], F32)
                hs_ = [sbuf(e1, f"hs{i}", [128, D], F32) for i in range(2)]
                sq = sbuf(e1, "sqE", [128, D], F32)
                ssE = sbuf(e1, "ssE", [128, NT], F32)
                rstE = sbuf(e1, "rstE", [128, NT], F32)
                xn32 = [sbuf(e1, f"xn32_{i}", [128, 8, 128], F32) for i in range(2)]
                wr = sbuf(e1, "wr", [128, 8, 36], F32)
                brt = sbuf(e1, "brt", [128, 36], F32)
                lg = sbuf(e1, "lg", [128, NT, 36], F32)
                PT = [psum(e1, f"PTE{i}", [128, 8, 128], F32) for i in range(2)]
                PL = psum(e1, "PLE", [128, 512], F32)
                PS1 = psum(e1, "PTE_rank", [128, 512], F32)
                PS2 = psum(e1, "PTE_cnt", [128, 512], F32)
                dma(wr[:], wr_d.rearrange("(c p) n -> p c n", p=128), writes=["wr"])
                dma(brt[:], br_d.partition_broadcast(128), writes=["brt"])
                dma(nfrow[:], nffnrow_d.partition_broadcast(128), writes=["nfrow"])
                dma(misc[:], misc_d, writes=["misc"])
                memset(ssE[:], 0.0, ["ssE"])
                for t in range(NT):
                    i = t % 2
                    act(sq[:], hacc[:, t, :], AF.Square, ["ssE", f"hacc{t}"], ["sqE", "ssE"], accum_out=ssE[:, t:t + 1])
                    rsqrt_small(rstE[:, t:t + 1], ssE[:, t:t + 1], 1.0 / D, ["ssE"], [f"rstE{t}"])
                    ts(hs_[i][:], hacc[:, t, :], rstE[:, t:t + 1], None, ALU.mult, None, [f"rstE{t}", f"hacc{t}"], [f"hs{i}"])
                    tt(xntok[:, t, :], hs_[i][:], nfrow[:], ALU.mult, [f"hs{i}", "nfrow"], [f"xntok{t}"], eng="pool")
                    for c in range(8):
                        mm(PT[i][:, c, :], hs_[i][:, c * 128:(c + 1) * 128], ident, True, True, [f"hs{i}", "cm"], [f"PTE{i}"])
                    tt(xn32[i][:], PT[i][:], nffn[:, :].unsqueeze(2).to_broadcast([128, 8, 128]), ALU.mult,
                       [f"PTE{i}", "nffn"], [f"xn32_{i}"])
                    for c in range(8):
                        mm(PL[:, 0:36], xn32[i][:, c, :], wr[:, c, :], c == 0, c == 7, [f"xn32_{i}", "wr"], ["PLE"])
                    tt(lg[:, t, :], PL[:, 0:36], brt[:], ALU.add, ["PLE", "brt"], ["lg"])
                gmax = sbuf(e1, "gmax", [128, NT], F32)
                ohg = sbuf(e1, "ohg", [128, NT, 4], F32)
                sh4 = sbuf(e1, "sh4", [128, NT, 4], F32)
                gw = sbuf(e1, "gw", [128, NT], F32)
                M32 = sbuf(e1, "M32", [128, NT, 32], F32)
                oh1 = sbuf(e1, "oh1", [128, NT, 32], F32)
                oh2 = sbuf(e1, "oh2", [128, NT, 32], F32)
                m1v = sbuf(e1, "m1v", [128, NT], F32)
                m2v = sbuf(e1, "m2v", [128, NT], F32)
                L4 = lg[:, :, 0:4]
                L32 = lg[:, :, 4:36]
                bc4 = lambda a: a.unsqueeze(2).to_broadcast([128, NT, 4])
                bc32 = lambda a: a.unsqueeze(2).to_broadcast([128, NT, 32])
                red(gmax[:], L4, ALU.max, ["lg"], ["gmax"])
                tt(ohg[:], L4, bc4(gmax[:, :]), ALU.is_equal, ["lg", "gmax"], ["ohg"])
                tt(sh4[:], L4, bc4(gmax[:, :]), ALU.subtract, ["lg", "gmax"], ["sh4"])
                act(sh4[:], sh4[:], AF.Exp, ["sh4"], ["sh4"])
                red(gw[:], sh4[:], ALU.add, ["sh4"], ["gw"])
                recip(gw[:], gw[:], ["gw"], ["gw"])
                ts(ohg[:], ohg[:], BIG, -BIG, ALU.mult, ALU.add, ["ohg"], ["ohg"])
                tt(M32[:].rearrange("p t (g e) -> p t g e", g=4), L32.rearrange("p t (g e) -> p t g e", g=4),
                   ohg[:, :, :].unsqueeze(3).to_broadcast([128, NT, 4, 8]), ALU.add, ["lg", "ohg"], ["M32"])
                red(m1v[:], M32[:], ALU.max, ["M32"], ["m1v"])
                tt(oh1[:], M32[:], bc32(m1v[:, :]), ALU.is_equal, ["M32", "m1v"], ["oh1"])
                stt(M32[:], oh1[:], -BIG, M32[:], ALU.mult, ALU.add, ["oh1", "M32"], ["M32"])
                red(m2v[:], M32[:], ALU.max, ["M32"], ["m2v"])
                tt(oh2[:], M32[:], bc32(m2v[:, :]), ALU.is_equal, ["M32", "m2v"], ["oh2"])
                tt(w2v[:], m2v[:], m1v[:], ALU.subtract, ["m1v", "m2v"], ["w2v"])
                act(w2v[:], w2v[:], AF.Exp, ["w2v"], ["w2v"])
                ts(w1v[:], w2v[:], 1.0, None, ALU.add, None, ["w2v"], ["w1v"])
                recip(w1v[:], w1v[:], ["w1v"], ["w1v"])
                tt(w2v[:], w2v[:], w1v[:], ALU.mult, ["w2v", "w1v"], ["w2v"])
                tt(w1v[:], w1v[:], gw[:], ALU.mult, ["w1v", "gw"], ["w1v"])
                tt(w2v[:], w2v[:], gw[:], ALU.mult, ["w2v", "gw"], ["w2v"])
                sel = sbuf(e1, "sel", [128, NT, 32], F32)
                tcs = sbuf(e1, "tcs", [128, NT, 32], F32)
                off = sbuf(e1, "off", [128, NT, 32], F32)
                slot = sbuf(e1, "slot", [128, NT, 32], F32)
                cnt = sbuf(e1, "cnt", [128, 32], F32)
                cmp8 = sbuf(e1, "cmp8", [128, 32, 8], F32)
                pfa = sbuf(e1, "pfa", [128, 32], F32)
                pfb = sbuf(e1, "pfb", [128, 32], F32)
                ntl = sbuf(e1, "ntl", [128, 32], F32)
                stt_ = sbuf(e1, "stt_", [128, 32], F32)
                s1f = sbuf(e1, "s1f", [128, NT], F32)
                s2f = sbuf(e1, "s2f", [128, NT], F32)
                A1 = sbuf(e1, "A1", [128, NTILE, 32], F32)
                A2 = sbuf(e1, "A2", [128, NTILE, 32], F32)
                tef = sbuf(e1, "tef", [128, NTILE], F32)
                tei = sbuf(e1, "tei", [128, NTILE], I32)
                thr8 = misc[:, 0:8]
                kk48 = misc[:, 8:56]
                eio = misc[:, 56:88]
                flat = lambda a: a.rearrange("p t e -> p (t e)")
                tt(sel[:], oh1[:], oh2[:], ALU.add, ["oh1", "oh2"], ["sel"])
                mm(PS1[:], cm[:, CM_UT, :], flat(sel[:]), True, True, ["cm", "sel"], ["PTE_rank"])
                mm(PS2[:], cm[:, CM_ONES, :], flat(sel[:]), True, True, ["cm", "sel"], ["PTE_cnt"])
                cp(flat(tcs[:]), PS2[:], ["PTE_cnt"], ["tcs"])
                memset(off[:, 0, :], 0.0, ["off"])
                for t in range(1, NT):
                    tt(off[:, t, :], off[:, t - 1, :], tcs[:, t - 1, :], ALU.add, ["off", "tcs"], ["off"])
                tt(cnt[:], off[:, NT - 1, :], tcs[:, NT - 1, :], ALU.add, ["off", "tcs"], ["cnt"])
                tt(cmp8[:], cnt[:, :].unsqueeze(2).to_broadcast([128, 32, 8]), thr8.unsqueeze(1).to_broadcast([128, 32, 8]),
                   ALU.is_gt, ["cnt", "misc"], ["cmp8"])
                red(ntl[:], cmp8[:], ALU.add, ["cmp8"], ["ntl"])
                cp(pfa[:], ntl[:], ["ntl"], ["pfa"])
                cur, nxt, ck, nk = pfa, pfb, "pfa", "pfb"
                for dd in (1, 2, 4, 8, 16):
                    cp(nxt[:], cur[:], [ck], [nk])
                    tt(nxt[:, dd:32], cur[:, dd:32], cur[:, 0:32 - dd], ALU.add, [ck, nk], [nk])
                    cur, nxt, ck, nk = nxt, cur, nk, ck
                incl, ik = cur, ck
                tt(stt_[:], incl[:], ntl[:], ALU.subtract, [ik, "ntl"], ["stt_"])
                ts(cnt[:], stt_[:], 256.0, None, ALU.mult, None, ["stt_"], ["cnt"])
                tt(off[:], off[:], cnt[:, :].unsqueeze(1).to_broadcast([128, NT, 32]), ALU.add, ["off", "cnt"], ["off"])
                tt(flat(slot[:]), PS1[:], flat(off[:]), ALU.add, ["PTE_rank", "off"], ["slot"])
                tt(sel[:], oh1[:], slot[:], ALU.mult, ["oh1", "slot"], ["sel"])
                red(s1f[:], sel[:], ALU.add, ["sel"], ["s1f"])
                tt(sel[:], oh2[:], slot[:], ALU.mult, ["oh2", "slot"], ["sel"])
                red(s2f[:], sel[:], ALU.add, ["sel"], ["s2f"])
                cp(s1i[:], s1f[:], ["s1f"], ["s1i"])
                cp(s2i[:], s2f[:], ["s2f"], ["s2i"])
                kkb = kk48.unsqueeze(2).to_broadcast([128, NTILE, 32])
                tt(A1[:], kkb, stt_[:, :].unsqueeze(1).to_broadcast([128, NTILE, 32]), ALU.is_ge, ["misc", "stt_"], ["A1"])
                tt(A2[:], kkb, incl[:, :].unsqueeze(1).to_broadcast([128, NTILE, 32]), ALU.is_lt, ["misc", ik], ["A2"])
                tt(A1[:], A1[:], A2[:], ALU.mult, ["A1", "A2"], ["A1"])
                valf = sbuf(e1, "valf", [128, NTILE], F32)
                bgf = sbuf(e1, "bgf", [128, NTILE], F32)
                bdf = sbuf(e1, "bdf", [128, NTILE], F32)
                gidxf = sbuf(e1, "gidxf", [128, NTILE, 8], F32)
                didxf = sbuf(e1, "didxf", [128, NTILE, 4], F32)
                pc8 = misc[:, 88:96]
                red(valf[:], A1[:], ALU.add, ["A1"], ["valf"])
                tt(A1[:], A1[:], eio.unsqueeze(1).to_broadcast([128, NTILE, 32]), ALU.mult, ["A1", "misc"], ["A1"])
                red(tef[:], A1[:], ALU.add, ["A1"], ["tef"])
                cp(tei[:], tef[:], ["tef"], ["tei"])
                dma(TE, tei[:], reads=["tei"], writes=["TE"])
                ts(valf[:], valf[:], 0.0, None, ALU.mult, None, ["valf"], ["valf"])
                stt(bgf[:], tef[:], 1024.0, valf[:], ALU.mult, ALU.add, ["tef", "valf"], ["bgf"])
                stt(bdf[:], tef[:], 512.0, valf[:], ALU.mult, ALU.add, ["tef", "valf"], ["bdf"])
                tt(gidxf[:], bgf[:, :].unsqueeze(2).to_broadcast([128, NTILE, 8]), pc8.unsqueeze(1).to_broadcast([128, NTILE, 8]),
                   ALU.add, ["bgf", "misc"], ["gidxf"])
                tt(didxf[:], bdf[:, :].unsqueeze(2).to_broadcast([128, NTILE, 4]), pc8[:, 0:4].unsqueeze(1).to_broadcast([128, NTILE, 4]),
                   ALU.add, ["bdf", "misc"], ["didxf"])
                cp(gidx[:], gidxf[:], ["gidxf"], ["gidx"])
                cp(didx[:], didxf[:], ["didxf"], ["didx"])
                for t in range(NT):
                    for (si, wv_, nm) in ((s1i, w1v, "a"), (s2i, w2v, "b")):
                        S.add("pool", lambda e, t=t, si=si: e.indirect_dma_start(
                            out=XS[:, :], out_offset=bass.IndirectOffsetOnAxis(ap=si[:, t:t + 1], axis=0),
                            in_=xntok[:, t, :], in_offset=None),
                            reads=[f"xntok{t}", "s1i", "s2i"], writes=["XS"], dma=True)
                if debug:
                    dbg["s12"] = nc.dram_tensor("dbg_s12", [128, 2 * NT], I32, kind="ExternalOutput").ap()
                    dma(dbg["s12"][:, 0:NT], s1i[:], reads=["s1i"], writes=["dbgs12"])
                    dma(dbg["s12"][:, NT:2 * NT], s2i[:], reads=["s2i"], writes=["dbgs12"])
                S.barrier()
                S.emit()
                if stop_after == "router":
                    return nc
            with ExitStack() as e2:
                NB = 3
                ewg = [sbuf(e2, f"ewg{i}", [128, 8, 512], BF16) for i in range(NB)]
                ewu = [sbuf(e2, f"ewu{i}", [128, 8, 512], BF16) for i in range(NB)]
                ewd = [sbuf(e2, f"ewd{i}", [128, 4, 1024], BF16) for i in range(NB)]
                xst = [sbuf(e2, f"xst{i}", [128, 2, D], BF16) for i in range(2)]
                wst_ = [sbuf(e2, f"wsl{i}", [128, 2], F32) for i in range(2)]
                xT = [sbuf(e2, f"xT{i}", [128, 8, 256], BF16) for i in range(2)]
                hidT = [sbuf(e2, f"hidT{i}", [128, 4, 256], BF16) for i in range(2)]
                sg = [sbuf(e2, f"sg{i}", [128, 256], BF16) for i in range(2)]
                yt = [sbuf(e2, f"yt{i}", [128, 2, D], F32) for i in range(2)]
                PTk = [psum(e2, f"BEt{i}", [128, 8, 128], BF16) for i in range(2)]
                Bgu = [psum(e2, f"BEg{i}", [128, 512], F32) for i in range(4)]
                Bd = [psum(e2, f"BEd{i}", [128, 512], F32) for i in range(2)]
                wg_flat = wg_d.rearrange("e d n -> (e d) n")
                wu_flat = wu_d.rearrange("e d n -> (e d) n")
                wd_flat = wd_d.rearrange("e f n -> (e f) n")

                def gather_w(dst, src_flat, idx_ap, bound, key):
                    S.add("pool", lambda e: e.indirect_dma_start(
                        out=dst, out_offset=None, in_=src_flat[:, :],
                        in_offset=bass.IndirectOffsetOnAxis(ap=idx_ap, axis=0)),
                        reads=["gidx", "didx"], writes=[key], dma=True)

                dcount = [0]
                for k in range(NTILE):
                    b3 = k % NB
                    b2 = k % 2
                    rs = slice(k * 256, (k + 1) * 256)
                    for c in range(8):
                        gather_w(ewg[b3][:, c, :], wg_flat, gidx[:, k, c:c + 1], 32 * 1024 - 1, f"ewg{b3}_{c}")
                    for c in range(8):
                        gather_w(ewu[b3][:, c, :], wu_flat, gidx[:, k, c:c + 1], 32 * 1024 - 1, f"ewu{b3}_{c}")
                    for c in range(4):
                        gather_w(ewd[b3][:, c, :], wd_flat, didx[:, k, c:c + 1], 32 * 512 - 1, f"ewd{b3}_{c}")
                    dma(xst[b2][:], XS[rs, :].rearrange("(h p) d -> p h d", p=128), reads=["XS"], writes=[f"xst{b2}"])
                    for hh in range(2):
                        for c in range(8):
                            tr(PTk[hh][:, c, :], xst[b2][:, hh, c * 128:(c + 1) * 128], identb[:], [f"xst{b2}", "identb"], [f"BEt{hh}"])
                        if hh == 0:
                            cp(xT[b2][:, :, 0:128], PTk[hh][:], [f"BEt{hh}"], [f"xT{b2}"])
                        else:
                            act(xT[b2][:, :, 128:256], PTk[hh][:], AF.Copy, [f"BEt{hh}"], [f"xT{b2}"])
                    for f in range(4):
                        j = f % 2
                        bg_, bu_ = Bgu[2 * j], Bgu[2 * j + 1]
                        for c in range(8):
                            mm(bg_[:, 0:256], ewg[b3][:, c, f * 128:(f + 1) * 128], xT[b2][:, c, :], c == 0, c == 7,
                               [f"ewg{b3}_{c}", f"xT{b2}"], [f"BEg{2 * j}"])
                        for c in range(8):
                            mm(bu_[:, 0:256], ewu[b3][:, c, f * 128:(f + 1) * 128], xT[b2][:, c, :], c == 0, c == 7,
                               [f"ewu{b3}_{c}", f"xT{b2}"], [f"BEg{2 * j + 1}"])
                        act(sg[j][:], bg_[:, 0:256], AF.Silu, [f"BEg{2 * j}"], [f"sg{j}"])
                        tt(hidT[b2][:, f, :], bu_[:, 0:256], sg[j][:], ALU.mult, [f"BEg{2 * j + 1}", f"sg{j}"], [f"hidT{b2}"])
                    for hh in range(2):
                        for cc in range(2):
                            bk = dcount[0] % 2
                            dcount[0] += 1
                            for f in range(4):
                                mm(Bd[bk][:], hidT[b2][:, f, hh * 128:(hh + 1) * 128], ewd[b3][:, f, cc * 512:(cc + 1) * 512],
                                   f == 0, f == 3, [f"ewd{b3}_{f}", f"hidT{b2}"], [f"BEd{bk}"])
                            if bk == 0:
                                cp(yt[b2][:, hh, cc * 512:(cc + 1) * 512], Bd[bk][:], [f"BEd{bk}"], [f"yt{b2}"])
                            else:
                                act(yt[b2][:, hh, cc * 512:(cc + 1) * 512], Bd[bk][:], AF.Copy, [f"BEd{bk}"], [f"yt{b2}"])
                    dma(YS[rs, :].rearrange("(h p) d -> p h d", p=128), yt[b2][:], reads=[f"yt{b2}"], writes=["YS"])
                S.barrier()
                S.emit()
                if stop_after == "experts":
                    return nc
            with ExitStack() as e3:
                nfw = sbuf(e3, "nfw", [128, D], F32)
                sq = sbuf(e3, "sqF", [128, D], F32)
                ssF = sbuf(e3, "ssF", [128, NT], F32)
                rstF = sbuf(e3, "rstF", [128, NT], F32)
                y1 = [sbuf(e3, f"y1_{i}", [128, D], F32) for i in range(2)]
                y2 = [sbuf(e3, f"y2_{i}", [128, D], F32) for i in range(2)]
                ob = [sbuf(e3, f"ob{i}", [128, D], F32) for i in range(2)]
                dma(nfw[:], nfin_d.partition_broadcast(128), writes=["nfw"])
                memset(ssF[:], 0.0, ["ssF"])
                for t in range(NT):
                    i = t % 2
                    S.add("pool", lambda e, t=t, i=i: e.indirect_dma_start(
                        out=y1[i][:, :], out_offset=None, in_=YS[:, :],
                        in_offset=bass.IndirectOffsetOnAxis(ap=s1i[:, t:t + 1], axis=0)),
                        reads=["YS", "s1i"], writes=[f"y1_{i}"], dma=True)
                    S.add("pool", lambda e, t=t, i=i: e.indirect_dma_start(
                        out=y2[i][:, :], out_offset=None, in_=YS[:, :],
                        in_offset=bass.IndirectOffsetOnAxis(ap=s2i[:, t:t + 1], axis=0)),
                        reads=["YS", "s2i"], writes=[f"y2_{i}"], dma=True)
                    stt(hacc[:, t, :], y1[i][:], w1v[:, t:t + 1], hacc[:, t, :], ALU.mult, ALU.add, [f"y1_{i}", f"hacc{t}", "w1v"], [f"hacc{t}"])
                    stt(hacc[:, t, :], y2[i][:], w2v[:, t:t + 1], hacc[:, t, :], ALU.mult, ALU.add, [f"y2_{i}", f"hacc{t}", "w2v"], [f"hacc{t}"])
                    act(sq[:], hacc[:, t, :], AF.Square, ["ssF", f"hacc{t}"], ["sqF", "ssF"], accum_out=ssF[:, t:t + 1])
                    rsqrt_small(rstF[:, t:t + 1], ssF[:, t:t + 1], 1.0 / D, ["ssF"], [f"rstF{t}"])
                    stt(ob[i][:], hacc[:, t, :], rstF[:, t:t + 1], nfw[:], ALU.mult, ALU.mult, [f"rstF{t}", "nfw", f"hacc{t}"], [f"ob{i}"])
                    dma(out[t * 128:(t + 1) * 128, :], ob[i][:], reads=[f"ob{i}"], writes=["out"])
                S.barrier()
                S.emit()
    return nc


_CACHE = {}


def _host_inputs(inputs):
    f = lambda a: np.ascontiguousarray(np.asarray(a, dtype=np.float32))
    w_in = f(inputs["w_in"][0])
    perm = np.arange(1024) ^ 1
    w_perm = np.ascontiguousarray(w_in[:, 2056:3080][:, perm])
    cw = np.ascontiguousarray(f(inputs["conv_w"][0]).T.reshape(12, 128, 4).transpose(1, 0, 2))
    cm, ret_dt, retvec, cdec, cosT, sinT = make_consts()
    shared = {
        "w_in": w_in, "w_perm": w_perm, "cw": cw,
        "A_log": f(inputs["A_log"][0]), "dt_bias": f(inputs["dt_bias"][0]),
        "gdn_norm_w": f(inputs["gdn_norm_w"][0]),
        "w_up_gdn": f(inputs["w_up_gdn"][0]), "w_up_ret": f(inputs["w_up_ret"][0]), "w_out": f(inputs["w_out"][0]),
        "nmix": np.ascontiguousarray(f(inputs["norm_mix_w"][0]).reshape(8, 128).T),
        "nffn": np.ascontiguousarray(f(inputs["norm_ffn_w"][0]).reshape(8, 128).T),
        "w_router": np.ascontiguousarray(np.concatenate([f(inputs["w_group"][0]), f(inputs["w_expert"][0])], axis=1)),
        "b_router": np.ascontiguousarray(np.concatenate([f(inputs["b_group"][0]), f(inputs["b_expert"][0])], axis=0)),
        "w_gate": f(inputs["w_gate"][0]), "w_up": f(inputs["w_up"][0]), "w_down": f(inputs["w_down"][0]),
        "norm_final_w": f(inputs["norm_final_w"]),
        "misc": np.ascontiguousarray(np.concatenate([
            np.broadcast_to(np.concatenate([256.0 * np.arange(8), np.arange(48), np.arange(32)]).astype(np.float32)[None, :], (128, 88)),
            (np.arange(8)[None, :] * 128 + np.arange(128)[:, None]).astype(np.float32)], axis=1)),
        "nffn_row": f(inputs["norm_ffn_w"][0]),
        "cm": cm, "ret_dt": ret_dt, "retvec": retvec, "cosT": cosT, "sinT": sinT,
    }
    return shared


def kernel(**inputs):
    x = np.asarray(inputs["x"], dtype=np.float32)
    nb = x.shape[0]
    shared = _host_inputs(inputs)
    nc = build()
    in_maps = []
    for b in range(nb):
        m = dict(shared)
        m["x"] = np.ascontiguousarray(x[b])
        in_maps.append(m)
    res = run_bass_kernel_spmd(nc, in_maps, core_ids=list(range(nb)))
    return np.stack([np.asarray(r["out"], dtype=np.float32) for r in res.results], axis=0)
```
